# Optimizing a Trainium2 kernel written in Bass

```python
import jax, jax.numpy as jnp
from jax import lax
import numpy as np

D_MODEL = 1024
BATCH = 4
SEQ = 8192
DEPTH = 2

D_CONV = 512
CONV_WIDTH = 31
D_POOL = 512
POOL_WINDOWS = (2, 4, 8, 16)
N_POOL_GROUPS = 4
POOL_GROUP = D_POOL // N_POOL_GROUPS
POOL_OUT_GROUP = D_MODEL // N_POOL_GROUPS
D_SGU = 512
SGU_HEADS = 4
SGU_HEAD_DIM = D_SGU // SGU_HEADS
CHUNK = 128
N_BRANCHES = 3
SPLIT_A = 2 * D_CONV
SPLIT_B = SPLIT_A + D_POOL
SPLIT_U = SPLIT_B + D_SGU
SPLIT_V = SPLIT_U + D_SGU
D_IN = SPLIT_V + N_BRANCHES * D_MODEL
D_FF = 2816
N_EXPERTS = 8
TOP_K = 2
D_EXPERT = 3584
MOE_BLOCK = 512
N_DENSE = (DEPTH + 1) // 2
N_MOE = DEPTH // 2
EPS = 1e-6

kernel_name = 'hybrid_conv_pool_sgu_moe_block'


def rmsnorm(x, g):
    xf = x.astype(jnp.float32)
    y = xf * lax.rsqrt(jnp.mean(xf * xf, axis=-1, keepdims=True) + EPS)
    return (y * g.astype(jnp.float32)).astype(x.dtype)


def layernorm(x, g, b):
    xf = x.astype(jnp.float32)
    mu = jnp.mean(xf, axis=-1, keepdims=True)
    xc = xf - mu
    var = jnp.mean(xc * xc, axis=-1, keepdims=True)
    y = xc * lax.rsqrt(var + EPS) * g.astype(jnp.float32) + b.astype(jnp.float32)
    return y.astype(x.dtype)


def conformer_conv(a_in, w_dw, b_dw, ln_g, ln_b, w_o):
    val, gate = jnp.split(a_in, 2, axis=-1)
    a = val * jax.nn.sigmoid(gate)
    a = lax.conv_general_dilated(
        a, w_dw[:, None, :], window_strides=(1,),
        padding=[(CONV_WIDTH - 1, 0)],
        dimension_numbers=('NWC', 'WIO', 'NWC'),
        feature_group_count=D_CONV) + b_dw
    a = jax.nn.silu(layernorm(a, ln_g, ln_b))
    return a @ w_o


def multiscale_pool(p, w_grp, scale):
    b_, s_, _ = p.shape
    pf = p.astype(jnp.float32)
    csum = jnp.cumsum(pf, axis=1)
    count = jnp.arange(1, s_ + 1, dtype=jnp.float32)[:, None]
    outs = []
    for g, w in enumerate(POOL_WINDOWS):
        sl = slice(g * POOL_GROUP, (g + 1) * POOL_GROUP)
        c = csum[..., sl]
        c_prev = jnp.pad(c, ((0, 0), (w, 0), (0, 0)))[:, :s_]
        mean = (c - c_prev) / jnp.minimum(count, float(w))
        outs.append(mean - pf[..., sl])
    pooled = jnp.stack(outs, axis=2).astype(p.dtype)
    y = jnp.einsum('bsgc,gcd->bsgd', pooled, w_grp).reshape(b_, s_, D_MODEL)
    return y * scale


def spatial_gating(u, v, ln_g, ln_b, w_s, b_s, w_o):
    b_, s_, _ = u.shape
    n_chunks = s_ // CHUNK
    vn = layernorm(v, ln_g, ln_b).reshape(b_, n_chunks, CHUNK, SGU_HEADS, SGU_HEAD_DIM)
    causal = jnp.tril(jnp.ones((CHUNK, CHUNK), dtype=bool))
    w = jnp.where(causal, w_s, 0)
    s = jnp.einsum('hij,bcjhd->bcihd', w, vn) + b_s.T[:, :, None]
    s = s.reshape(b_, s_, D_SGU)
    return (u * s) @ w_o


def swiglu(h, w1, w3, w2):
    return (jax.nn.silu(h @ w1) * (h @ w3)) @ w2


def moe_swiglu(h, w_router, w1, w3, w2):
    b_, s_, d = h.shape
    n_tok = b_ * s_
    n_assign = n_tok * TOP_K
    ht = h.reshape(n_tok, d)
    logits = (ht @ w_router).astype(jnp.float32)
    top_v, top_i = lax.top_k(logits, TOP_K)
    top_w = jax.nn.softmax(top_v, axis=-1)
    flat_e = top_i.reshape(-1)
    flat_w = top_w.reshape(-1)
    flat_tok = jnp.repeat(jnp.arange(n_tok, dtype=jnp.int32), TOP_K)
    order = jnp.argsort(flat_e)
    sorted_e = flat_e[order]
    counts = jnp.zeros((N_EXPERTS,), jnp.int32).at[flat_e].add(1)
    padded = (counts + MOE_BLOCK - 1) // MOE_BLOCK * MOE_BLOCK
    start = jnp.cumsum(counts) - counts
    pend = jnp.cumsum(padded)
    pstart = pend - padded
    rank = jnp.arange(n_assign, dtype=jnp.int32) - start[sorted_e]
    dest = pstart[sorted_e] + rank
    n_rows = -(-n_assign // MOE_BLOCK) * MOE_BLOCK + N_EXPERTS * MOE_BLOCK
    n_blocks = n_rows // MOE_BLOCK
    row_tok = jnp.zeros((n_rows,), jnp.int32).at[dest].set(flat_tok[order])
    row_w = jnp.zeros((n_rows,), jnp.float32).at[dest].set(flat_w[order])
    block_start = jnp.arange(n_blocks, dtype=jnp.int32) * MOE_BLOCK
    block_e = jnp.minimum(jnp.searchsorted(pend, block_start, side='right'), N_EXPERTS - 1)
    xb = ht[row_tok].reshape(n_blocks, MOE_BLOCK, d)

    def expert_block(args):
        xblk, e = args
        return swiglu(xblk, w1[e], w3[e], w2[e])

    yb = lax.map(expert_block, (xb, block_e)).reshape(n_rows, d)
    out = jnp.zeros((n_tok, d), jnp.float32).at[row_tok].add(yb.astype(jnp.float32) * row_w[:, None])
    return out.astype(h.dtype).reshape(b_, s_, d)


def setup_inputs(seed: int = 0) -> dict:
    key = jax.random.key(seed)
    ks = jax.random.split(key, 32)
    f32 = jnp.float32

    def nrm(k, shape, scale):
        return jax.random.normal(k, shape, f32) * scale

    def gain(k, shape):
        return 1.0 + 0.02 * jax.random.normal(k, shape, f32)

    return {
        'x': jax.random.normal(ks[0], (BATCH, SEQ, D_MODEL), f32),
        'mix_norm_g': gain(ks[1], (DEPTH, D_MODEL)),
        'w_in': nrm(ks[2], (DEPTH, D_MODEL, D_IN), D_MODEL ** -0.5),
        'conv_w': nrm(ks[3], (DEPTH, CONV_WIDTH, D_CONV), CONV_WIDTH ** -0.5),
        'conv_b': nrm(ks[4], (DEPTH, D_CONV), 0.02),
        'conv_ln_g': gain(ks[5], (DEPTH, D_CONV)),
        'conv_ln_b': nrm(ks[6], (DEPTH, D_CONV), 0.02),
        'conv_w_out': nrm(ks[7], (DEPTH, D_CONV, D_MODEL), D_CONV ** -0.5),
        'pool_w': nrm(ks[8], (DEPTH, N_POOL_GROUPS, POOL_GROUP, POOL_OUT_GROUP), POOL_GROUP ** -0.5),
        'pool_scale': gain(ks[9], (DEPTH, D_MODEL)),
        'sgu_ln_g': gain(ks[10], (DEPTH, D_SGU)),
        'sgu_ln_b': nrm(ks[11], (DEPTH, D_SGU), 0.02),
        'sgu_w_s': nrm(ks[12], (DEPTH, SGU_HEADS, CHUNK, CHUNK), CHUNK ** -0.5),
        'sgu_b_s': gain(ks[13], (DEPTH, SGU_HEADS, CHUNK)),
        'sgu_w_out': nrm(ks[14], (DEPTH, D_SGU, D_MODEL), D_SGU ** -0.5),
        'w_out': nrm(ks[15], (DEPTH, D_MODEL, D_MODEL), D_MODEL ** -0.5),
        'ffn_norm_g': gain(ks[16], (DEPTH, D_MODEL)),
        'dense_w1': nrm(ks[17], (N_DENSE, D_MODEL, D_FF), D_MODEL ** -0.5),
        'dense_w3': nrm(ks[18], (N_DENSE, D_MODEL, D_FF), D_MODEL ** -0.5),
        'dense_w2': nrm(ks[19], (N_DENSE, D_FF, D_MODEL), D_FF ** -0.5),
        'router_w': nrm(ks[20], (N_MOE, D_MODEL, N_EXPERTS), D_MODEL ** -0.5),
        'expert_w1': nrm(ks[21], (N_MOE, N_EXPERTS, D_MODEL, D_EXPERT), D_MODEL ** -0.5),
        'expert_w3': nrm(ks[22], (N_MOE, N_EXPERTS, D_MODEL, D_EXPERT), D_MODEL ** -0.5),
        'expert_w2': nrm(ks[23], (N_MOE, N_EXPERTS, D_EXPERT, D_MODEL), D_EXPERT ** -0.5),
        'final_norm_g': gain(ks[24], (D_MODEL,)),
    }


def reference(x, mix_norm_g, w_in, conv_w, conv_b, conv_ln_g, conv_ln_b, conv_w_out,
              pool_w, pool_scale, sgu_ln_g, sgu_ln_b, sgu_w_s, sgu_b_s, sgu_w_out,
              w_out, ffn_norm_g, dense_w1, dense_w3, dense_w2,
              router_w, expert_w1, expert_w3, expert_w2, final_norm_g):
    for layer in range(DEPTH):
        h = rmsnorm(x, mix_norm_g[layer])
        proj = h @ w_in[layer]
        a_in, p_in, u, v, gate_logits = jnp.split(proj, [SPLIT_A, SPLIT_B, SPLIT_U, SPLIT_V], axis=-1)
        gates = jax.nn.sigmoid(gate_logits)
        y_a = conformer_conv(a_in, conv_w[layer], conv_b[layer], conv_ln_g[layer],
                             conv_ln_b[layer], conv_w_out[layer])
        y_b = multiscale_pool(p_in, pool_w[layer], pool_scale[layer])
        y_c = spatial_gating(u, v, sgu_ln_g[layer], sgu_ln_b[layer], sgu_w_s[layer],
                             sgu_b_s[layer], sgu_w_out[layer])
        mixed = (gates[..., :D_MODEL] * y_a
                 + gates[..., D_MODEL:2 * D_MODEL] * y_b
                 + gates[..., 2 * D_MODEL:] * y_c)
        x = x + mixed @ w_out[layer]
        h = rmsnorm(x, ffn_norm_g[layer])
        idx = layer // 2
        if layer % 2 == 0:
            x = x + swiglu(h, dense_w1[idx], dense_w3[idx], dense_w2[idx])
        else:
            x = x + moe_swiglu(h, router_w[idx], expert_w1[idx], expert_w3[idx], expert_w2[idx])
    return rmsnorm(x, final_norm_g)
```

```python
from contextlib import ExitStack

import numpy as np
import concourse.bass as bass
import concourse.mybir as mybir
from concourse.bass_utils import run_bass_kernel_spmd

F32 = mybir.dt.float32
BF16 = mybir.dt.bfloat16
I32 = mybir.dt.int32
AF = mybir.ActivationFunctionType
ALU = mybir.AluOpType

NCORES = 8
D = 1024
KD = 8
T_OWN = 4096
HALO = 128
TT = T_OWN + HALO
D_IN = 5632
SPLIT_V = 2560
D_FF = 2816
D_EXP = 3584
NEXP = 8
CAP = 1536
NSLOT = NEXP * CAP
EPS = 1e-6
BIGIDX = float(1 << 20)

COMPUTE = ("pe", "act", "dve", "pool")
ALL_ENG = ("pe", "act", "dve", "pool", "sp")


class Sched:
    def __init__(self, nc):
        self.nc = nc
        self.streams = {e: [] for e in ALL_ENG}
        self.sem = {}
        self.cnt = {}
        for e in COMPUTE:
            self.sem[e] = nc.alloc_semaphore("prog_" + e)
            self.cnt[e] = 0
        self.known = {e: {} for e in ALL_ENG}
        self.last_write = {}
        self.readers = {}
        self.lanes = {}
        self.free_sems = {"pool": [], "sp": []}
        self.lane_q = {}
        self.nsem = 0

    def lane(self, name, queue="sp"):
        queue = "pool" if queue == "pool" else "sp"
        if name not in self.lanes:
            if self.free_sems[queue]:
                self.lanes[name] = self.free_sems[queue].pop()
            else:
                self.nsem += 1
                self.lanes[name] = [self.nc.alloc_semaphore(f"ln{self.nsem}"), 0]
            self.lane_q[name] = queue
        assert self.lane_q[name] == queue, (name, queue)
        return self.lanes[name]

    def _deps(self, reads, writes):
        deps = {}

        def add(d):
            if d is None:
                return
            k, v = d
            if deps.get(k, 0) < v:
                deps[k] = v

        for k in reads:
            add(self.last_write.get(k))
        for k in writes:
            add(self.last_write.get(k))
            for rk, rv in self.readers.get(k, {}).items():
                add((rk, rv))
        return deps

    def _emit_waits(self, eng, deps):
        st = self.streams[eng]
        kn = self.known[eng]
        for k, v in deps.items():
            if kn.get(k, 0) >= v:
                continue
            kn[k] = v
            st.append(("wait", self.semof(k), v))

    def _record(self, key, reads, writes):
        sk, sv = key
        for k in reads:
            r = self.readers.setdefault(k, {})
            if r.get(sk, 0) < sv:
                r[sk] = sv
        for k in writes:
            self.last_write[k] = key
            self.readers[k] = {}

    def op(self, eng, emit, reads=(), writes=()):
        deps = self._deps(reads, writes)
        sk = ("c", eng)
        self._emit_waits(eng, deps)
        self.cnt[eng] += 1
        self.streams[eng].append(("op", emit, self.semof(sk)))
        self._record((sk, self.cnt[eng]), reads, writes)

    def dma(self, queue, lane, emit, reads=(), writes=()):
        ln = self.lane(lane, queue)
        sk = ("l", lane)
        deps = self._deps(reads, writes)
        if ln[1] > 0 and deps.get(sk, 0) < ln[1]:
            deps[sk] = ln[1]
        self._emit_waits(queue, deps)
        ln[1] += 16
        self.streams[queue].append(("dma", emit, ln[0]))
        self._record((sk, ln[1]), reads, writes)

    def barrier(self):
        deps = {("c", e): self.cnt[e] for e in COMPUTE if self.cnt[e] > 0}
        for name, ln in self.lanes.items():
            if ln[1] > 0:
                deps[("l", name)] = ln[1]
        for eng in ALL_ENG:
            self._emit_waits(eng, dict(deps))
        for name, ln in self.lanes.items():
            self.free_sems[self.lane_q[name]].append(ln)
        self.lanes = {}
        self.lane_q = {}
        for eng in ALL_ENG:
            self.known[eng] = {k: v for k, v in self.known[eng].items() if k[0] == "c"}
        self.last_write = {k: v for k, v in self.last_write.items() if v[0][0] == "c"}
        for k in list(self.readers.keys()):
            self.readers[k] = {rk: rv for rk, rv in self.readers[k].items() if rk[0] == "c"}

    def final_wait(self, eng, keys):
        deps = {}
        for k in keys:
            d = self.last_write.get(k)
            if d is not None and deps.get(d[0], 0) < d[1]:
                deps[d[0]] = d[1]
        self._emit_waits(eng, deps)

    def semof(self, k):
        if k[0] == "c":
            return self.sem[k[1]]
        return self.lanes[k[1]][0]

    def emit(self):
        nc = self.nc
        with nc.Block() as block:
            def run(e, stream):
                for item in stream:
                    if item[0] == "wait":
                        e.wait_ge(item[1], item[2])
                    elif item[0] == "op":
                        item[1](e).then_inc(item[2], 1)
                    else:
                        item[1](e).then_inc(item[2], 16)

            @block.tensor
            def _(e):
                run(e, self.streams["pe"])

            @block.scalar
            def _(e):
                run(e, self.streams["act"])

            @block.vector
            def _(e):
                run(e, self.streams["dve"])

            @block.gpsimd
            def _(e):
                with e.register("bnd") as reg, e.register("bnd2") as reg2:
                    e.reg_mov(reg, NSLOT - 1)
                    e.reg_mov(reg2, NSLOT + 127)
                    self.bnd = reg
                    self.bnd2 = reg2
                    run(e, self.streams["pool"])

            @block.sync
            def _(e):
                run(e, self.streams["sp"])


def tiles_of():
    res = [(0, HALO)]
    for i in range(T_OWN // 512):
        res.append((HALO + i * 512, 512))
    return res


class Builder:
    def __init__(self, phases=99, debug=False):
        self.phases = phases
        self.debug = debug
        nc = bass.Bass("TRN2", target_bir_lowering=False)
        self.nc = nc
        self.S = Sched(nc)
        self.psn = 0
        self.ps = [nc.alloc_psum_tensor(f"psb{i}", [128, 512], F32) for i in range(8)]
        self.dram_in = {}
        self.uid = 0

    def din(self, name, shape, dt=F32):
        t = self.nc.dram_tensor(name, list(shape), dt, kind="ExternalInput").ap()
        self.dram_in[name] = t
        return t

    def dscr(self, name, shape, dt):
        kind = "ExternalOutput" if self.debug else "Internal"
        if self.debug:
            return self.nc.dram_tensor(name, list(shape), dt, kind="ExternalOutput").ap()
        return self.nc.dram_tensor(name, list(shape), dt).ap()

    def PS(self):
        i = self.psn % 8
        self.psn += 1
        return self.ps[i], f"ps{i}"

    def lname(self, base):
        self.uid += 1
        return f"{base}"

    def mm_group(self, out_ap, pairs, reads, writes):
        n = len(pairs)

        def emit(e, pairs=pairs, out_ap=out_ap, n=n):
            ins = None
            for j, (l, r) in enumerate(pairs):
                ins = e.matmul(out_ap, lhsT=l, rhs=r, start=(j == 0), stop=(j == n - 1))
            return ins

        self.S.op("pe", emit, reads=reads, writes=writes)

    def build(self):
        nc, S = self.nc, self.S
        din = self.din
        xT = din("xT", [D, TT])
        hmask = din("hmask", [128, 1])
        rc_d = din("rc", [128, 64])
        ident_d = din("ident", [128, 128])
        tril_d = din("tril", [128, 128])
        tri_d = din("tri", [128, 128])
        eoff_d = din("eoff", [128, 256])
        gmix_d = din("gmix", [128, 16])
        gffn_d = din("gffn", [128, 16])
        convw_d = din("convw", [128, 2 * 4 * 31])
        convb_d = din("convb", [128, 8])
        clng_d = din("clng", [128, 8])
        clnb_d = din("clnb", [128, 8])
        pscale_d = din("pscale", [128, 16])
        slng_d = din("slng", [128, 1024])
        slnb_d = din("slnb", [128, 1024])
        wsT_d = din("wsT", [128, 2 * 4 * 128])
        bs_d = din("bs", [1, 2 * 4 * 128])
        fing_d = din("fing", [128, 1024])
        wr_d = din("wr", [128, 64])
        w_in = din("w_in", [2, D, D_IN])
        conv_w_out = din("conv_w_out", [2, 512, D])
        pool_w = din("pool_w", [2, 4, 128, 256])
        sgu_w_out = din("sgu_w_out", [2, 512, D])
        w_out = din("w_out", [2, D, D])
        dense_w1 = din("dense_w1", [1, D, D_FF])
        dense_w3 = din("dense_w3", [1, D, D_FF])
        dense_w2 = din("dense_w2", [1, D_FF, D])
        expert_w1 = din("expert_w1", [1, NEXP, D, D_EXP])
        expert_w3 = din("expert_w3", [1, NEXP, D, D_EXP])
        expert_w2 = din("expert_w2", [1, NEXP, D_EXP, D])
        out_d = nc.dram_tensor("out", [T_OWN, D], F32, kind="ExternalOutput").ap()
        hT_s = self.dscr("hT_s", [D, TT], BF16)
        T_s = self.dscr("T_s", [D, TT], F32)
        xm_s = [self.dscr("xm0_s", [D, TT], F32), self.dscr("xm1_s", [D, TT], F32)]
        h2T_s = self.dscr("h2T_s", [D, TT], BF16)
        xf0_s = self.dscr("xf0_s", [D, TT], F32)
        xg_s = self.dscr("xg_s", [NSLOT, D], BF16)
        ys_s = self.dscr("ys_s", [NSLOT + 128, D], F32)
        cnt_s = self.dscr("cnt_s", [128, 8], F32)

        with ExitStack() as gs:
            def sb(name, shape, dt, es=gs):
                return es.enter_context(nc.sbuf_tensor("g_" + name, list(shape), dt))

            ident_f = sb("ident_f", [128, 128], F32)
            ident_b = sb("ident_b", [128, 128], BF16)
            ones_b = sb("ones_b", [128, 128], BF16)
            hmask_t = sb("hmask_t", [128, 1], F32)
            rc_t = sb("rc_t", [128, 64], F32)
            gmix = sb("gmix", [128, 16], F32)
            gffn = sb("gffn", [128, 16], F32)
            convw = sb("convw", [128, 248], F32)
            convb = sb("convb", [128, 8], F32)
            clng = sb("clng", [128, 8], F32)
            clnb = sb("clnb", [128, 8], F32)
            pscale = sb("pscale", [128, 16], F32)
            smalls = [(ident_f, ident_d), (hmask_t, hmask), (rc_t, rc_d), (gmix, gmix_d), (gffn, gffn_d),
                      (convw, convw_d), (convb, convb_d), (clng, clng_d), (clnb, clnb_d), (pscale, pscale_d)]
            for j, (t, d_) in enumerate(smalls):
                S.dma("sp", "cst", lambda e, t=t, d_=d_: e.dma_start(out=t[:], in_=d_), writes=[f"cst{j}"])
            CST = [f"cst{j}" for j in range(len(smalls))]
            S.op("dve", lambda e: e.tensor_copy(out=ident_b[:], in_=ident_f[:]), reads=CST, writes=["ident_b"])
            S.op("dve", lambda e: e.memset(ones_b[:], 1.0), writes=["ones_b"])
            CST += ["ident_b", "ones_b"]
            self.CST = CST
            self.c = dict(ident_f=ident_f, ident_b=ident_b, ones_b=ones_b, hmask=hmask_t, rc=rc_t, gmix=gmix,
                          gffn=gffn, convw=convw, convb=convb, clng=clng, clnb=clnb, pscale=pscale)

            tiles = tiles_of()
            ph = 0
            for layer in range(2):
                xsrc = xT if layer == 0 else xf0_s
                xsrc_key = "xT" if layer == 0 else "xf0"
                ph += 1
                if self.phases >= ph:
                    self.pass_a(layer, tiles, xsrc, xsrc_key, w_in, conv_w_out, hT_s, T_s)
                    S.barrier()
                ph += 1
                if self.phases >= ph:
                    self.pass_b(layer, tiles, w_in, pool_w, hT_s, T_s)
                    S.barrier()
                ph += 1
                if self.phases >= ph:
                    self.pass_c(layer, tiles, xsrc, xsrc_key, w_in, sgu_w_out, w_out, slng_d, slnb_d, wsT_d, bs_d,
                                tril_d, hT_s, T_s, xm_s[layer], h2T_s)
                    S.barrier()
                ph += 1
                if self.phases >= ph:
                    if layer == 0:
                        self.dense_ffn(tiles, dense_w1, dense_w3, dense_w2, h2T_s, xm_s[0], xf0_s, xg_s, ys_s)
                        S.barrier()
                    else:
                        self.moe(expert_w1, expert_w3, expert_w2, wr_d, tri_d, eoff_d, fing_d, h2T_s, xm_s[1],
                                 xg_s, ys_s, cnt_s, out_d)
            S.final_wait("sp", list(S.last_write.keys()))
        S.emit()
        return nc

    def norm_fm(self, xt, xkey, N, gcol, sq, sd, rs, h, hkey):
        S = self.S
        ones_b = self.c["ones_b"]
        for c in range(KD):
            S.op("act", lambda e, c=c: e.activation(out=sq[:, c, :N], in_=xt[:, c, :N], func=AF.Square),
                 reads=[xkey], writes=[f"sq{c}"])
        ps, pk = self.PS()
        self.mm_group(ps[:, :N], [(ones_b[:], sq[:, c, :N]) for c in range(KD)],
                      reads=[f"sq{c}" for c in range(KD)] + ["ones_b"], writes=[pk])
        S.op("act", lambda e: e.activation(out=sd[:, :N], in_=ps[:, :N], func=AF.Sqrt, scale=1.0 / D, bias=EPS),
             reads=[pk], writes=["sd"])
        S.op("dve", lambda e: e.reciprocal(out=rs[:, :N], in_=sd[:, :N]), reads=["sd"], writes=["rs"])
        for c in range(KD):
            S.op("dve", lambda e, c=c: e.scalar_tensor_tensor(out=h[:, c, :N], in0=xt[:, c, :N], scalar=gcol[:, c:c + 1],
                                                            in1=rs[:, :N], op0=ALU.mult, op1=ALU.mult),
                 reads=[xkey, "rs"] + self.CST, writes=[hkey])

    def load_w(self, dst, src, key):
        self.S.dma("pool", "w_" + key, lambda e: e.dma_start(out=dst, in_=src), writes=[key])

    def pass_a(self, layer, tiles, xsrc, xsrc_key, w_in, conv_w_out, hT_s, T_s):
        nc, S, c = self.nc, self.S, self.c
        with ExitStack() as es:
            def sb(name, shape, dt):
                return es.enter_context(nc.sbuf_tensor(f"a{layer}_{name}", list(shape), dt))
            xt = [sb(f"xt{i}", [128, 8, 512], F32) for i in range(2)]
            sq = sb("sq", [128, 8, 512], BF16)
            sd = sb("sd", [128, 512], F32)
            rs = sb("rs", [128, 512], F32)
            h = [sb(f"h{i}", [128, 8, 512], BF16) for i in range(2)]
            wA = sb("wA", [128, 8, 2048], BF16)
            wco = sb("wco", [128, 4, 1024], BF16)
            diag = sb("diag", [128, 124, 128], BF16)
            sgA = [sb(f"sgA{i}", [128, 8, 512], BF16) for i in range(2)]
            sg4 = sb("sg4", [128, 4, 512], BF16)
            abuf = [sb(f"abuf{i}", [128, 4, 30 + 512], BF16) for i in range(2)]
            cc = sb("cc", [128, 4, 512], F32)
            csq = sb("csq", [128, 4, 512], BF16)
            cbf = sb("cbf", [128, 4, 512], BF16)
            msq = sb("msq", [128, 512], F32)
            var = sb("var", [128, 512], F32)
            sd2 = sb("sd2", [128, 512], F32)
            rs2 = sb("rs2", [128, 512], F32)
            ctmp = [sb(f"ctmp{i}", [128, 512], F32) for i in range(2)]
            cact = sb("cact", [128, 4, 512], BF16)
            tout = [sb(f"tout{i}", [128, 512], F32) for i in range(4)]

            w_l = w_in[layer].rearrange("(k p) n -> p k n", p=128)
            for half in range(2):
                self.load_w(wA[:, :, 1024 + half * 512:1024 + (half + 1) * 512],
                            w_l[:, :, half * 512:(half + 1) * 512], f"wA{2 + half}")
            for half in range(2):
                self.load_w(wA[:, :, half * 512:(half + 1) * 512],
                            w_l[:, :, SPLIT_V + half * 512:SPLIT_V + (half + 1) * 512], f"wA{half}")
            WA = [f"wA{i}" for i in range(4)]
            cwo = conv_w_out[layer].rearrange("(k p) n -> p k n", p=128)
            for half in range(2):
                self.load_w(wco[:, :, half * 512:(half + 1) * 512], cwo[:, :, half * 512:(half + 1) * 512], f"wco{half}")
            WCO = ["wco0", "wco1"]
            for j in range(124):
                col = layer * 124 + j
                if j % 3 != 2:
                    S.op("dve", lambda e, j=j, col=col: e.tensor_scalar(out=diag[:, j, :], in0=c["ident_f"][:],
                                                                       scalar1=c["convw"][:, col:col + 1], scalar2=None,
                                                                       op0=ALU.mult),
                         reads=self.CST, writes=[f"diag_{j}"])
                else:
                    S.op("act", lambda e, j=j, col=col: e.activation(out=diag[:, j, :], in_=c["ident_f"][:], func=AF.Copy,
                                                                    scale=c["convw"][:, col:col + 1]),
                         reads=self.CST, writes=[f"diag_{j}"])
            S.op("pool", lambda e: e.memset(abuf[0][:, :, 0:30], 0.0), writes=[f"abuf0_{m}" for m in range(4)])
            nt = len(tiles)

            def stage0(i):
                if i >= nt:
                    return
                col0, N = tiles[i]
                s = i % 2
                S.dma("sp", f"ax{s}", lambda e: e.dma_start(out=xt[s][:, :, :N],
                                                           in_=xsrc[:, col0:col0 + N].rearrange("(k p) n -> p k n", p=128)),
                      reads=[(xsrc_key, i)], writes=[f"xt{s}"])
                self.norm_fm(xt[s], f"xt{s}", N, c["gmix"][:, layer * 8:(layer + 1) * 8], sq, sd, rs, h[s], f"h{s}")
                S.dma("pool", f"ah{s}", lambda e: e.dma_start(out=hT_s[:, col0:col0 + N].rearrange("(k p) n -> p k n", p=128),
                                                             in_=h[s][:, :, :N]),
                      reads=[f"h{s}"], writes=[("hT", i)])

            def is_full(i):
                return not (layer == 1 and i == 0)

            def front(i):
                if i >= nt:
                    return
                col0, N = tiles[i]
                s = i % 2
                hk = f"h{s}"
                ab = abuf[s]
                for m in range(4):
                    ps, pk = self.PS()
                    cb = 1024 + 512 + m * 128
                    self.mm_group(ps[:, :N], [(wA[:, k, cb:cb + 128], h[s][:, k, :N]) for k in range(KD)],
                                  reads=[hk] + WA, writes=[pk])
                    S.op("act", lambda e, ps=ps, m=m: e.activation(out=sg4[:, m, :N], in_=ps[:, :N], func=AF.Sigmoid),
                         reads=[pk], writes=[f"sg4{m}"])
                for m in range(4):
                    ps, pk = self.PS()
                    cb = 1024 + m * 128
                    self.mm_group(ps[:, :N], [(wA[:, k, cb:cb + 128], h[s][:, k, :N]) for k in range(KD)],
                                  reads=[hk] + WA, writes=[pk])
                    S.op("dve", lambda e, ps=ps, m=m: e.tensor_tensor(out=ab[:, m, 30:30 + N], in0=ps[:, :N],
                                                                    in1=sg4[:, m, :N], op=ALU.mult),
                         reads=[pk, f"sg4{m}"], writes=[f"abuf{s}_{m}"])
                    S.op("pool", lambda e, m=m: e.tensor_copy(out=abuf[1 - s][:, m, 0:30], in_=ab[:, m, N:N + 30]),
                         reads=[f"abuf{s}_{m}"], writes=[f"abuf{1 - s}_{m}"])
                if is_full(i):
                    for m in range(8):
                        ps, pk = self.PS()
                        self.mm_group(ps[:, :N], [(wA[:, k, m * 128:(m + 1) * 128], h[s][:, k, :N]) for k in range(KD)],
                                      reads=[hk] + WA, writes=[pk])
                        S.op("act", lambda e, ps=ps, m=m: e.activation(out=sgA[s][:, m, :N], in_=ps[:, :N], func=AF.Sigmoid),
                             reads=[pk], writes=[f"sgA{s}_{m}"])

            def back1(i):
                if not is_full(i):
                    return
                col0, N = tiles[i]
                s = i % 2
                ab = abuf[s]
                for m in range(4):
                    ps, pk = self.PS()
                    self.mm_group(ps[:, :N], [(diag[:, m * 31 + k, :], ab[:, m, k:k + N]) for k in range(31)],
                                  reads=[f"abuf{s}_{m}"] + [f"diag_{m * 31 + k}" for k in range(31)], writes=[pk])
                    S.op("act", lambda e, ps=ps, m=m: e.activation(out=cc[:, m, :N], in_=ps[:, :N], func=AF.Identity,
                                                                 bias=c["convb"][:, layer * 4 + m:layer * 4 + m + 1],
                                                                 scale=1.0),
                         reads=[pk] + self.CST, writes=[f"cc{m}"])
                    S.op("act", lambda e, ps=ps, m=m: e.activation(out=csq[:, m, :N], in_=ps[:, :N], func=AF.Square,
                                                                 bias=c["convb"][:, layer * 4 + m:layer * 4 + m + 1],
                                                                 scale=1.0),
                         reads=[pk] + self.CST, writes=[f"csq{m}"])
                    S.op("pool", lambda e, m=m: e.tensor_copy(out=cbf[:, m, :N], in_=cc[:, m, :N]),
                         reads=[f"cc{m}"], writes=[f"cbf{m}"])
                psM, pkM = self.PS()
                self.mm_group(psM[:, :N], [(c["ones_b"][:], cbf[:, m, :N]) for m in range(4)],
                              reads=[f"cbf{m}" for m in range(4)] + ["ones_b"], writes=[pkM])
                psQ, pkQ = self.PS()
                self.mm_group(psQ[:, :N], [(c["ones_b"][:], csq[:, m, :N]) for m in range(4)],
                              reads=[f"csq{m}" for m in range(4)] + ["ones_b"], writes=[pkQ])
                S.op("act", lambda e: e.activation(out=msq[:, :N], in_=psM[:, :N], func=AF.Square, scale=1.0 / 512),
                     reads=[pkM], writes=["msq"])
                S.op("dve", lambda e: e.scalar_tensor_tensor(out=var[:, :N], in0=psQ[:, :N], scalar=1.0 / 512,
                                                            in1=msq[:, :N], op0=ALU.mult, op1=ALU.subtract),
                     reads=[pkQ, "msq"], writes=["var"])
                S.op("act", lambda e: e.activation(out=sd2[:, :N], in_=var[:, :N], func=AF.Sqrt, scale=1.0, bias=EPS),
                     reads=["var"], writes=["sd2"])
                S.op("dve", lambda e: e.reciprocal(out=rs2[:, :N], in_=sd2[:, :N]), reads=["sd2"], writes=["rs2"])
                for m in range(4):
                    t = ctmp[m % 2]
                    tk = f"ctmp{m % 2}"
                    S.op("dve", lambda e, m=m, t=t: e.scalar_tensor_tensor(out=t[:, :N], in0=psM[:, :N], scalar=-1.0 / 512,
                                                                         in1=cc[:, m, :N], op0=ALU.mult, op1=ALU.add),
                         reads=[pkM, f"cc{m}"], writes=[tk])
                    S.op("dve", lambda e, t=t: e.tensor_tensor(out=t[:, :N], in0=t[:, :N], in1=rs2[:, :N], op=ALU.mult),
                         reads=[tk, "rs2"], writes=[tk])
                    S.op("act", lambda e, m=m, t=t: e.activation(out=cact[:, m, :N], in_=t[:, :N], func=AF.Silu,
                                                               scale=c["clng"][:, layer * 4 + m:layer * 4 + m + 1],
                                                               bias=c["clnb"][:, layer * 4 + m:layer * 4 + m + 1]),
                         reads=[tk] + self.CST, writes=[f"cact{m}"])

            def back2(i):
                if not is_full(i):
                    return
                col0, N = tiles[i]
                s = i % 2
                for cc_ in range(8):
                    ps, pk = self.PS()
                    self.mm_group(ps[:, :N], [(wco[:, k, cc_ * 128:(cc_ + 1) * 128], cact[:, k, :N]) for k in range(4)],
                                  reads=[f"cact{m}" for m in range(4)] + WCO, writes=[pk])
                    r = cc_ % 4
                    S.op("dve", lambda e, ps=ps, cc_=cc_, r=r: e.tensor_tensor(out=tout[r][:, :N], in0=ps[:, :N],
                                                                             in1=sgA[s][:, cc_, :N], op=ALU.mult),
                         reads=[pk, f"sgA{s}_{cc_}"], writes=[f"tout{r}"])
                    S.dma("pool", f"at{r}", lambda e, cc_=cc_, r=r: e.dma_start(
                        out=T_s[cc_ * 128:(cc_ + 1) * 128, col0:col0 + N], in_=tout[r][:, :N]),
                        reads=[f"tout{r}"], writes=[("T", i, cc_)])

            stage0(0)
            stage0(1)
            front(0)
            for i in range(nt):
                back1(i)
                front(i + 1)
                back2(i)
                stage0(i + 2)

    def pass_b(self, layer, tiles, w_in, pool_w, hT_s, T_s):
        nc, S, c = self.nc, self.S, self.c
        with ExitStack() as es:
            def sb(name, shape, dt):
                return es.enter_context(nc.sbuf_tensor(f"b{layer}_{name}", list(shape), dt))
            hb = [sb(f"hb{i}", [128, 8, 512], BF16) for i in range(2)]
            wB = sb("wB", [128, 8, 1536], BF16)
            wp = sb("wp", [128, 4, 256], BF16)
            sgB = [sb(f"sgB{i}", [128, 8, 512], BF16) for i in range(2)]
            L = 16 + 512
            pbuf = [sb(f"pbuf{i}", [128, 4, L], F32) for i in range(3)]
            P1 = sb("P1", [128, 4, L], F32)
            P2 = sb("P2", [128, 4, L], F32)
            pooled = sb("pooled", [128, 4, 512], BF16)
            t16 = sb("t16", [128, 16], F32)
            tin = [[sb(f"tin{j}_{i}", [128, 512], F32) for i in range(8)] for j in range(2)]
            tmpb = [sb(f"tmpb{i}", [128, 512], F32) for i in range(2)]
            tout = [sb(f"tout{i}", [128, 512], F32) for i in range(4)]

            w_l = w_in[layer].rearrange("(k p) n -> p k n", p=128)
            self.load_w(wB[:, :, 1024:1536], w_l[:, :, 1024:1536], "wB2")
            for half in range(2):
                cb = SPLIT_V + 1024 + half * 512
                self.load_w(wB[:, :, half * 512:(half + 1) * 512], w_l[:, :, cb:cb + 512], f"wB{half}")
            WB = ["wB0", "wB1", "wB2"]
            self.load_w(wp[:], pool_w[layer].rearrange("g p n -> p g n"), "wp")
            S.op("pool", lambda e: e.memset(pbuf[0][:, :, 0:16], 0.0), writes=["pbuf0"])
            nt = len(tiles)

            def is_full(i):
                return not (layer == 1 and i == 0)

            def stage0(i):
                if i >= nt:
                    return
                col0, N = tiles[i]
                s = i % 2
                S.dma("sp", f"bh{s}", lambda e: e.dma_start(out=hb[s][:, :, :N],
                                                           in_=hT_s[:, col0:col0 + N].rearrange("(k p) n -> p k n", p=128)),
                      reads=[("hT", i)], writes=[f"hb{s}"])

            def front(i):
                if i >= nt:
                    return
                col0, N = tiles[i]
                s = i % 2
                hk = f"hb{s}"
                s3 = i % 3
                n3 = (i + 1) % 3
                pb = pbuf[s3]
                for g in range(4):
                    ps, pk = self.PS()
                    cb = 1024 + g * 128
                    self.mm_group(ps[:, :N], [(wB[:, k, cb:cb + 128], hb[s][:, k, :N]) for k in range(KD)],
                                  reads=[hk] + WB, writes=[pk])
                    S.op("act", lambda e, ps=ps, g=g: e.activation(out=pb[:, g, 16:16 + N], in_=ps[:, :N], func=AF.Copy),
                         reads=[pk], writes=[f"pbuf{s3}"])
                S.op("pool", lambda e: e.tensor_copy(out=pbuf[n3][:, :, 0:16], in_=pb[:, :, N:N + 16]),
                     reads=[f"pbuf{s3}"], writes=[f"pbuf{n3}"])
                if is_full(i):
                    for cc_ in range(8):
                        S.dma("sp", f"bt{s}_{cc_}", lambda e, cc_=cc_: e.dma_start(
                            out=tin[s][cc_][:, :N], in_=T_s[cc_ * 128:(cc_ + 1) * 128, col0:col0 + N]),
                            reads=[("T", i, cc_)], writes=[f"tin{s}_{cc_}"])
                    for m in range(8):
                        ps, pk = self.PS()
                        self.mm_group(ps[:, :N], [(wB[:, k, m * 128:(m + 1) * 128], hb[s][:, k, :N]) for k in range(KD)],
                                      reads=[hk] + WB, writes=[pk])
                        S.op("act", lambda e, ps=ps, m=m: e.activation(out=sgB[s][:, m, :N], in_=ps[:, :N], func=AF.Sigmoid),
                             reads=[pk], writes=[f"sgB{s}_{m}"])

            def back(i):
                if not is_full(i):
                    return
                col0, N = tiles[i]
                s = i % 2
                pb = pbuf[i % 3]
                pbk = f"pbuf{i % 3}"
                Le = 16 + N
                S.op("pool", lambda e: e.tensor_tensor(out=P1[:, :, 1:Le], in0=pb[:, :, 1:Le], in1=pb[:, :, 0:Le - 1],
                                                      op=ALU.add), reads=[pbk], writes=["P1"])
                S.op("pool", lambda e: e.tensor_tensor(out=P2[:, 1:4, 3:Le], in0=P1[:, 1:4, 3:Le], in1=P1[:, 1:4, 1:Le - 2],
                                                      op=ALU.add), reads=["P1"], writes=["P2"])
                S.op("pool", lambda e: e.tensor_tensor(out=P1[:, 2:4, 7:Le], in0=P2[:, 2:4, 7:Le], in1=P2[:, 2:4, 3:Le - 4],
                                                      op=ALU.add), reads=["P2"], writes=["P1"])
                S.op("pool", lambda e: e.tensor_tensor(out=P2[:, 3:4, 15:Le], in0=P1[:, 3:4, 15:Le], in1=P1[:, 3:4, 7:Le - 8],
                                                      op=ALU.add), reads=["P1"], writes=["P2"])
                srcs = [P1, P2, P1, P2]
                skeys = [["P1"], ["P2"], ["P1"], ["P2"]]
                for g in range(4):
                    wv = float(2 << g)
                    S.op("dve", lambda e, g=g, wv=wv: e.scalar_tensor_tensor(
                        out=pooled[:, g, :N], in0=srcs[g][:, g, 16:16 + N], scalar=1.0 / wv, in1=pb[:, g, 16:16 + N],
                        op0=ALU.mult, op1=ALU.subtract), reads=skeys[g] + [pbk], writes=[f"pooled{g}"])
                    if i == 1:
                        S.op("dve", lambda e, g=g: e.tensor_tensor(out=t16[:], in0=srcs[g][:, g, 16:32],
                                                                  in1=c["rc"][:, g * 16:(g + 1) * 16], op=ALU.mult),
                             reads=skeys[g] + self.CST, writes=["t16"])
                        S.op("dve", lambda e, g=g: e.tensor_tensor(out=pooled[:, g, 0:16], in0=t16[:],
                                                                  in1=pb[:, g, 16:32], op=ALU.subtract),
                             reads=["t16", pbk], writes=[f"pooled{g}"])
                for cc_ in range(8):
                    g, jj = cc_ // 2, cc_ % 2
                    r = cc_ % 4
                    ps, pk = self.PS()
                    self.mm_group(ps[:, :N], [(wp[:, g, jj * 128:(jj + 1) * 128], pooled[:, g, :N])],
                                  reads=[f"pooled{g}", "wp"], writes=[pk])
                    tb = tmpb[cc_ % 2]
                    tbk = f"tmpb{cc_ % 2}"
                    S.op("dve", lambda e, ps=ps, cc_=cc_, tb=tb: e.scalar_tensor_tensor(
                        out=tb[:, :N], in0=ps[:, :N], scalar=c["pscale"][:, layer * 8 + cc_:layer * 8 + cc_ + 1],
                        in1=sgB[s][:, cc_, :N], op0=ALU.mult, op1=ALU.mult),
                        reads=[pk, f"sgB{s}_{cc_}"] + self.CST, writes=[tbk])
                    S.op("dve", lambda e, tb=tb, r=r, cc_=cc_: e.tensor_tensor(out=tout[r][:, :N], in0=tb[:, :N],
                                                                             in1=tin[s][cc_][:, :N], op=ALU.add),
                         reads=[tbk, f"tin{s}_{cc_}"], writes=[f"tout{r}"])
                    S.dma("pool", f"bo{r}", lambda e, cc_=cc_, r=r: e.dma_start(
                        out=T_s[cc_ * 128:(cc_ + 1) * 128, col0:col0 + N], in_=tout[r][:, :N]),
                        reads=[f"tout{r}"], writes=[("T", i, cc_)])

            stage0(0)
            stage0(1)
            front(0)
            for i in range(nt):
                front(i + 1)
                back(i)
                stage0(i + 2)

    def pass_c(self, layer, tiles, xsrc, xsrc_key, w_in, sgu_w_out, w_out, slng_d, slnb_d, wsT_d, bs_d, tril_d,
               hT_s, T_s, xm_s, h2T_s):
        nc, S, c = self.nc, self.S, self.c
        with ExitStack() as es:
            def sb(name, shape, dt):
                return es.enter_context(nc.sbuf_tensor(f"c{layer}_{name}", list(shape), dt))
            hb = [sb(f"hb{i}", [128, 8, 512], BF16) for i in range(2)]
            xt = [sb(f"xt{i}", [128, 8, 512], F32) for i in range(2)]
            wC = sb("wC", [128, 8, 2048], BF16)
            wso = sb("wso", [128, 4, 1024], BF16)
            wo = sb("wo", [128, 8, 1024], BF16)
            sgC = [sb(f"sgC{i}", [128, 8, 512], BF16) for i in range(2)]
            u = sb("u", [128, 4, 512], F32)
            st6 = sb("st6", [128, 4, 6], F32)
            mv = sb("mv", [128, 4, 2], F32)
            sdv = sb("sdv", [128, 4], F32)
            rv = sb("rv", [128, 4], F32)
            nb = sb("nb", [128, 4], F32)
            vtmp = [sb(f"vtmp{i}", [128, 512], F32) for i in range(4)]
            vn = sb("vn", [128, 4, 512], BF16)
            slng = sb("slng", [128, 512], F32)
            slnb = sb("slnb", [128, 512], F32)
            wsf = sb("wsf", [128, 512], F32)
            trl = sb("trl", [128, 128], F32)
            wsm = sb("wsm", [128, 4, 128], BF16)
            bsf = sb("bsf", [1, 512], F32)
            bsr = sb("bsr", [1, 512], F32)
            bsh = sb("bsh", [128, 512], BF16)
            bsl = sb("bsl", [128, 512], BF16)
            e0 = sb("e0", [128, 128], BF16)
            us = sb("us", [128, 4, 512], BF16)
            tin = [sb(f"tin{i}", [128, 512], F32) for i in range(4)]
            tmpc = [sb(f"tmpc{i}", [128, 512], F32) for i in range(2)]
            mixed = sb("mixed", [128, 8, 512], BF16)
            sq = sb("sq", [128, 8, 512], BF16)
            sd = sb("sd", [128, 512], F32)
            rs = sb("rs", [128, 512], F32)
            h2 = sb("h2", [128, 8, 512], BF16)

            w_l = w_in[layer].rearrange("(k p) n -> p k n", p=128)
            self.load_w(wC[:, :, 1536:2048], w_l[:, :, 2048:2560], "wC3")
            self.load_w(wC[:, :, 1024:1536], w_l[:, :, 1536:2048], "wC2")
            for half in range(2):
                cb = SPLIT_V + 2048 + half * 512
                self.load_w(wC[:, :, half * 512:(half + 1) * 512], w_l[:, :, cb:cb + 512], f"wC{half}")
            WC = [f"wC{i}" for i in range(4)]
            swo = sgu_w_out[layer].rearrange("(k p) n -> p k n", p=128)
            wol = w_out[layer].rearrange("(k p) n -> p k n", p=128)
            for half in range(2):
                self.load_w(wso[:, :, half * 512:(half + 1) * 512], swo[:, :, half * 512:(half + 1) * 512], f"wso{half}")
            for half in range(2):
                self.load_w(wo[:, :, half * 512:(half + 1) * 512], wol[:, :, half * 512:(half + 1) * 512], f"wo{half}")
            WSO = ["wso0", "wso1"]
            WO = ["wo0", "wo1"]
            loads = [(slng[:], slng_d[:, layer * 512:(layer + 1) * 512]), (slnb[:], slnb_d[:, layer * 512:(layer + 1) * 512]),
                     (wsf[:], wsT_d[:, layer * 512:(layer + 1) * 512]), (trl[:], tril_d),
                     (bsf[:], bs_d[:, layer * 512:(layer + 1) * 512])]
            for j, (dst, src) in enumerate(loads):
                S.dma("sp", "cl", lambda e, dst=dst, src=src: e.dma_start(out=dst, in_=src), writes=[f"cl{j}"])
            CL = [f"cl{j}" for j in range(len(loads))]
            for hd in range(4):
                S.op("dve", lambda e, hd=hd: e.tensor_tensor(out=wsm[:, hd, :], in0=wsf[:, hd * 128:(hd + 1) * 128],
                                                            in1=trl[:], op=ALU.mult), reads=CL, writes=["wsm"])
            S.op("dve", lambda e: e.memset(bsh[:], 0.0), writes=["bsh"])
            S.op("dve", lambda e: e.memset(bsl[:], 0.0), writes=["bsl"])
            S.op("dve", lambda e: e.memset(e0[:], 0.0), writes=["e0"])
            S.op("dve", lambda e: e.memset(e0[0:1, :], 1.0), reads=["e0"], writes=["e0"])
            S.op("dve", lambda e: e.tensor_copy(out=bsh[0:1, :], in_=bsf[:]), reads=CL + ["bsh"], writes=["bsh"])
            S.op("dve", lambda e: e.tensor_tensor(out=bsr[:], in0=bsf[:], in1=bsh[0:1, :], op=ALU.subtract),
                 reads=CL + ["bsh"], writes=["bsr"])
            S.op("dve", lambda e: e.tensor_copy(out=bsl[0:1, :], in_=bsr[:]), reads=["bsr", "bsl"], writes=["bsl"])
            CL += ["wsm", "bsh", "bsl", "e0"]
            nt = len(tiles)

            def stage0(i):
                if i >= nt:
                    return
                col0, N = tiles[i]
                s = i % 2
                S.dma("sp", f"ch{s}", lambda e: e.dma_start(out=hb[s][:, :, :N],
                                                           in_=hT_s[:, col0:col0 + N].rearrange("(k p) n -> p k n", p=128)),
                      reads=[("hT", i)], writes=[f"hb{s}"])
                S.dma("sp", f"cx{s}", lambda e: e.dma_start(out=xt[s][:, :, :N],
                                                           in_=xsrc[:, col0:col0 + N].rearrange("(k p) n -> p k n", p=128)),
                      reads=[(xsrc_key, i)], writes=[f"xt{s}"])

            def front(i):
                if i >= nt:
                    return
                col0, N = tiles[i]
                s = i % 2
                hk = f"hb{s}"
                nsub = N // 128
                vps = []
                for sub in range(nsub):
                    ps, pk = self.PS()
                    vps.append((ps, pk))
                    self.mm_group(ps[:, :], [(hb[s][:, k, sub * 128:(sub + 1) * 128], wC[:, k, 1536:2048]) for k in range(KD)],
                                  reads=[hk] + WC, writes=[pk])
                    S.op("dve", lambda e, ps=ps, sub=sub: e.bn_stats(out=st6[:, sub, :], in_=ps[:, :]), reads=[pk], writes=[f"st6_{sub}"])
                    S.op("dve", lambda e, sub=sub: e.bn_aggr(out=mv[:, sub, :], in_=st6[:, sub, :]), reads=[f"st6_{sub}"], writes=[f"mv_{sub}"])
                MV = [f"mv_{sub}" for sub in range(nsub)]
                S.op("act", lambda e: e.activation(out=sdv[:, :nsub], in_=mv[:, :nsub, 1], func=AF.Sqrt, scale=1.0, bias=EPS),
                     reads=MV, writes=["sdv"])
                S.op("dve", lambda e: e.reciprocal(out=rv[:, :nsub], in_=sdv[:, :nsub]), reads=["sdv"], writes=["rv"])
                S.op("dve", lambda e: e.scalar_tensor_tensor(out=nb[:, :nsub], in0=mv[:, :nsub, 0], scalar=-1.0, in1=rv[:, :nsub],
                                                            op0=ALU.mult, op1=ALU.mult), reads=MV + ["rv"], writes=["nb"])
                for sub in range(nsub):
                    ps, pk = vps[sub]
                    vt = vtmp[sub]
                    vk = f"vtmp{sub}"
                    S.op("act", lambda e, ps=ps, vt=vt, sub=sub: e.activation(out=vt[:], in_=ps[:, :], func=AF.Identity,
                                                                            scale=rv[:, sub:sub + 1], bias=nb[:, sub:sub + 1]),
                         reads=[pk, "rv", "nb"], writes=[vk])
                    S.op("pool", lambda e, vt=vt: e.tensor_tensor(out=vt[:], in0=vt[:], in1=slng[:], op=ALU.mult),
                         reads=[vk] + CL, writes=[vk])
                    S.op("pool", lambda e, vt=vt, sub=sub: e.tensor_tensor(out=vn[:, sub, :], in0=vt[:], in1=slnb[:], op=ALU.add),
                         reads=[vk] + CL, writes=[f"vn{sub}"])
                for m in range(4):
                    ps, pk = self.PS()
                    cb = 1024 + m * 128
                    self.mm_group(ps[:, :N], [(wC[:, k, cb:cb + 128], hb[s][:, k, :N]) for k in range(KD)],
                                  reads=[hk] + WC, writes=[pk])
                    S.op("act", lambda e, ps=ps, m=m: e.activation(out=u[:, m, :N], in_=ps[:, :N], func=AF.Copy),
                         reads=[pk], writes=[f"u{m}"])
                for m in range(8):
                    ps, pk = self.PS()
                    self.mm_group(ps[:, :N], [(wC[:, k, m * 128:(m + 1) * 128], hb[s][:, k, :N]) for k in range(KD)],
                                  reads=[hk] + WC, writes=[pk])
                    S.op("act", lambda e, ps=ps, m=m: e.activation(out=sgC[s][:, m, :N], in_=ps[:, :N], func=AF.Sigmoid),
                         reads=[pk], writes=[f"sgC{s}_{m}"])

            def back1(i):
                col0, N = tiles[i]
                nsub = N // 128
                for hd in range(4):
                    ps, pk = self.PS()

                    def emit(e, ps=ps, hd=hd):
                        ins = None
                        for sub in range(nsub):
                            o = ps[:, sub * 128:(sub + 1) * 128]
                            e.matmul(o, lhsT=vn[:, sub, hd * 128:(hd + 1) * 128], rhs=wsm[:, hd, :], start=True, stop=False)
                            e.matmul(o, lhsT=e0[:], rhs=bsh[:, hd * 128:(hd + 1) * 128], start=False, stop=False)
                            ins = e.matmul(o, lhsT=e0[:], rhs=bsl[:, hd * 128:(hd + 1) * 128], start=False, stop=True)
                        return ins
                    S.op("pe", emit, reads=[f"vn{sub}" for sub in range(nsub)] + CL, writes=[pk])
                    S.op("dve", lambda e, ps=ps, hd=hd: e.tensor_tensor(out=us[:, hd, :N], in0=ps[:, :N], in1=u[:, hd, :N],
                                                                      op=ALU.mult),
                         reads=[pk, f"u{hd}"], writes=[f"us{hd}"])

            def back2(i):
                col0, N = tiles[i]
                s = i % 2
                for cc_ in range(8):
                    r = cc_ % 4
                    S.dma("sp", f"ct{r}", lambda e, cc_=cc_, r=r: e.dma_start(
                        out=tin[r][:, :N], in_=T_s[cc_ * 128:(cc_ + 1) * 128, col0:col0 + N]),
                        reads=[("T", i, cc_)], writes=[f"tin{r}"])
                    ps, pk = self.PS()
                    self.mm_group(ps[:, :N], [(wso[:, k, cc_ * 128:(cc_ + 1) * 128], us[:, k, :N]) for k in range(4)],
                                  reads=[f"us{k}" for k in range(4)] + WSO, writes=[pk])
                    tc_ = tmpc[cc_ % 2]
                    tck = f"tmpc{cc_ % 2}"
                    S.op("dve", lambda e, ps=ps, cc_=cc_, tc_=tc_: e.tensor_tensor(out=tc_[:, :N], in0=ps[:, :N],
                                                                                 in1=sgC[s][:, cc_, :N], op=ALU.mult),
                         reads=[pk, f"sgC{s}_{cc_}"], writes=[tck])
                    S.op("pool", lambda e, cc_=cc_, tc_=tc_, r=r: e.tensor_tensor(out=mixed[:, cc_, :N], in0=tc_[:, :N],
                                                                                in1=tin[r][:, :N], op=ALU.add),
                         reads=[tck, f"tin{r}"], writes=[f"mixed{cc_}"])
                for cc_ in range(8):
                    ps, pk = self.PS()
                    self.mm_group(ps[:, :N], [(wo[:, k, cc_ * 128:(cc_ + 1) * 128], mixed[:, k, :N]) for k in range(KD)],
                                  reads=[f"mixed{k}" for k in range(8)] + WO, writes=[pk])
                    S.op("dve", lambda e, ps=ps, cc_=cc_: e.tensor_tensor(out=xt[s][:, cc_, :N], in0=ps[:, :N],
                                                                        in1=xt[s][:, cc_, :N], op=ALU.add),
                         reads=[pk, f"xt{s}"], writes=[f"xt{s}"])
                S.dma("pool", f"cxo{s}", lambda e: e.dma_start(out=xm_s[:, col0:col0 + N].rearrange("(k p) n -> p k n", p=128),
                                                              in_=xt[s][:, :, :N]),
                      reads=[f"xt{s}"], writes=[("xm", layer, i)])

            def back4(i):
                col0, N = tiles[i]
                s = i % 2
                self.norm_fm(xt[s], f"xt{s}", N, c["gffn"][:, layer * 8:(layer + 1) * 8], sq, sd, rs, h2, "h2")
                S.dma("pool", "ch2", lambda e: e.dma_start(out=h2T_s[:, col0:col0 + N].rearrange("(k p) n -> p k n", p=128),
                                                          in_=h2[:, :, :N]),
                      reads=["h2"], writes=[("h2T", i)])

            first = 1 if layer == 1 else 0
            stage0(first)
            stage0(first + 1)
            front(first)
            back1(first)
            for i in range(first, nt):
                front(i + 1)
                back2(i)
                if i + 1 < nt:
                    back1(i + 1)
                back4(i)
                stage0(i + 2)

    def ffn_stream(self, pfx, sb, jobs, RM):
        S = self.S
        w1g = [sb(f"{pfx}w1g{i}", [128, 8, 512], BF16) for i in range(2)]
        w3g = [sb(f"{pfx}w3g{i}", [128, 8, 512], BF16) for i in range(2)]
        w2g = [sb(f"{pfx}w2g{i}", [128, 4, 1024], BF16) for i in range(2)]
        Hh = [sb(f"{pfx}Hh{i}", [128, 4, RM], BF16) for i in range(2)]
        sgt = [sb(f"{pfx}sgt{i}", [128, 512], F32) for i in range(2)]
        groups = []
        for ji, job in enumerate(jobs):
            for jg in range((job["nF"] + 3) // 4):
                groups.append((ji, jg))

        def load(gi):
            ji, jg = groups[gi]
            job = jobs[ji]
            b = gi % 2
            nj = min(4, job["nF"] - jg * 4)
            w1v = job["w1"].rearrange("(k p) n -> p k n", p=128)
            w3v = job["w3"].rearrange("(k p) n -> p k n", p=128)
            w2v = job["w2"].rearrange("(j p) n -> p j n", p=128)
            self.load_w(w1g[b][:, :, :nj * 128], w1v[:, :, jg * 512:jg * 512 + nj * 128], f"{pfx}w1g{b}")
            self.load_w(w3g[b][:, :, :nj * 128], w3v[:, :, jg * 512:jg * 512 + nj * 128], f"{pfx}w3g{b}")
            self.load_w(w2g[b][:, :nj, :], w2v[:, jg * 4:jg * 4 + nj, :], f"{pfx}w2g{b}")

        load(0)
        jobs[0]["pre"]()
        n_sg = 0
        for gi, (ji, jg) in enumerate(groups):
            job = jobs[ji]
            b = gi % 2
            nj = min(4, job["nF"] - jg * 4)
            if gi + 1 < len(groups):
                load(gi + 1)
                if groups[gi + 1][0] != ji and jobs[ji + 1].get("early_pre"):
                    jobs[ji + 1]["pre"]()
            if jg == 0 and ji > 0 and not job.get("early_pre"):
                job["pre"]()
            hs, hs_keys = job["hs"], job["hs_keys"]
            for (c0, n) in job["col_tiles"]:
                for jj in range(nj):
                    psG, pkG = self.PS()
                    self.mm_group(psG[:, :n], [(w1g[b][:, k, jj * 128:(jj + 1) * 128], hs[:, k, c0:c0 + n]) for k in range(KD)],
                                  reads=hs_keys + [f"{pfx}w1g{b}"], writes=[pkG])
                    psU, pkU = self.PS()
                    self.mm_group(psU[:, :n], [(w3g[b][:, k, jj * 128:(jj + 1) * 128], hs[:, k, c0:c0 + n]) for k in range(KD)],
                                  reads=hs_keys + [f"{pfx}w3g{b}"], writes=[pkU])
                    st = sgt[n_sg % 2]
                    sk = f"{pfx}sgt{n_sg % 2}"
                    n_sg += 1
                    S.op("act", lambda e, psG=psG, st=st, n=n: e.activation(out=st[:, :n], in_=psG[:, :n], func=AF.Silu),
                         reads=[pkG], writes=[sk])
                    S.op("dve", lambda e, psU=psU, st=st, n=n, jj=jj, c0=c0, b=b: e.tensor_tensor(
                        out=Hh[b][:, jj, c0:c0 + n], in0=psU[:, :n], in1=st[:, :n], op=ALU.mult),
                        reads=[pkU, sk], writes=[f"{pfx}Hh{b}"])
            job["emit_y"](jg, nj, Hh[b], [f"{pfx}Hh{b}"], w2g[b], [f"{pfx}w2g{b}"])
            if gi + 1 == len(groups) or groups[gi + 1][0] != ji:
                job["post"]()

    def dense_ffn(self, tiles, dw1, dw3, dw2, h2T_s, xm_s, xf_s, xg_s, ys_s):
        nc, S, c = self.nc, self.S, self.c
        supers = [[0, 1, 2], [3, 4, 5], [6, 7, 8]]
        with ExitStack() as es:
            def sb(name, shape, dt):
                return es.enter_context(nc.sbuf_tensor(f"f_{name}", list(shape), dt))
            RM = 1536
            hs = sb("hs", [128, 8, RM], BF16)
            ya = sb("ya", [128, 8, RM], F32)
            zrow = sb("zrow", [128, 4, 1024], BF16)
            S.op("pool", lambda e: e.memset(zrow[:], 0.0), writes=["zrow"])
            zrowf = sb("zrowf", [128, 1024], F32)
            S.op("pool", lambda e: e.memset(zrowf[:], 0.0), writes=["zrowf"])

            def init_xg():
                for j in range(NSLOT // 512):
                    S.dma("sp", f"xgi{j % 4}", lambda e, j=j: e.dma_start(
                        out=xg_s[j * 512:(j + 1) * 512, :].rearrange("(b p) n -> p b n", p=128), in_=zrow[:]),
                        reads=["zrow"], writes=[f"xgi{j % 4}"])
                S.dma("sp", "xgi0", lambda e: e.dma_start(out=ys_s[NSLOT:NSLOT + 128, :], in_=zrowf[:]),
                      reads=["zrowf"], writes=["ysz"])
            jobs = []
            for si, st_ in enumerate(supers):
                base = tiles[st_[0]][0]
                col_tiles = [(tiles[t][0] - base, tiles[t][1]) for t in st_]
                R = sum(n for _, n in col_tiles)

                def pre(base=base, R=R, st_=st_, si=si):
                    S.dma("sp", "fh", lambda e: e.dma_start(
                        out=hs[:, :, :R], in_=h2T_s[:, base:base + R].rearrange("(k p) n -> p k n", p=128)),
                        reads=[("h2T", t) for t in st_], writes=["f_hs"])
                    S.dma("sp", "fx", lambda e: e.dma_start(
                        out=ya[:, :, :R], in_=xm_s[:, base:base + R].rearrange("(k p) n -> p k n", p=128)),
                        reads=[("xm", 0, t) for t in st_], writes=["f_ya"])
                    if si == 1:
                        init_xg()

                def emit_y(jg, nj, Hh, hkeys, w2g, wkeys, col_tiles=col_tiles):
                    for (c0, n) in col_tiles:
                        for cc_ in range(8):
                            ps, pk = self.PS()
                            self.mm_group(ps[:, :n], [(w2g[:, jj, cc_ * 128:(cc_ + 1) * 128], Hh[:, jj, c0:c0 + n])
                                                      for jj in range(nj)], reads=hkeys + wkeys, writes=[pk])
                            S.op("dve", lambda e, ps=ps, cc_=cc_, c0=c0, n=n: e.tensor_tensor(
                                out=ya[:, cc_, c0:c0 + n], in0=ps[:, :n], in1=ya[:, cc_, c0:c0 + n], op=ALU.add),
                                reads=[pk, "f_ya"], writes=["f_ya"])

                def post(si=si, base=base, R=R, st_=st_):
                    if si == 0:
                        S.op("dve", lambda e: e.tensor_scalar(out=ya[:, :, 0:HALO], in0=ya[:, :, 0:HALO],
                                                             scalar1=c["hmask"][:, 0:1], scalar2=None, op0=ALU.mult),
                             reads=["f_ya"] + self.CST, writes=["f_ya"])
                    S.dma("sp", "fo", lambda e: e.dma_start(
                        out=xf_s[:, base:base + R].rearrange("(k p) n -> p k n", p=128), in_=ya[:, :, :R]),
                        reads=["f_ya"], writes=[("xf0", t) for t in st_])

                jobs.append(dict(hs=hs, hs_keys=["f_hs"], col_tiles=col_tiles, w1=dw1[0], w3=dw3[0], w2=dw2[0],
                                 nF=D_FF // 128, pre=pre, emit_y=emit_y, post=post))
            self.ffn_stream("f", sb, jobs, RM)

    def moe(self, ew1, ew3, ew2, wr_d, tri_d, eoff_d, fing_d, h2T_s, xm_s, xg_s, ys_s, cnt_s, out_d):
        nc, S, c = self.nc, self.S, self.c
        NT = T_OWN // 128
        NE = NT * 8
        with ExitStack() as gs2:
            def sbg(name, shape, dt):
                return gs2.enter_context(nc.sbuf_tensor(f"m_{name}", list(shape), dt))
            gw = sbg("gw", [128, NT, 2], F32)
            idx = sbg("idx", [128, NT, 2], I32)
            idxg = sbg("idxg", [128, NT, 2], I32)
            with ExitStack() as es:
                def sb(name, shape, dt):
                    return es.enter_context(nc.sbuf_tensor(f"r_{name}", list(shape), dt))
                wrf = sb("wrf", [128, 64], F32)
                wrb = sb("wrb", [128, 8, 8], BF16)
                trif = sb("trif", [128, 128], F32)
                trib = sb("trib", [128, 128], BF16)
                eoff = sb("eoff", [128, NE], F32)
                ht = sb("ht", [128, 8, T_OWN], BF16)
                lg = sb("lg", [128, NT, 8], F32)
                lg2 = sb("lg2", [128, NT, 8], F32)
                m1v = sb("m1v", [128, NT], F32)
                m2v = sb("m2v", [128, NT], F32)
                k1 = sb("k1", [128, NT, 8], F32)
                k2 = sb("k2", [128, NT, 8], F32)
                dd = sb("dd", [128, NT], F32)
                selb = sb("selb", [128, NE], BF16)
                csA = sb("csA", [128, NT, 8], F32)
                csB = sb("csB", [128, NT, 8], F32)
                cs0 = sb("cs0", [128, NT, 8], F32)
                pos = sb("pos", [128, NT, 8], F32)
                ovf = sb("ovf", [128, NT, 8], F32)
                prod = sb("prod", [128, NT, 8], F32)
                idf = sb("idf", [128, 2, NT], F32)
                hrow = [sb(f"hrow{i}", [128, 1024], BF16) for i in range(4)]
                XGI = [f"xgi{j}" for j in range(4)]
                for j, (dst, src) in enumerate([(wrf[:], wr_d), (trif[:], tri_d), (eoff[:], eoff_d)]):
                    S.dma("sp", "rl", lambda e, dst=dst, src=src: e.dma_start(out=dst, in_=src), writes=[f"rl{j}"])
                RL = ["rl0", "rl1", "rl2"]
                S.op("dve", lambda e: e.tensor_copy(out=wrb[:].rearrange("p k e -> p (k e)"), in_=wrf[:]), reads=RL, writes=["wrb"])
                S.op("dve", lambda e: e.tensor_copy(out=trib[:], in_=trif[:]), reads=RL, writes=["trib"])
                RL += ["wrb", "trib"]
                HT = []
                for j in range(8):
                    col0 = HALO + j * 512
                    S.dma("sp", "rh", lambda e, j=j, col0=col0: e.dma_start(
                        out=ht[:, :, j * 512:(j + 1) * 512], in_=h2T_s[:, col0:col0 + 512].rearrange("(k p) n -> p k n", p=128)),
                        reads=[("h2T", 1 + j)], writes=[f"rht{j}"])
                    HT.append(f"rht{j}")
                psL, pkL = self.PS()

                def emit_l(e):
                    ins = None
                    for i in range(NT):
                        for k in range(KD):
                            ins = e.matmul(psL[:, i * 8:(i + 1) * 8], lhsT=ht[:, k, i * 128:(i + 1) * 128], rhs=wrb[:, k, :],
                                           start=(k == 0), stop=(k == KD - 1))
                    return ins
                S.op("pe", emit_l, reads=HT + RL, writes=[pkL])
                lgf = lg[:].rearrange("p t e -> p (t e)")
                S.op("act", lambda e: e.activation(out=lgf, in_=psL[:, 0:NE], func=AF.Copy), reads=[pkL], writes=["lg"])
                S.op("dve", lambda e: e.tensor_reduce(out=m1v[:], in_=lg[:], axis=mybir.AxisListType.X, op=ALU.max),
                     reads=["lg"], writes=["m1v"])
                S.op("dve", lambda e: e.tensor_tensor(out=k1[:], in0=lg[:], in1=m1v[:].unsqueeze(2).broadcast_to([128, NT, 8]),
                                                     op=ALU.is_equal), reads=["lg", "m1v"], writes=["k1"])
                S.op("dve", lambda e: e.scalar_tensor_tensor(out=lg2[:].rearrange("p t e -> p (t e)"),
                                                            in0=k1[:].rearrange("p t e -> p (t e)"), scalar=-1.0e30,
                                                            in1=lgf, op0=ALU.mult, op1=ALU.add),
                     reads=["k1", "lg"], writes=["lg2"])
                S.op("dve", lambda e: e.tensor_reduce(out=m2v[:], in_=lg2[:], axis=mybir.AxisListType.X, op=ALU.max),
                     reads=["lg2"], writes=["m2v"])
                S.op("dve", lambda e: e.tensor_tensor(out=k2[:], in0=lg2[:], in1=m2v[:].unsqueeze(2).broadcast_to([128, NT, 8]),
                                                     op=ALU.is_equal), reads=["lg2", "m2v"], writes=["k2"])
                S.op("dve", lambda e: e.tensor_tensor(out=dd[:], in0=m1v[:], in1=m2v[:], op=ALU.subtract),
                     reads=["m1v", "m2v"], writes=["dd"])
                S.op("act", lambda e: e.activation(out=gw[:, :, 0], in_=dd[:], func=AF.Sigmoid), reads=["dd"], writes=["gw"])
                S.op("act", lambda e: e.activation(out=gw[:, :, 1], in_=dd[:], func=AF.Sigmoid, scale=-1.0),
                     reads=["dd"], writes=["gw"])
                S.op("dve", lambda e: e.tensor_tensor(out=selb[:], in0=k1[:].rearrange("p t e -> p (t e)"),
                                                     in1=k2[:].rearrange("p t e -> p (t e)"), op=ALU.add),
                     reads=["k1", "k2"], writes=["selb"])
                psP, pkP = self.PS()

                def emit_p(e):
                    e.matmul(psP[:, 0:NE], lhsT=trib[:], rhs=selb[:], start=True, stop=True)
                    return e.matmul(psP[:, NE:2 * NE], lhsT=c["ones_b"][:], rhs=selb[:], start=True, stop=True)
                S.op("pe", emit_p, reads=["selb", "ones_b"] + RL, writes=[pkP])
                S.op("act", lambda e: e.activation(out=cs0[:].rearrange("p t e -> p (t e)"), in_=psP[:, NE:2 * NE], func=AF.Copy),
                     reads=[pkP], writes=["cs0"])
                cur, ck = cs0, "cs0"
                bufs = [(csA, "csA"), (csB, "csB")]
                for si, sh in enumerate((1, 2, 4, 8, 16)):
                    dst, dk = bufs[si % 2]
                    S.op("dve", lambda e, cur=cur, dst=dst, sh=sh: e.tensor_tensor(out=dst[:, sh:, :], in0=cur[:, sh:, :],
                                                                                 in1=cur[:, :NT - sh, :], op=ALU.add),
                         reads=[ck], writes=[dk])
                    S.op("dve", lambda e, cur=cur, dst=dst, sh=sh: e.tensor_copy(out=dst[:, :sh, :], in_=cur[:, :sh, :]),
                         reads=[ck, dk], writes=[dk])
                    cur, ck = dst, dk
                S.dma("sp", "cnto", lambda e, cur=cur: e.dma_start(out=cnt_s, in_=cur[:, NT - 1, :]), reads=[ck], writes=["cnt_s"])
                S.op("dve", lambda e, cur=cur: e.tensor_tensor(out=pos[:], in0=cur[:], in1=cs0[:], op=ALU.subtract),
                     reads=[ck, "cs0"], writes=["pos"])
                S.op("dve", lambda e: e.tensor_tensor(out=pos[:].rearrange("p t e -> p (t e)"), in0=psP[:, 0:NE],
                                                     in1=pos[:].rearrange("p t e -> p (t e)"), op=ALU.add),
                     reads=[pkP, "pos"], writes=["pos"])
                S.op("dve", lambda e: e.tensor_scalar(out=ovf[:], in0=pos[:], scalar1=float(CAP), scalar2=BIGIDX,
                                                     op0=ALU.is_ge, op1=ALU.mult), reads=["pos"], writes=["ovf"])
                S.op("dve", lambda e: e.tensor_tensor(out=pos[:].rearrange("p t e -> p (t e)"),
                                                     in0=pos[:].rearrange("p t e -> p (t e)"), in1=eoff[:], op=ALU.add),
                     reads=["pos"] + RL, writes=["pos"])
                S.op("dve", lambda e: e.tensor_tensor(out=pos[:], in0=pos[:], in1=ovf[:], op=ALU.add),
                     reads=["pos", "ovf"], writes=["pos"])
                for kk, km in enumerate((k1, k2)):
                    S.op("dve", lambda e, km=km: e.tensor_tensor(out=prod[:], in0=km[:], in1=pos[:], op=ALU.mult),
                         reads=["k1", "k2", "pos"], writes=["prod"])
                    S.op("dve", lambda e, kk=kk: e.tensor_reduce(out=idf[:, kk, :], in_=prod[:], axis=mybir.AxisListType.X,
                                                                op=ALU.add), reads=["prod"], writes=["idf"])
                    S.op("dve", lambda e, kk=kk: e.tensor_copy(out=idx[:, :, kk], in_=idf[:, kk, :]), reads=["idf"], writes=["idx"])
                    S.op("dve", lambda e, kk=kk: e.tensor_scalar(out=idf[:, kk, :], in0=idf[:, kk, :], scalar1=float(NSLOT),
                                                                scalar2=None, op0=ALU.min), reads=["idf", "idx"], writes=["idf"])
                    S.op("dve", lambda e, kk=kk: e.tensor_copy(out=idxg[:, :, kk], in_=idf[:, kk, :]), reads=["idf"], writes=["idxg"])
                for i in range(NT):
                    s = i % 4
                    psT, pkT = self.PS()
                    psTb = psT.bitcast(BF16)

                    def emit_t(e, psTb=psTb, i=i):
                        ins = None
                        for k in range(KD):
                            ins = e.transpose(psTb[:, k * 128:(k + 1) * 128], ht[:, k, i * 128:(i + 1) * 128], c["ident_b"][:])
                        return ins
                    S.op("pe", emit_t, reads=HT + ["ident_b"], writes=[pkT])
                    S.op("act", lambda e, psTb=psTb, s=s: e.activation(out=hrow[s][:], in_=psTb[:, 0:1024], func=AF.Copy),
                         reads=[pkT], writes=[f"hrow{s}"])
                    for kk in range(2):
                        S.dma("pool", f"rs{kk}_{s}", lambda e, s=s, i=i, kk=kk: e.indirect_dma_start(
                            out=xg_s[:, :], out_offset=bass.IndirectOffsetOnAxis(ap=idx[:, i, kk:kk + 1], axis=0),
                            in_=hrow[s][:], in_offset=None, bounds_check=S.bnd, oob_is_err=False),
                            reads=[f"hrow{s}", "idx"] + XGI, writes=["xg"])
            S.barrier()

            with ExitStack() as es:
                def sb(name, shape, dt):
                    return es.enter_context(nc.sbuf_tensor(f"e_{name}", list(shape), dt))
                NB = CAP // 128
                xr = [sb(f"xr{i}", [128, 1024], BF16) for i in range(2)]
                xgT = [sb(f"xgT{i}", [128, 8, CAP], BF16) for i in range(2)]
                yacc = sb("yacc", [128, NB, 1024], F32)
                col_tiles = []
                c0 = 0
                while c0 < CAP:
                    n = min(512, CAP - c0)
                    col_tiles.append((c0, n))
                    c0 += n
                jobs = []
                for ex in range(NEXP):
                    xb = ex % 2

                    def pre(ex=ex, xb=xb):
                        for blk in range(NB):
                            s = blk % 2
                            r0 = ex * CAP + blk * 128
                            S.dma("sp", f"ex{s}", lambda e, s=s, r0=r0: e.dma_start(out=xr[s][:], in_=xg_s[r0:r0 + 128, :]),
                                  reads=["xg"], writes=[f"xr{s}"])
                            psT, pkT = self.PS()
                            psTb = psT.bitcast(BF16)

                            def emit_t(e, psTb=psTb, s=s):
                                ins = None
                                for k in range(KD):
                                    ins = e.transpose(psTb[:, k * 128:(k + 1) * 128], xr[s][:, k * 128:(k + 1) * 128],
                                                      c["ident_b"][:])
                                return ins
                            S.op("pe", emit_t, reads=[f"xr{s}", "ident_b"], writes=[pkT])
                            S.op("act", lambda e, psTb=psTb, blk=blk: e.activation(
                                out=xgT[xb][:, :, blk * 128:(blk + 1) * 128],
                                in_=psTb[:, 0:1024].rearrange("p (k n) -> p k n", k=8), func=AF.Copy),
                                reads=[pkT], writes=[f"xgT{xb}"])

                    def emit_y(jg, nj, Hh, hkeys, w2g, wkeys):
                        for blk in range(NB):
                            for half in range(2):
                                ps, pk = self.PS()
                                self.mm_group(ps[:, :], [(Hh[:, jj, blk * 128:(blk + 1) * 128], w2g[:, jj, half * 512:(half + 1) * 512])
                                                         for jj in range(nj)], reads=hkeys + wkeys, writes=[pk])
                                dst = yacc[:, blk, half * 512:(half + 1) * 512]
                                if jg == 0:
                                    S.op("act", lambda e, ps=ps, dst=dst: e.activation(out=dst, in_=ps[:, :], func=AF.Copy),
                                         reads=[pk], writes=["yacc"])
                                else:
                                    S.op("dve", lambda e, ps=ps, dst=dst: e.tensor_tensor(out=dst, in0=ps[:, :], in1=dst, op=ALU.add),
                                         reads=[pk, "yacc"], writes=["yacc"])

                    def post(ex=ex):
                        S.dma("sp", "ey", lambda e: e.dma_start(
                            out=ys_s[ex * CAP:(ex + 1) * CAP, :].rearrange("(b p) n -> p b n", p=128), in_=yacc[:]),
                            reads=["yacc"], writes=["ys"])

                    jobs.append(dict(hs=xgT[xb], hs_keys=[f"xgT{xb}"], col_tiles=col_tiles, w1=ew1[0, ex], w3=ew3[0, ex],
                                     w2=ew2[0, ex], nF=D_EXP // 128, pre=pre, emit_y=emit_y, post=post, early_pre=True))
                self.ffn_stream("x", sb, jobs, CAP)
            S.barrier()

            with ExitStack() as es:
                def sb(name, shape, dt):
                    return es.enter_context(nc.sbuf_tensor(f"o_{name}", list(shape), dt))
                fing = sb("fing", [128, 1024], F32)
                xt4 = [sb(f"xt4_{i}", [128, 8, 512], F32) for i in range(2)]
                y1 = [sb(f"y1{i}", [128, 1024], F32) for i in range(4)]
                y2 = [sb(f"y2{i}", [128, 1024], F32) for i in range(4)]
                acc = [sb(f"acc{i}", [128, 1024], F32) for i in range(4)]
                jk = sb("jk", [128, 1024], F32)
                ssq = [sb(f"ssq{i}", [128, 1], F32) for i in range(2)]
                sdo = [sb(f"sdo{i}", [128, 1], F32) for i in range(2)]
                ro = [sb(f"ro{i}", [128, 1], F32) for i in range(2)]
                S.dma("sp", "ol", lambda e: e.dma_start(out=fing[:], in_=fing_d), writes=["fing"])

                def part1(i):
                    s = i % 4
                    ti = 1 + i // 4
                    xb = (i // 4) % 2
                    xo = (i % 4) * 128
                    if i % 4 == 0:
                        col0 = HALO + i * 128
                        S.dma("sp", f"ox{xb}", lambda e, xb=xb, col0=col0: e.dma_start(
                            out=xt4[xb][:], in_=xm_s[:, col0:col0 + 512].rearrange("(k p) n -> p k n", p=128)),
                            reads=[("xm", 1, ti)], writes=[f"oxt{xb}"])
                    for kk, yb in enumerate((y1, y2)):
                        S.dma("pool", f"og{kk}{s}", lambda e, yb=yb, s=s, i=i, kk=kk: e.indirect_dma_start(
                            out=yb[s][:], out_offset=None, in_=ys_s[:, :],
                            in_offset=bass.IndirectOffsetOnAxis(ap=idxg[:, i, kk:kk + 1], axis=0),
                            bounds_check=S.bnd2, oob_is_err=False),
                            reads=["ys", "idxg"], writes=[f"oy{kk}{s}"])
                    for half in range(2):
                        ps, pk = self.PS()

                        def emit_t(e, ps=ps, xb=xb, xo=xo, half=half):
                            ins = None
                            for k in range(4):
                                kk_ = half * 4 + k
                                ins = e.transpose(ps[:, k * 128:(k + 1) * 128], xt4[xb][:, kk_, xo:xo + 128], c["ident_f"][:])
                            return ins
                        S.op("pe", emit_t, reads=[f"oxt{xb}"] + self.CST, writes=[pk])
                        sl = slice(half * 512, (half + 1) * 512)
                        S.op("dve", lambda e, ps=ps, s=s, sl=sl, i=i: e.scalar_tensor_tensor(
                            out=acc[s][:, sl], in0=y1[s][:, sl], scalar=gw[:, i, 0:1], in1=ps[:, :], op0=ALU.mult, op1=ALU.add),
                            reads=[pk, f"oy0{s}", "gw"], writes=[f"oacc{s}"])
                        S.op("dve", lambda e, s=s, sl=sl, i=i: e.scalar_tensor_tensor(
                            out=acc[s][:, sl], in0=y2[s][:, sl], scalar=gw[:, i, 1:2], in1=acc[s][:, sl], op0=ALU.mult, op1=ALU.add),
                            reads=[f"oy1{s}", "gw", f"oacc{s}"], writes=[f"oacc{s}"])
                    r = i % 2
                    S.op("act", lambda e, s=s, r=r: e.activation(out=jk[:], in_=acc[s][:], func=AF.Square, accum_out=ssq[r][:]),
                         reads=[f"oacc{s}"], writes=["jk", f"ssq{r}"])
                    S.op("act", lambda e, r=r: e.activation(out=sdo[r][:], in_=ssq[r][:], func=AF.Sqrt, scale=1.0 / D, bias=EPS),
                         reads=[f"ssq{r}"], writes=[f"sdo{r}"])

                def part2(i):
                    s = i % 4
                    r = i % 2
                    S.op("dve", lambda e, r=r: e.reciprocal(out=ro[r][:], in_=sdo[r][:]), reads=[f"sdo{r}"], writes=[f"ro{r}"])
                    S.op("dve", lambda e, s=s, r=r: e.scalar_tensor_tensor(out=acc[s][:], in0=acc[s][:], scalar=ro[r][:, 0:1],
                                                                          in1=fing[:], op0=ALU.mult, op1=ALU.mult),
                         reads=[f"oacc{s}", f"ro{r}", "fing"], writes=[f"oacc{s}"])
                    S.dma("act", f"oo{s}", lambda e, s=s, i=i: e.dma_start(out=out_d[i * 128:(i + 1) * 128, :], in_=acc[s][:]),
                          reads=[f"oacc{s}"], writes=[("out", i)])

                part1(0)
                for i in range(NT):
                    if i + 1 < NT:
                        part1(i + 1)
                    part2(i)


def host_inputs(inputs):
    f = np.float32
    x = np.asarray(inputs["x"], dtype=f)

    def pc(v, nchunk):
        v = np.asarray(v, dtype=f)
        L = v.shape[0]
        return np.ascontiguousarray(v.reshape(L, nchunk, 128).transpose(2, 0, 1).reshape(128, L * nchunk))

    shared = {}
    shared["ident"] = np.eye(128, dtype=f)
    jj, ii = np.meshgrid(np.arange(128), np.arange(128), indexing="ij")
    shared["tril"] = (jj <= ii).astype(f)
    shared["tri"] = (jj < ii).astype(f)
    shared["eoff"] = np.ascontiguousarray(np.broadcast_to(np.tile((np.arange(8) * CAP).astype(f), 32)[None, :], (128, 256)))
    shared["gmix"] = pc(inputs["mix_norm_g"], 8)
    shared["gffn"] = pc(inputs["ffn_norm_g"], 8)
    cw = np.asarray(inputs["conv_w"], dtype=f)
    shared["convw"] = np.ascontiguousarray(cw.reshape(2, 31, 4, 128).transpose(3, 0, 2, 1).reshape(128, 248))
    shared["convb"] = pc(inputs["conv_b"], 4)
    shared["clng"] = pc(inputs["conv_ln_g"], 4)
    shared["clnb"] = pc(inputs["conv_ln_b"], 4)
    shared["pscale"] = pc(inputs["pool_scale"], 8)
    shared["slng"] = np.ascontiguousarray(np.broadcast_to(np.asarray(inputs["sgu_ln_g"], dtype=f).reshape(1, 1024), (128, 1024)))
    shared["slnb"] = np.ascontiguousarray(np.broadcast_to(np.asarray(inputs["sgu_ln_b"], dtype=f).reshape(1, 1024), (128, 1024)))
    ws = np.asarray(inputs["sgu_w_s"], dtype=f)
    shared["wsT"] = np.ascontiguousarray(ws.transpose(3, 0, 1, 2).reshape(128, 1024))
    shared["bs"] = np.ascontiguousarray(np.asarray(inputs["sgu_b_s"], dtype=f).reshape(1, 1024))
    shared["fing"] = np.ascontiguousarray(np.broadcast_to(np.asarray(inputs["final_norm_g"], dtype=f).reshape(1, 1024), (128, 1024)))
    wr = np.asarray(inputs["router_w"], dtype=f)[0]
    shared["wr"] = np.ascontiguousarray(wr.reshape(8, 128, 8).transpose(1, 0, 2).reshape(128, 64))
    for k in ("w_in", "conv_w_out", "pool_w", "sgu_w_out", "w_out", "dense_w1", "dense_w3", "dense_w2",
              "expert_w1", "expert_w3", "expert_w2"):
        shared[k] = np.ascontiguousarray(np.asarray(inputs[k], dtype=f))
    in_maps = []
    for core in range(NCORES):
        b, half = core // 2, core % 2
        t0 = half * T_OWN
        xs = np.zeros((TT, D), dtype=f)
        xs[HALO:] = x[b, t0:t0 + T_OWN]
        if half == 1:
            xs[:HALO] = x[b, t0 - HALO:t0]
        m = dict(shared)
        m["xT"] = np.ascontiguousarray(xs.T)
        m["hmask"] = np.full((128, 1), float(half), dtype=f)
        rc = np.zeros((4, 16), dtype=f)
        for g in range(4):
            w = 2 << g
            for t in range(16):
                rc[g, t] = 1.0 / (min(t + 1, w) if half == 0 else w)
        m["rc"] = np.ascontiguousarray(np.broadcast_to(rc.reshape(1, 64), (128, 64)))
        in_maps.append(m)
    return in_maps


_NC_CACHE = {}


def kernel(**inputs):
    in_maps = host_inputs(inputs)
    if "nc" not in _NC_CACHE:
        _NC_CACHE["nc"] = Builder().build()
    nc = _NC_CACHE["nc"]
    res = run_bass_kernel_spmd(nc, in_maps, core_ids=list(range(NCORES)))
    out = np.empty((4, 8192, D), dtype=np.float32)
    for core in range(NCORES):
        b, half = core // 2, core % 2
        out[b, half * T_OWN:(half + 1) * T_OWN] = res.results[core]["out"]
    return out
```

```python
from contextlib import ExitStack

import numpy as np
import concourse.bass as bass
import concourse.mybir as mybir
from concourse.bass_utils import run_bass_kernel_spmd

F32 = mybir.dt.float32
BF16 = mybir.dt.bfloat16
I32 = mybir.dt.int32
AF = mybir.ActivationFunctionType
ALU = mybir.AluOpType

NCORES = 8
D = 1024
KD = 8
T_OWN = 4096
HALO = 128
TT = T_OWN + HALO
D_IN = 5632
SPLIT_V = 2560
D_FF = 2816
D_EXP = 3584
NEXP = 8
CAP = 1536
NSLOT = NEXP * CAP
EPS = 1e-6
BIGIDX = float(1 << 20)

COMPUTE = ("pe", "act", "dve", "pool")
ALL_ENG = ("pe", "act", "dve", "pool", "sp")


class Sched:
    def __init__(self, nc):
        self.nc = nc
        self.streams = {e: [] for e in ALL_ENG}
        self.sem = {}
        self.cnt = {}
        for e in COMPUTE:
            self.sem[e] = nc.alloc_semaphore("prog_" + e)
            self.cnt[e] = 0
        self.known = {e: {} for e in ALL_ENG}
        self.last_write = {}
        self.readers = {}
        self.lanes = {}
        self.free_sems = {"pool": [], "sp": []}
        self.lane_q = {}
        self.nsem = 0

    def lane(self, name, queue="sp"):
        queue = "pool" if queue == "pool" else "sp"
        if name not in self.lanes:
            if self.free_sems[queue]:
                self.lanes[name] = self.free_sems[queue].pop()
            else:
                self.nsem += 1
                self.lanes[name] = [self.nc.alloc_semaphore(f"ln{self.nsem}"), 0]
            self.lane_q[name] = queue
        assert self.lane_q[name] == queue, (name, queue)
        return self.lanes[name]

    def _deps(self, reads, writes):
        deps = {}

        def add(d):
            if d is None:
                return
            k, v = d
            if deps.get(k, 0) < v:
                deps[k] = v

        for k in reads:
            add(self.last_write.get(k))
        for k in writes:
            add(self.last_write.get(k))
            for rk, rv in self.readers.get(k, {}).items():
                add((rk, rv))
        return deps

    def _emit_waits(self, eng, deps):
        st = self.streams[eng]
        kn = self.known[eng]
        for k, v in deps.items():
            if kn.get(k, 0) >= v:
                continue
            kn[k] = v
            st.append(("wait", self.semof(k), v))

    def _record(self, key, reads, writes):
        sk, sv = key
        for k in reads:
            r = self.readers.setdefault(k, {})
            if r.get(sk, 0) < sv:
                r[sk] = sv
        for k in writes:
            self.last_write[k] = key
            self.readers[k] = {}

    def op(self, eng, emit, reads=(), writes=()):
        deps = self._deps(reads, writes)
        sk = ("c", eng)
        self._emit_waits(eng, deps)
        self.cnt[eng] += 1
        self.streams[eng].append(("op", emit, self.semof(sk)))
        self._record((sk, self.cnt[eng]), reads, writes)

    def dma(self, queue, lane, emit, reads=(), writes=()):
        ln = self.lane(lane, queue)
        sk = ("l", lane)
        deps = self._deps(reads, writes)
        if ln[1] > 0 and deps.get(sk, 0) < ln[1]:
            deps[sk] = ln[1]
        self._emit_waits(queue, deps)
        ln[1] += 16
        self.streams[queue].append(("dma", emit, ln[0]))
        self._record((sk, ln[1]), reads, writes)

    def barrier(self):
        deps = {("c", e): self.cnt[e] for e in COMPUTE if self.cnt[e] > 0}
        for name, ln in self.lanes.items():
            if ln[1] > 0:
                deps[("l", name)] = ln[1]
        for eng in ALL_ENG:
            self._emit_waits(eng, dict(deps))
        for name, ln in self.lanes.items():
            self.free_sems[self.lane_q[name]].append(ln)
        self.lanes = {}
        self.lane_q = {}
        for eng in ALL_ENG:
            self.known[eng] = {k: v for k, v in self.known[eng].items() if k[0] == "c"}
        self.last_write = {k: v for k, v in self.last_write.items() if v[0][0] == "c"}
        for k in list(self.readers.keys()):
            self.readers[k] = {rk: rv for rk, rv in self.readers[k].items() if rk[0] == "c"}

    def final_wait(self, eng, keys):
        deps = {}
        for k in keys:
            d = self.last_write.get(k)
            if d is not None and deps.get(d[0], 0) < d[1]:
                deps[d[0]] = d[1]
        self._emit_waits(eng, deps)

    def semof(self, k):
        if k[0] == "c":
            return self.sem[k[1]]
        return self.lanes[k[1]][0]

    def emit(self):
        nc = self.nc
        with nc.Block() as block:
            def run(e, stream):
                for item in stream:
                    if item[0] == "wait":
                        e.wait_ge(item[1], item[2])
                    elif item[0] == "op":
                        item[1](e).then_inc(item[2], 1)
                    else:
                        item[1](e).then_inc(item[2], 16)

            @block.tensor
            def _(e):
                run(e, self.streams["pe"])

            @block.scalar
            def _(e):
                run(e, self.streams["act"])

            @block.vector
            def _(e):
                run(e, self.streams["dve"])

            @block.gpsimd
            def _(e):
                with e.register("bnd") as reg, e.register("bnd2") as reg2:
                    e.reg_mov(reg, NSLOT - 1)
                    e.reg_mov(reg2, NSLOT + 127)
                    self.bnd = reg
                    self.bnd2 = reg2
                    run(e, self.streams["pool"])

            @block.sync
            def _(e):
                run(e, self.streams["sp"])


def tiles_of():
    res = [(0, HALO)]
    for i in range(T_OWN // 512):
        res.append((HALO + i * 512, 512))
    return res


class Builder:
    def __init__(self, phases=99, debug=False):
        self.phases = phases
        self.debug = debug
        nc = bass.Bass("TRN2", target_bir_lowering=False)
        self.nc = nc
        self.S = Sched(nc)
        self.psn = 0
        self.ps = [nc.alloc_psum_tensor(f"psb{i}", [128, 512], F32) for i in range(8)]
        self.dram_in = {}
        self.uid = 0

    def din(self, name, shape, dt=F32):
        t = self.nc.dram_tensor(name, list(shape), dt, kind="ExternalInput").ap()
        self.dram_in[name] = t
        return t

    def dscr(self, name, shape, dt):
        kind = "ExternalOutput" if self.debug else "Internal"
        if self.debug:
            return self.nc.dram_tensor(name, list(shape), dt, kind="ExternalOutput").ap()
        return self.nc.dram_tensor(name, list(shape), dt).ap()

    def PS(self):
        i = self.psn % 8
        self.psn += 1
        return self.ps[i], f"ps{i}"

    def lname(self, base):
        self.uid += 1
        return f"{base}"

    def mm_group(self, out_ap, pairs, reads, writes):
        n = len(pairs)

        def emit(e, pairs=pairs, out_ap=out_ap, n=n):
            ins = None
            for j, (l, r) in enumerate(pairs):
                ins = e.matmul(out_ap, lhsT=l, rhs=r, start=(j == 0), stop=(j == n - 1))
            return ins

        self.S.op("pe", emit, reads=reads, writes=writes)

    def build(self):
        nc, S = self.nc, self.S
        din = self.din
        xT = din("xT", [D, TT])
        hmask = din("hmask", [128, 1])
        rc_d = din("rc", [128, 64])
        ident_d = din("ident", [128, 128])
        tril_d = din("tril", [128, 128])
        tri_d = din("tri", [128, 128])
        eoff_d = din("eoff", [128, 256])
        gmix_d = din("gmix", [128, 16])
        gffn_d = din("gffn", [128, 16])
        convw_d = din("convw", [128, 2 * 4 * 31])
        convb_d = din("convb", [128, 8])
        clng_d = din("clng", [128, 8])
        clnb_d = din("clnb", [128, 8])
        pscale_d = din("pscale", [128, 16])
        slng_d = din("slng", [128, 1024])
        slnb_d = din("slnb", [128, 1024])
        wsT_d = din("wsT", [128, 2 * 4 * 128])
        bs_d = din("bs", [1, 2 * 4 * 128])
        fing_d = din("fing", [128, 1024])
        wr_d = din("wr", [128, 64])
        w_in = din("w_in", [2, D, D_IN])
        conv_w_out = din("conv_w_out", [2, 512, D])
        pool_w = din("pool_w", [2, 4, 128, 256])
        sgu_w_out = din("sgu_w_out", [2, 512, D])
        w_out = din("w_out", [2, D, D])
        dense_w1 = din("dense_w1", [1, D, D_FF])
        dense_w3 = din("dense_w3", [1, D, D_FF])
        dense_w2 = din("dense_w2", [1, D_FF, D])
        expert_w1 = din("expert_w1", [1, NEXP, D, D_EXP])
        expert_w3 = din("expert_w3", [1, NEXP, D, D_EXP])
        expert_w2 = din("expert_w2", [1, NEXP, D_EXP, D])
        out_d = nc.dram_tensor("out", [T_OWN, D], F32, kind="ExternalOutput").ap()
        hT_s = self.dscr("hT_s", [D, TT], BF16)
        T_s = self.dscr("T_s", [D, TT], F32)
        xm_s = [self.dscr("xm0_s", [D, TT], F32), self.dscr("xm1_s", [D, TT], F32)]
        h2T_s = self.dscr("h2T_s", [D, TT], BF16)
        xf0_s = self.dscr("xf0_s", [D, TT], F32)
        xg_s = self.dscr("xg_s", [NSLOT, D], BF16)
        ys_s = self.dscr("ys_s", [NSLOT + 128, D], F32)
        cnt_s = self.dscr("cnt_s", [128, 8], F32)

        with ExitStack() as gs:
            def sb(name, shape, dt, es=gs):
                return es.enter_context(nc.sbuf_tensor("g_" + name, list(shape), dt))

            ident_f = sb("ident_f", [128, 128], F32)
            ident_b = sb("ident_b", [128, 128], BF16)
            ones_b = sb("ones_b", [128, 128], BF16)
            hmask_t = sb("hmask_t", [128, 1], F32)
            rc_t = sb("rc_t", [128, 64], F32)
            gmix = sb("gmix", [128, 16], F32)
            gffn = sb("gffn", [128, 16], F32)
            convw = sb("convw", [128, 248], F32)
            convb = sb("convb", [128, 8], F32)
            clng = sb("clng", [128, 8], F32)
            clnb = sb("clnb", [128, 8], F32)
            pscale = sb("pscale", [128, 16], F32)
            smalls = [(ident_f, ident_d), (hmask_t, hmask), (rc_t, rc_d), (gmix, gmix_d), (gffn, gffn_d),
                      (convw, convw_d), (convb, convb_d), (clng, clng_d), (clnb, clnb_d), (pscale, pscale_d)]
            for j, (t, d_) in enumerate(smalls):
                S.dma("sp", "cst", lambda e, t=t, d_=d_: e.dma_start(out=t[:], in_=d_), writes=[f"cst{j}"])
            CST = [f"cst{j}" for j in range(len(smalls))]
            S.op("dve", lambda e: e.tensor_copy(out=ident_b[:], in_=ident_f[:]), reads=CST, writes=["ident_b"])
            S.op("dve", lambda e: e.memset(ones_b[:], 1.0), writes=["ones_b"])
            CST += ["ident_b", "ones_b"]
            self.CST = CST
            self.c = dict(ident_f=ident_f, ident_b=ident_b, ones_b=ones_b, hmask=hmask_t, rc=rc_t, gmix=gmix,
                          gffn=gffn, convw=convw, convb=convb, clng=clng, clnb=clnb, pscale=pscale)

            tiles = tiles_of()
            ph = 0
            for layer in range(2):
                xsrc = xT if layer == 0 else xf0_s
                xsrc_key = "xT" if layer == 0 else "xf0"
                ph += 1
                if self.phases >= ph:
                    self.pass_a(layer, tiles, xsrc, xsrc_key, w_in, conv_w_out, hT_s, T_s)
                    S.barrier()
                ph += 1
                if self.phases >= ph:
                    self.pass_b(layer, tiles, w_in, pool_w, hT_s, T_s)
                    S.barrier()
                ph += 1
                if self.phases >= ph:
                    self.pass_c(layer, tiles, xsrc, xsrc_key, w_in, sgu_w_out, w_out, slng_d, slnb_d, wsT_d, bs_d,
                                tril_d, hT_s, T_s, xm_s[layer], h2T_s)
                    S.barrier()
                ph += 1
                if self.phases >= ph:
                    if layer == 0:
                        self.dense_ffn(tiles, dense_w1, dense_w3, dense_w2, h2T_s, xm_s[0], xf0_s, xg_s, ys_s)
                        S.barrier()
                    else:
                        self.moe(expert_w1, expert_w3, expert_w2, wr_d, tri_d, eoff_d, fing_d, h2T_s, xm_s[1],
                                 xg_s, ys_s, cnt_s, out_d)
            S.final_wait("sp", list(S.last_write.keys()))
        S.emit()
        return nc

    def norm_fm(self, xt, xkey, N, gcol, sq, sd, rs, h, hkey):
        S = self.S
        ones_b = self.c["ones_b"]
        for c in range(KD):
            S.op("act", lambda e, c=c: e.activation(out=sq[:, c, :N], in_=xt[:, c, :N], func=AF.Square),
                 reads=[xkey], writes=[f"sq{c}"])
        ps, pk = self.PS()
        self.mm_group(ps[:, :N], [(ones_b[:], sq[:, c, :N]) for c in range(KD)],
                      reads=[f"sq{c}" for c in range(KD)] + ["ones_b"], writes=[pk])
        S.op("act", lambda e: e.activation(out=sd[:, :N], in_=ps[:, :N], func=AF.Sqrt, scale=1.0 / D, bias=EPS),
             reads=[pk], writes=["sd"])
        S.op("dve", lambda e: e.reciprocal(out=rs[:, :N], in_=sd[:, :N]), reads=["sd"], writes=["rs"])
        for c in range(KD):
            S.op("dve", lambda e, c=c: e.scalar_tensor_tensor(out=h[:, c, :N], in0=xt[:, c, :N], scalar=gcol[:, c:c + 1],
                                                            in1=rs[:, :N], op0=ALU.mult, op1=ALU.mult),
                 reads=[xkey, "rs"] + self.CST, writes=[hkey])

    def load_w(self, dst, src, key):
        self.S.dma("pool", "w_" + key, lambda e: e.dma_start(out=dst, in_=src), writes=[key])

    def pass_a(self, layer, tiles, xsrc, xsrc_key, w_in, conv_w_out, hT_s, T_s):
        nc, S, c = self.nc, self.S, self.c
        with ExitStack() as es:
            def sb(name, shape, dt):
                return es.enter_context(nc.sbuf_tensor(f"a{layer}_{name}", list(shape), dt))
            xt = [sb(f"xt{i}", [128, 8, 512], F32) for i in range(2)]
            sq = sb("sq", [128, 8, 512], BF16)
            sd = sb("sd", [128, 512], F32)
            rs = sb("rs", [128, 512], F32)
            h = [sb(f"h{i}", [128, 8, 512], BF16) for i in range(2)]
            wA = sb("wA", [128, 8, 2048], BF16)
            wco = sb("wco", [128, 4, 1024], BF16)
            diag = sb("diag", [128, 124, 128], BF16)
            sgA = [sb(f"sgA{i}", [128, 8, 512], BF16) for i in range(2)]
            sg4 = sb("sg4", [128, 4, 512], BF16)
            abuf = [sb(f"abuf{i}", [128, 4, 30 + 512], BF16) for i in range(2)]
            cc = sb("cc", [128, 4, 512], F32)
            csq = sb("csq", [128, 4, 512], BF16)
            cbf = sb("cbf", [128, 4, 512], BF16)
            msq = sb("msq", [128, 512], F32)
            var = sb("var", [128, 512], F32)
            sd2 = sb("sd2", [128, 512], F32)
            rs2 = sb("rs2", [128, 512], F32)
            ctmp = [sb(f"ctmp{i}", [128, 512], F32) for i in range(2)]
            cact = sb("cact", [128, 4, 512], BF16)
            tout = [sb(f"tout{i}", [128, 512], F32) for i in range(4)]

            w_l = w_in[layer].rearrange("(k p) n -> p k n", p=128)
            for half in range(2):
                self.load_w(wA[:, :, 1024 + half * 512:1024 + (half + 1) * 512],
                            w_l[:, :, half * 512:(half + 1) * 512], f"wA{2 + half}")
            for half in range(2):
                self.load_w(wA[:, :, half * 512:(half + 1) * 512],
                            w_l[:, :, SPLIT_V + half * 512:SPLIT_V + (half + 1) * 512], f"wA{half}")
            WA = [f"wA{i}" for i in range(4)]
            cwo = conv_w_out[layer].rearrange("(k p) n -> p k n", p=128)
            for half in range(2):
                self.load_w(wco[:, :, half * 512:(half + 1) * 512], cwo[:, :, half * 512:(half + 1) * 512], f"wco{half}")
            WCO = ["wco0", "wco1"]
            for j in range(124):
                col = layer * 124 + j
                if j % 3 != 2:
                    S.op("dve", lambda e, j=j, col=col: e.tensor_scalar(out=diag[:, j, :], in0=c["ident_f"][:],
                                                                       scalar1=c["convw"][:, col:col + 1], scalar2=None,
                                                                       op0=ALU.mult),
                         reads=self.CST, writes=[f"diag_{j}"])
                else:
                    S.op("act", lambda e, j=j, col=col: e.activation(out=diag[:, j, :], in_=c["ident_f"][:], func=AF.Copy,
                                                                    scale=c["convw"][:, col:col + 1]),
                         reads=self.CST, writes=[f"diag_{j}"])
            S.op("pool", lambda e: e.memset(abuf[0][:, :, 0:30], 0.0), writes=[f"abuf0_{m}" for m in range(4)])
            nt = len(tiles)

            def stage0(i):
                if i >= nt:
                    return
                col0, N = tiles[i]
                s = i % 2
                S.dma("sp", f"ax{s}", lambda e: e.dma_start(out=xt[s][:, :, :N],
                                                           in_=xsrc[:, col0:col0 + N].rearrange("(k p) n -> p k n", p=128)),
                      reads=[(xsrc_key, i)], writes=[f"xt{s}"])
                self.norm_fm(xt[s], f"xt{s}", N, c["gmix"][:, layer * 8:(layer + 1) * 8], sq, sd, rs, h[s], f"h{s}")
                S.dma("pool", f"ah{s}", lambda e: e.dma_start(out=hT_s[:, col0:col0 + N].rearrange("(k p) n -> p k n", p=128),
                                                             in_=h[s][:, :, :N]),
                      reads=[f"h{s}"], writes=[("hT", i)])

            def is_full(i):
                return not (layer == 1 and i == 0)

            def front(i):
                if i >= nt:
                    return
                col0, N = tiles[i]
                s = i % 2
                hk = f"h{s}"
                ab = abuf[s]
                for m in range(4):
                    ps, pk = self.PS()
                    cb = 1024 + 512 + m * 128
                    self.mm_group(ps[:, :N], [(wA[:, k, cb:cb + 128], h[s][:, k, :N]) for k in range(KD)],
                                  reads=[hk] + WA, writes=[pk])
                    S.op("act", lambda e, ps=ps, m=m: e.activation(out=sg4[:, m, :N], in_=ps[:, :N], func=AF.Sigmoid),
                         reads=[pk], writes=[f"sg4{m}"])
                for m in range(4):
                    ps, pk = self.PS()
                    cb = 1024 + m * 128
                    self.mm_group(ps[:, :N], [(wA[:, k, cb:cb + 128], h[s][:, k, :N]) for k in range(KD)],
                                  reads=[hk] + WA, writes=[pk])
                    S.op("dve", lambda e, ps=ps, m=m: e.tensor_tensor(out=ab[:, m, 30:30 + N], in0=ps[:, :N],
                                                                    in1=sg4[:, m, :N], op=ALU.mult),
                         reads=[pk, f"sg4{m}"], writes=[f"abuf{s}_{m}"])
                    S.op("pool", lambda e, m=m: e.tensor_copy(out=abuf[1 - s][:, m, 0:30], in_=ab[:, m, N:N + 30]),
                         reads=[f"abuf{s}_{m}"], writes=[f"abuf{1 - s}_{m}"])
                if is_full(i):
                    for m in range(8):
                        ps, pk = self.PS()
                        self.mm_group(ps[:, :N], [(wA[:, k, m * 128:(m + 1) * 128], h[s][:, k, :N]) for k in range(KD)],
                                      reads=[hk] + WA, writes=[pk])
                        S.op("act", lambda e, ps=ps, m=m: e.activation(out=sgA[s][:, m, :N], in_=ps[:, :N], func=AF.Sigmoid),
                             reads=[pk], writes=[f"sgA{s}_{m}"])

            def back1(i):
                if not is_full(i):
                    return
                col0, N = tiles[i]
                s = i % 2
                ab = abuf[s]
                for m in range(4):
                    ps, pk = self.PS()
                    self.mm_group(ps[:, :N], [(diag[:, m * 31 + k, :], ab[:, m, k:k + N]) for k in range(31)],
                                  reads=[f"abuf{s}_{m}"] + [f"diag_{m * 31 + k}" for k in range(31)], writes=[pk])
                    S.op("act", lambda e, ps=ps, m=m: e.activation(out=cc[:, m, :N], in_=ps[:, :N], func=AF.Identity,
                                                                 bias=c["convb"][:, layer * 4 + m:layer * 4 + m + 1],
                                                                 scale=1.0),
                         reads=[pk] + self.CST, writes=[f"cc{m}"])
                    S.op("act", lambda e, ps=ps, m=m: e.activation(out=csq[:, m, :N], in_=ps[:, :N], func=AF.Square,
                                                                 bias=c["convb"][:, layer * 4 + m:layer * 4 + m + 1],
                                                                 scale=1.0),
                         reads=[pk] + self.CST, writes=[f"csq{m}"])
                    S.op("act", lambda e, ps=ps, m=m: e.activation(out=cbf[:, m, :N], in_=ps[:, :N], func=AF.Identity,
                                                                 bias=c["convb"][:, layer * 4 + m:layer * 4 + m + 1],
                                                                 scale=1.0),
                         reads=[pk] + self.CST, writes=[f"cbf{m}"])
                psM, pkM = self.PS()
                self.mm_group(psM[:, :N], [(c["ones_b"][:], cbf[:, m, :N]) for m in range(4)],
                              reads=[f"cbf{m}" for m in range(4)] + ["ones_b"], writes=[pkM])
                psQ, pkQ = self.PS()
                self.mm_group(psQ[:, :N], [(c["ones_b"][:], csq[:, m, :N]) for m in range(4)],
                              reads=[f"csq{m}" for m in range(4)] + ["ones_b"], writes=[pkQ])
                S.op("act", lambda e: e.activation(out=msq[:, :N], in_=psM[:, :N], func=AF.Square, scale=1.0 / 512),
                     reads=[pkM], writes=["msq"])
                S.op("dve", lambda e: e.scalar_tensor_tensor(out=var[:, :N], in0=psQ[:, :N], scalar=1.0 / 512,
                                                            in1=msq[:, :N], op0=ALU.mult, op1=ALU.subtract),
                     reads=[pkQ, "msq"], writes=["var"])
                S.op("act", lambda e: e.activation(out=sd2[:, :N], in_=var[:, :N], func=AF.Sqrt, scale=1.0, bias=EPS),
                     reads=["var"], writes=["sd2"])
                S.op("dve", lambda e: e.reciprocal(out=rs2[:, :N], in_=sd2[:, :N]), reads=["sd2"], writes=["rs2"])
                for m in range(4):
                    t = ctmp[m % 2]
                    tk = f"ctmp{m % 2}"
                    S.op("dve", lambda e, m=m, t=t: e.scalar_tensor_tensor(out=t[:, :N], in0=psM[:, :N], scalar=-1.0 / 512,
                                                                         in1=cc[:, m, :N], op0=ALU.mult, op1=ALU.add),
                         reads=[pkM, f"cc{m}"], writes=[tk])
                    S.op("dve", lambda e, t=t: e.tensor_tensor(out=t[:, :N], in0=t[:, :N], in1=rs2[:, :N], op=ALU.mult),
                         reads=[tk, "rs2"], writes=[tk])
                    S.op("act", lambda e, m=m, t=t: e.activation(out=cact[:, m, :N], in_=t[:, :N], func=AF.Silu,
                                                               scale=c["clng"][:, layer * 4 + m:layer * 4 + m + 1],
                                                               bias=c["clnb"][:, layer * 4 + m:layer * 4 + m + 1]),
                         reads=[tk] + self.CST, writes=[f"cact{m}"])

            def back2(i):
                if not is_full(i):
                    return
                col0, N = tiles[i]
                s = i % 2
                for cc_ in range(8):
                    ps, pk = self.PS()
                    self.mm_group(ps[:, :N], [(wco[:, k, cc_ * 128:(cc_ + 1) * 128], cact[:, k, :N]) for k in range(4)],
                                  reads=[f"cact{m}" for m in range(4)] + WCO, writes=[pk])
                    r = cc_ % 4
                    S.op("dve", lambda e, ps=ps, cc_=cc_, r=r: e.tensor_tensor(out=tout[r][:, :N], in0=ps[:, :N],
                                                                             in1=sgA[s][:, cc_, :N], op=ALU.mult),
                         reads=[pk, f"sgA{s}_{cc_}"], writes=[f"tout{r}"])
                    S.dma("pool", f"at{r}", lambda e, cc_=cc_, r=r: e.dma_start(
                        out=T_s[cc_ * 128:(cc_ + 1) * 128, col0:col0 + N], in_=tout[r][:, :N]),
                        reads=[f"tout{r}"], writes=[("T", i, cc_)])

            stage0(0)
            stage0(1)
            front(0)
            for i in range(nt):
                back1(i)
                front(i + 1)
                back2(i)
                stage0(i + 2)

    def pass_b(self, layer, tiles, w_in, pool_w, hT_s, T_s):
        nc, S, c = self.nc, self.S, self.c
        with ExitStack() as es:
            def sb(name, shape, dt):
                return es.enter_context(nc.sbuf_tensor(f"b{layer}_{name}", list(shape), dt))
            hb = [sb(f"hb{i}", [128, 8, 512], BF16) for i in range(2)]
            wB = sb("wB", [128, 8, 1536], BF16)
            wp = sb("wp", [128, 4, 256], BF16)
            sgB = [sb(f"sgB{i}", [128, 8, 512], BF16) for i in range(2)]
            L = 16 + 512
            pbuf = [sb(f"pbuf{i}", [128, 4, L], F32) for i in range(3)]
            P1 = sb("P1", [128, 4, L], F32)
            P2 = sb("P2", [128, 4, L], F32)
            pooled = sb("pooled", [128, 4, 512], BF16)
            t16 = sb("t16", [128, 16], F32)
            tin = [[sb(f"tin{j}_{i}", [128, 512], F32) for i in range(8)] for j in range(2)]
            tmpb = [sb(f"tmpb{i}", [128, 512], F32) for i in range(2)]
            tout = [sb(f"tout{i}", [128, 512], F32) for i in range(4)]

            w_l = w_in[layer].rearrange("(k p) n -> p k n", p=128)
            self.load_w(wB[:, :, 1024:1536], w_l[:, :, 1024:1536], "wB2")
            for half in range(2):
                cb = SPLIT_V + 1024 + half * 512
                self.load_w(wB[:, :, half * 512:(half + 1) * 512], w_l[:, :, cb:cb + 512], f"wB{half}")
            WB = ["wB0", "wB1", "wB2"]
            self.load_w(wp[:], pool_w[layer].rearrange("g p n -> p g n"), "wp")
            S.op("pool", lambda e: e.memset(pbuf[0][:, :, 0:16], 0.0), writes=["pbuf0"])
            nt = len(tiles)

            def is_full(i):
                return not (layer == 1 and i == 0)

            def stage0(i):
                if i >= nt:
                    return
                col0, N = tiles[i]
                s = i % 2
                S.dma("sp", f"bh{s}", lambda e: e.dma_start(out=hb[s][:, :, :N],
                                                           in_=hT_s[:, col0:col0 + N].rearrange("(k p) n -> p k n", p=128)),
                      reads=[("hT", i)], writes=[f"hb{s}"])

            def front(i):
                if i >= nt:
                    return
                col0, N = tiles[i]
                s = i % 2
                hk = f"hb{s}"
                s3 = i % 3
                n3 = (i + 1) % 3
                pb = pbuf[s3]
                for g in range(4):
                    ps, pk = self.PS()
                    cb = 1024 + g * 128
                    self.mm_group(ps[:, :N], [(wB[:, k, cb:cb + 128], hb[s][:, k, :N]) for k in range(KD)],
                                  reads=[hk] + WB, writes=[pk])
                    S.op("act", lambda e, ps=ps, g=g: e.activation(out=pb[:, g, 16:16 + N], in_=ps[:, :N], func=AF.Copy),
                         reads=[pk], writes=[f"pbuf{s3}"])
                S.op("pool", lambda e: e.tensor_copy(out=pbuf[n3][:, :, 0:16], in_=pb[:, :, N:N + 16]),
                     reads=[f"pbuf{s3}"], writes=[f"pbuf{n3}"])
                if is_full(i):
                    for cc_ in range(8):
                        S.dma("sp", f"bt{s}_{cc_}", lambda e, cc_=cc_: e.dma_start(
                            out=tin[s][cc_][:, :N], in_=T_s[cc_ * 128:(cc_ + 1) * 128, col0:col0 + N]),
                            reads=[("T", i, cc_)], writes=[f"tin{s}_{cc_}"])
                    for m in range(8):
                        ps, pk = self.PS()
                        self.mm_group(ps[:, :N], [(wB[:, k, m * 128:(m + 1) * 128], hb[s][:, k, :N]) for k in range(KD)],
                                      reads=[hk] + WB, writes=[pk])
                        S.op("act", lambda e, ps=ps, m=m: e.activation(out=sgB[s][:, m, :N], in_=ps[:, :N], func=AF.Sigmoid),
                             reads=[pk], writes=[f"sgB{s}_{m}"])

            def back(i):
                if not is_full(i):
                    return
                col0, N = tiles[i]
                s = i % 2
                pb = pbuf[i % 3]
                pbk = f"pbuf{i % 3}"
                Le = 16 + N
                S.op("pool", lambda e: e.tensor_tensor(out=P1[:, :, 1:Le], in0=pb[:, :, 1:Le], in1=pb[:, :, 0:Le - 1],
                                                      op=ALU.add), reads=[pbk], writes=["P1"])
                S.op("pool", lambda e: e.tensor_tensor(out=P2[:, 1:4, 3:Le], in0=P1[:, 1:4, 3:Le], in1=P1[:, 1:4, 1:Le - 2],
                                                      op=ALU.add), reads=["P1"], writes=["P2"])
                S.op("pool", lambda e: e.tensor_tensor(out=P1[:, 2:4, 7:Le], in0=P2[:, 2:4, 7:Le], in1=P2[:, 2:4, 3:Le - 4],
                                                      op=ALU.add), reads=["P2"], writes=["P1"])
                S.op("pool", lambda e: e.tensor_tensor(out=P2[:, 3:4, 15:Le], in0=P1[:, 3:4, 15:Le], in1=P1[:, 3:4, 7:Le - 8],
                                                      op=ALU.add), reads=["P1"], writes=["P2"])
                srcs = [P1, P2, P1, P2]
                skeys = [["P1"], ["P2"], ["P1"], ["P2"]]
                for g in range(4):
                    wv = float(2 << g)
                    S.op("dve", lambda e, g=g, wv=wv: e.scalar_tensor_tensor(
                        out=pooled[:, g, :N], in0=srcs[g][:, g, 16:16 + N], scalar=1.0 / wv, in1=pb[:, g, 16:16 + N],
                        op0=ALU.mult, op1=ALU.subtract), reads=skeys[g] + [pbk], writes=[f"pooled{g}"])
                    if i == 1:
                        S.op("dve", lambda e, g=g: e.tensor_tensor(out=t16[:], in0=srcs[g][:, g, 16:32],
                                                                  in1=c["rc"][:, g * 16:(g + 1) * 16], op=ALU.mult),
                             reads=skeys[g] + self.CST, writes=["t16"])
                        S.op("dve", lambda e, g=g: e.tensor_tensor(out=pooled[:, g, 0:16], in0=t16[:],
                                                                  in1=pb[:, g, 16:32], op=ALU.subtract),
                             reads=["t16", pbk], writes=[f"pooled{g}"])
                for cc_ in range(8):
                    g, jj = cc_ // 2, cc_ % 2
                    r = cc_ % 4
                    ps, pk = self.PS()
                    self.mm_group(ps[:, :N], [(wp[:, g, jj * 128:(jj + 1) * 128], pooled[:, g, :N])],
                                  reads=[f"pooled{g}", "wp"], writes=[pk])
                    tb = tmpb[cc_ % 2]
                    tbk = f"tmpb{cc_ % 2}"
                    S.op("dve", lambda e, ps=ps, cc_=cc_, tb=tb: e.scalar_tensor_tensor(
                        out=tb[:, :N], in0=ps[:, :N], scalar=c["pscale"][:, layer * 8 + cc_:layer * 8 + cc_ + 1],
                        in1=sgB[s][:, cc_, :N], op0=ALU.mult, op1=ALU.mult),
                        reads=[pk, f"sgB{s}_{cc_}"] + self.CST, writes=[tbk])
                    S.op("dve", lambda e, tb=tb, r=r, cc_=cc_: e.tensor_tensor(out=tout[r][:, :N], in0=tb[:, :N],
                                                                             in1=tin[s][cc_][:, :N], op=ALU.add),
                         reads=[tbk, f"tin{s}_{cc_}"], writes=[f"tout{r}"])
                    S.dma("pool", f"bo{r}", lambda e, cc_=cc_, r=r: e.dma_start(
                        out=T_s[cc_ * 128:(cc_ + 1) * 128, col0:col0 + N], in_=tout[r][:, :N]),
                        reads=[f"tout{r}"], writes=[("T", i, cc_)])

            stage0(0)
            stage0(1)
            front(0)
            for i in range(nt):
                front(i + 1)
                back(i)
                stage0(i + 2)

    def pass_c(self, layer, tiles, xsrc, xsrc_key, w_in, sgu_w_out, w_out, slng_d, slnb_d, wsT_d, bs_d, tril_d,
               hT_s, T_s, xm_s, h2T_s):
        nc, S, c = self.nc, self.S, self.c
        with ExitStack() as es:
            def sb(name, shape, dt):
                return es.enter_context(nc.sbuf_tensor(f"c{layer}_{name}", list(shape), dt))
            hb = [sb(f"hb{i}", [128, 8, 512], BF16) for i in range(2)]
            xt = [sb(f"xt{i}", [128, 8, 512], F32) for i in range(2)]
            wC = sb("wC", [128, 8, 2048], BF16)
            wso = sb("wso", [128, 4, 1024], BF16)
            wo = sb("wo", [128, 8, 1024], BF16)
            sgC = [sb(f"sgC{i}", [128, 8, 512], BF16) for i in range(2)]
            u = sb("u", [128, 4, 512], F32)
            st6 = sb("st6", [128, 4, 6], F32)
            mv = sb("mv", [128, 4, 2], F32)
            sdv = sb("sdv", [128, 4], F32)
            rv = sb("rv", [128, 4], F32)
            nb = sb("nb", [128, 4], F32)
            vtmp = [sb(f"vtmp{i}", [128, 512], F32) for i in range(4)]
            vn = sb("vn", [128, 4, 512], BF16)
            slng = sb("slng", [128, 512], F32)
            slnb = sb("slnb", [128, 512], F32)
            wsf = sb("wsf", [128, 512], F32)
            trl = sb("trl", [128, 128], F32)
            wsm = sb("wsm", [128, 4, 128], BF16)
            bsf = sb("bsf", [1, 512], F32)
            bsr = sb("bsr", [1, 512], F32)
            bsh = sb("bsh", [128, 512], BF16)
            bsl = sb("bsl", [128, 512], BF16)
            e0 = sb("e0", [128, 128], BF16)
            us = sb("us", [128, 4, 512], BF16)
            tin = [sb(f"tin{i}", [128, 512], F32) for i in range(4)]
            tmpc = [sb(f"tmpc{i}", [128, 512], F32) for i in range(2)]
            mixed = sb("mixed", [128, 8, 512], BF16)
            sq = sb("sq", [128, 8, 512], BF16)
            sd = sb("sd", [128, 512], F32)
            rs = sb("rs", [128, 512], F32)
            h2 = sb("h2", [128, 8, 512], BF16)

            w_l = w_in[layer].rearrange("(k p) n -> p k n", p=128)
            self.load_w(wC[:, :, 1536:2048], w_l[:, :, 2048:2560], "wC3")
            self.load_w(wC[:, :, 1024:1536], w_l[:, :, 1536:2048], "wC2")
            for half in range(2):
                cb = SPLIT_V + 2048 + half * 512
                self.load_w(wC[:, :, half * 512:(half + 1) * 512], w_l[:, :, cb:cb + 512], f"wC{half}")
            WC = [f"wC{i}" for i in range(4)]
            swo = sgu_w_out[layer].rearrange("(k p) n -> p k n", p=128)
            wol = w_out[layer].rearrange("(k p) n -> p k n", p=128)
            for half in range(2):
                self.load_w(wso[:, :, half * 512:(half + 1) * 512], swo[:, :, half * 512:(half + 1) * 512], f"wso{half}")
            for half in range(2):
                self.load_w(wo[:, :, half * 512:(half + 1) * 512], wol[:, :, half * 512:(half + 1) * 512], f"wo{half}")
            WSO = ["wso0", "wso1"]
            WO = ["wo0", "wo1"]
            loads = [(slng[:], slng_d[:, layer * 512:(layer + 1) * 512]), (slnb[:], slnb_d[:, layer * 512:(layer + 1) * 512]),
                     (wsf[:], wsT_d[:, layer * 512:(layer + 1) * 512]), (trl[:], tril_d),
                     (bsf[:], bs_d[:, layer * 512:(layer + 1) * 512])]
            for j, (dst, src) in enumerate(loads):
                S.dma("sp", "cl", lambda e, dst=dst, src=src: e.dma_start(out=dst, in_=src), writes=[f"cl{j}"])
            CL = [f"cl{j}" for j in range(len(loads))]
            for hd in range(4):
                S.op("dve", lambda e, hd=hd: e.tensor_tensor(out=wsm[:, hd, :], in0=wsf[:, hd * 128:(hd + 1) * 128],
                                                            in1=trl[:], op=ALU.mult), reads=CL, writes=["wsm"])
            S.op("dve", lambda e: e.memset(bsh[:], 0.0), writes=["bsh"])
            S.op("dve", lambda e: e.memset(bsl[:], 0.0), writes=["bsl"])
            S.op("dve", lambda e: e.memset(e0[:], 0.0), writes=["e0"])
            S.op("dve", lambda e: e.memset(e0[0:1, :], 1.0), reads=["e0"], writes=["e0"])
            S.op("dve", lambda e: e.tensor_copy(out=bsh[0:1, :], in_=bsf[:]), reads=CL + ["bsh"], writes=["bsh"])
            S.op("dve", lambda e: e.tensor_tensor(out=bsr[:], in0=bsf[:], in1=bsh[0:1, :], op=ALU.subtract),
                 reads=CL + ["bsh"], writes=["bsr"])
            S.op("dve", lambda e: e.tensor_copy(out=bsl[0:1, :], in_=bsr[:]), reads=["bsr", "bsl"], writes=["bsl"])
            CL += ["wsm", "bsh", "bsl", "e0"]
            nt = len(tiles)

            def stage0(i):
                if i >= nt:
                    return
                col0, N = tiles[i]
                s = i % 2
                S.dma("sp", f"ch{s}", lambda e: e.dma_start(out=hb[s][:, :, :N],
                                                           in_=hT_s[:, col0:col0 + N].rearrange("(k p) n -> p k n", p=128)),
                      reads=[("hT", i)], writes=[f"hb{s}"])
                S.dma("sp", f"cx{s}", lambda e: e.dma_start(out=xt[s][:, :, :N],
                                                           in_=xsrc[:, col0:col0 + N].rearrange("(k p) n -> p k n", p=128)),
                      reads=[(xsrc_key, i)], writes=[f"xt{s}"])

            def front(i):
                if i >= nt:
                    return
                col0, N = tiles[i]
                s = i % 2
                hk = f"hb{s}"
                nsub = N // 128
                vps = []
                for sub in range(nsub):
                    ps, pk = self.PS()
                    vps.append((ps, pk))
                    self.mm_group(ps[:, :], [(hb[s][:, k, sub * 128:(sub + 1) * 128], wC[:, k, 1536:2048]) for k in range(KD)],
                                  reads=[hk] + WC, writes=[pk])
                    S.op("dve", lambda e, ps=ps, sub=sub: e.bn_stats(out=st6[:, sub, :], in_=ps[:, :]), reads=[pk], writes=[f"st6_{sub}"])
                    S.op("dve", lambda e, sub=sub: e.bn_aggr(out=mv[:, sub, :], in_=st6[:, sub, :]), reads=[f"st6_{sub}"], writes=[f"mv_{sub}"])
                MV = [f"mv_{sub}" for sub in range(nsub)]
                S.op("act", lambda e: e.activation(out=sdv[:, :nsub], in_=mv[:, :nsub, 1], func=AF.Sqrt, scale=1.0, bias=EPS),
                     reads=MV, writes=["sdv"])
                S.op("dve", lambda e: e.reciprocal(out=rv[:, :nsub], in_=sdv[:, :nsub]), reads=["sdv"], writes=["rv"])
                S.op("dve", lambda e: e.scalar_tensor_tensor(out=nb[:, :nsub], in0=mv[:, :nsub, 0], scalar=-1.0, in1=rv[:, :nsub],
                                                            op0=ALU.mult, op1=ALU.mult), reads=MV + ["rv"], writes=["nb"])
                for sub in range(nsub):
                    ps, pk = vps[sub]
                    vt = vtmp[sub]
                    vk = f"vtmp{sub}"
                    S.op("act", lambda e, ps=ps, vt=vt, sub=sub: e.activation(out=vt[:], in_=ps[:, :], func=AF.Identity,
                                                                            scale=rv[:, sub:sub + 1], bias=nb[:, sub:sub + 1]),
                         reads=[pk, "rv", "nb"], writes=[vk])
                    S.op("pool", lambda e, vt=vt: e.tensor_tensor(out=vt[:], in0=vt[:], in1=slng[:], op=ALU.mult),
                         reads=[vk] + CL, writes=[vk])
                    S.op("pool", lambda e, vt=vt, sub=sub: e.tensor_tensor(out=vn[:, sub, :], in0=vt[:], in1=slnb[:], op=ALU.add),
                         reads=[vk] + CL, writes=[f"vn{sub}"])
                for m in range(4):
                    ps, pk = self.PS()
                    cb = 1024 + m * 128
                    self.mm_group(ps[:, :N], [(wC[:, k, cb:cb + 128], hb[s][:, k, :N]) for k in range(KD)],
                                  reads=[hk] + WC, writes=[pk])
                    S.op("act", lambda e, ps=ps, m=m: e.activation(out=u[:, m, :N], in_=ps[:, :N], func=AF.Copy),
                         reads=[pk], writes=[f"u{m}"])
                for m in range(8):
                    ps, pk = self.PS()
                    self.mm_group(ps[:, :N], [(wC[:, k, m * 128:(m + 1) * 128], hb[s][:, k, :N]) for k in range(KD)],
                                  reads=[hk] + WC, writes=[pk])
                    S.op("act", lambda e, ps=ps, m=m: e.activation(out=sgC[s][:, m, :N], in_=ps[:, :N], func=AF.Sigmoid),
                         reads=[pk], writes=[f"sgC{s}_{m}"])

            def back1(i):
                col0, N = tiles[i]
                nsub = N // 128
                for hd in range(4):
                    ps, pk = self.PS()

                    def emit(e, ps=ps, hd=hd):
                        ins = None
                        for sub in range(nsub):
                            o = ps[:, sub * 128:(sub + 1) * 128]
                            e.matmul(o, lhsT=vn[:, sub, hd * 128:(hd + 1) * 128], rhs=wsm[:, hd, :], start=True, stop=False)
                            e.matmul(o, lhsT=e0[:], rhs=bsh[:, hd * 128:(hd + 1) * 128], start=False, stop=False)
                            ins = e.matmul(o, lhsT=e0[:], rhs=bsl[:, hd * 128:(hd + 1) * 128], start=False, stop=True)
                        return ins
                    S.op("pe", emit, reads=[f"vn{sub}" for sub in range(nsub)] + CL, writes=[pk])
                    S.op("dve", lambda e, ps=ps, hd=hd: e.tensor_tensor(out=us[:, hd, :N], in0=ps[:, :N], in1=u[:, hd, :N],
                                                                      op=ALU.mult),
                         reads=[pk, f"u{hd}"], writes=[f"us{hd}"])

            def back2(i):
                col0, N = tiles[i]
                s = i % 2
                for cc_ in range(8):
                    r = cc_ % 4
                    S.dma("sp", f"ct{r}", lambda e, cc_=cc_, r=r: e.dma_start(
                        out=tin[r][:, :N], in_=T_s[cc_ * 128:(cc_ + 1) * 128, col0:col0 + N]),
                        reads=[("T", i, cc_)], writes=[f"tin{r}"])
                    ps, pk = self.PS()
                    self.mm_group(ps[:, :N], [(wso[:, k, cc_ * 128:(cc_ + 1) * 128], us[:, k, :N]) for k in range(4)],
                                  reads=[f"us{k}" for k in range(4)] + WSO, writes=[pk])
                    tc_ = tmpc[cc_ % 2]
                    tck = f"tmpc{cc_ % 2}"
                    S.op("dve", lambda e, ps=ps, cc_=cc_, tc_=tc_: e.tensor_tensor(out=tc_[:, :N], in0=ps[:, :N],
                                                                                 in1=sgC[s][:, cc_, :N], op=ALU.mult),
                         reads=[pk, f"sgC{s}_{cc_}"], writes=[tck])
                    S.op("pool", lambda e, cc_=cc_, tc_=tc_, r=r: e.tensor_tensor(out=mixed[:, cc_, :N], in0=tc_[:, :N],
                                                                                in1=tin[r][:, :N], op=ALU.add),
                         reads=[tck, f"tin{r}"], writes=[f"mixed{cc_}"])
                for cc_ in range(8):
                    ps, pk = self.PS()
                    self.mm_group(ps[:, :N], [(wo[:, k, cc_ * 128:(cc_ + 1) * 128], mixed[:, k, :N]) for k in range(KD)],
                                  reads=[f"mixed{k}" for k in range(8)] + WO, writes=[pk])
                    S.op("dve", lambda e, ps=ps, cc_=cc_: e.tensor_tensor(out=xt[s][:, cc_, :N], in0=ps[:, :N],
                                                                        in1=xt[s][:, cc_, :N], op=ALU.add),
                         reads=[pk, f"xt{s}"], writes=[f"xt{s}"])
                S.dma("pool", f"cxo{s}", lambda e: e.dma_start(out=xm_s[:, col0:col0 + N].rearrange("(k p) n -> p k n", p=128),
                                                              in_=xt[s][:, :, :N]),
                      reads=[f"xt{s}"], writes=[("xm", layer, i)])

            def back4(i):
                col0, N = tiles[i]
                s = i % 2
                self.norm_fm(xt[s], f"xt{s}", N, c["gffn"][:, layer * 8:(layer + 1) * 8], sq, sd, rs, h2, "h2")
                S.dma("pool", "ch2", lambda e: e.dma_start(out=h2T_s[:, col0:col0 + N].rearrange("(k p) n -> p k n", p=128),
                                                          in_=h2[:, :, :N]),
                      reads=["h2"], writes=[("h2T", i)])

            first = 1 if layer == 1 else 0
            stage0(first)
            stage0(first + 1)
            front(first)
            back1(first)
            for i in range(first, nt):
                front(i + 1)
                back2(i)
                if i + 1 < nt:
                    back1(i + 1)
                back4(i)
                stage0(i + 2)

    def ffn_stream(self, pfx, sb, jobs, RM):
        S = self.S
        w1g = [sb(f"{pfx}w1g{i}", [128, 8, 512], BF16) for i in range(2)]
        w3g = [sb(f"{pfx}w3g{i}", [128, 8, 512], BF16) for i in range(2)]
        w2g = [sb(f"{pfx}w2g{i}", [128, 4, 1024], BF16) for i in range(2)]
        Hh = [sb(f"{pfx}Hh{i}", [128, 4, RM], BF16) for i in range(2)]
        sgt = [sb(f"{pfx}sgt{i}", [128, 512], F32) for i in range(2)]
        groups = []
        for ji, job in enumerate(jobs):
            for jg in range((job["nF"] + 3) // 4):
                groups.append((ji, jg))

        def load(gi):
            ji, jg = groups[gi]
            job = jobs[ji]
            b = gi % 2
            nj = min(4, job["nF"] - jg * 4)
            w1v = job["w1"].rearrange("(k p) n -> p k n", p=128)
            w3v = job["w3"].rearrange("(k p) n -> p k n", p=128)
            w2v = job["w2"].rearrange("(j p) n -> p j n", p=128)
            self.load_w(w1g[b][:, :, :nj * 128], w1v[:, :, jg * 512:jg * 512 + nj * 128], f"{pfx}w1g{b}")
            self.load_w(w3g[b][:, :, :nj * 128], w3v[:, :, jg * 512:jg * 512 + nj * 128], f"{pfx}w3g{b}")
            self.load_w(w2g[b][:, :nj, :], w2v[:, jg * 4:jg * 4 + nj, :], f"{pfx}w2g{b}")

        load(0)
        jobs[0]["pre"]()
        n_sg = 0
        for gi, (ji, jg) in enumerate(groups):
            job = jobs[ji]
            b = gi % 2
            nj = min(4, job["nF"] - jg * 4)
            if gi + 1 < len(groups):
                load(gi + 1)
                if groups[gi + 1][0] != ji and jobs[ji + 1].get("early_pre"):
                    jobs[ji + 1]["pre"]()
            if jg == 0 and ji > 0 and not job.get("early_pre"):
                job["pre"]()
            hs, hs_keys = job["hs"], job["hs_keys"]
            for (c0, n) in job["col_tiles"]:
                for jj in range(nj):
                    psG, pkG = self.PS()
                    self.mm_group(psG[:, :n], [(w1g[b][:, k, jj * 128:(jj + 1) * 128], hs[:, k, c0:c0 + n]) for k in range(KD)],
                                  reads=hs_keys + [f"{pfx}w1g{b}"], writes=[pkG])
                    psU, pkU = self.PS()
                    self.mm_group(psU[:, :n], [(w3g[b][:, k, jj * 128:(jj + 1) * 128], hs[:, k, c0:c0 + n]) for k in range(KD)],
                                  reads=hs_keys + [f"{pfx}w3g{b}"], writes=[pkU])
                    st = sgt[n_sg % 2]
                    sk = f"{pfx}sgt{n_sg % 2}"
                    n_sg += 1
                    S.op("act", lambda e, psG=psG, st=st, n=n: e.activation(out=st[:, :n], in_=psG[:, :n], func=AF.Silu),
                         reads=[pkG], writes=[sk])
                    S.op("dve", lambda e, psU=psU, st=st, n=n, jj=jj, c0=c0, b=b: e.tensor_tensor(
                        out=Hh[b][:, jj, c0:c0 + n], in0=psU[:, :n], in1=st[:, :n], op=ALU.mult),
                        reads=[pkU, sk], writes=[f"{pfx}Hh{b}"])
            job["emit_y"](jg, nj, Hh[b], [f"{pfx}Hh{b}"], w2g[b], [f"{pfx}w2g{b}"])
            if gi + 1 == len(groups) or groups[gi + 1][0] != ji:
                job["post"]()

    def dense_ffn(self, tiles, dw1, dw3, dw2, h2T_s, xm_s, xf_s, xg_s, ys_s):
        nc, S, c = self.nc, self.S, self.c
        supers = [[0, 1, 2], [3, 4, 5], [6, 7, 8]]
        with ExitStack() as es:
            def sb(name, shape, dt):
                return es.enter_context(nc.sbuf_tensor(f"f_{name}", list(shape), dt))
            RM = 1536
            hs = sb("hs", [128, 8, RM], BF16)
            ya = sb("ya", [128, 8, RM], F32)
            zrow = sb("zrow", [128, 4, 1024], BF16)
            S.op("pool", lambda e: e.memset(zrow[:], 0.0), writes=["zrow"])
            zrowf = sb("zrowf", [128, 1024], F32)
            S.op("pool", lambda e: e.memset(zrowf[:], 0.0), writes=["zrowf"])

            def init_xg():
                for j in range(NSLOT // 512):
                    S.dma("sp", f"xgi{j % 4}", lambda e, j=j: e.dma_start(
                        out=xg_s[j * 512:(j + 1) * 512, :].rearrange("(b p) n -> p b n", p=128), in_=zrow[:]),
                        reads=["zrow"], writes=[f"xgi{j % 4}"])
                S.dma("sp", "xgi0", lambda e: e.dma_start(out=ys_s[NSLOT:NSLOT + 128, :], in_=zrowf[:]),
                      reads=["zrowf"], writes=["ysz"])
            jobs = []
            for si, st_ in enumerate(supers):
                base = tiles[st_[0]][0]
                col_tiles = [(tiles[t][0] - base, tiles[t][1]) for t in st_]
                R = sum(n for _, n in col_tiles)

                def pre(base=base, R=R, st_=st_, si=si):
                    S.dma("sp", "fh", lambda e: e.dma_start(
                        out=hs[:, :, :R], in_=h2T_s[:, base:base + R].rearrange("(k p) n -> p k n", p=128)),
                        reads=[("h2T", t) for t in st_], writes=["f_hs"])
                    S.dma("sp", "fx", lambda e: e.dma_start(
                        out=ya[:, :, :R], in_=xm_s[:, base:base + R].rearrange("(k p) n -> p k n", p=128)),
                        reads=[("xm", 0, t) for t in st_], writes=["f_ya"])
                    if si == 1:
                        init_xg()

                def emit_y(jg, nj, Hh, hkeys, w2g, wkeys, col_tiles=col_tiles):
                    for (c0, n) in col_tiles:
                        for cc_ in range(8):
                            ps, pk = self.PS()
                            self.mm_group(ps[:, :n], [(w2g[:, jj, cc_ * 128:(cc_ + 1) * 128], Hh[:, jj, c0:c0 + n])
                                                      for jj in range(nj)], reads=hkeys + wkeys, writes=[pk])
                            S.op("dve", lambda e, ps=ps, cc_=cc_, c0=c0, n=n: e.tensor_tensor(
                                out=ya[:, cc_, c0:c0 + n], in0=ps[:, :n], in1=ya[:, cc_, c0:c0 + n], op=ALU.add),
                                reads=[pk, "f_ya"], writes=["f_ya"])

                def post(si=si, base=base, R=R, st_=st_):
                    if si == 0:
                        S.op("dve", lambda e: e.tensor_scalar(out=ya[:, :, 0:HALO], in0=ya[:, :, 0:HALO],
                                                             scalar1=c["hmask"][:, 0:1], scalar2=None, op0=ALU.mult),
                             reads=["f_ya"] + self.CST, writes=["f_ya"])
                    S.dma("sp", "fo", lambda e: e.dma_start(
                        out=xf_s[:, base:base + R].rearrange("(k p) n -> p k n", p=128), in_=ya[:, :, :R]),
                        reads=["f_ya"], writes=[("xf0", t) for t in st_])

                jobs.append(dict(hs=hs, hs_keys=["f_hs"], col_tiles=col_tiles, w1=dw1[0], w3=dw3[0], w2=dw2[0],
                                 nF=D_FF // 128, pre=pre, emit_y=emit_y, post=post))
            self.ffn_stream("f", sb, jobs, RM)

    def moe(self, ew1, ew3, ew2, wr_d, tri_d, eoff_d, fing_d, h2T_s, xm_s, xg_s, ys_s, cnt_s, out_d):
        nc, S, c = self.nc, self.S, self.c
        NT = T_OWN // 128
        NE = NT * 8
        with ExitStack() as gs2:
            def sbg(name, shape, dt):
                return gs2.enter_context(nc.sbuf_tensor(f"m_{name}", list(shape), dt))
            gw = sbg("gw", [128, NT, 2], F32)
            idx = sbg("idx", [128, NT, 2], I32)
            idxg = sbg("idxg", [128, NT, 2], I32)
            with ExitStack() as es:
                def sb(name, shape, dt):
                    return es.enter_context(nc.sbuf_tensor(f"r_{name}", list(shape), dt))
                wrf = sb("wrf", [128, 64], F32)
                wrb = sb("wrb", [128, 8, 8], BF16)
                trif = sb("trif", [128, 128], F32)
                trib = sb("trib", [128, 128], BF16)
                eoff = sb("eoff", [128, NE], F32)
                ht = sb("ht", [128, 8, T_OWN], BF16)
                lg = sb("lg", [128, NT, 8], F32)
                lg2 = sb("lg2", [128, NT, 8], F32)
                m1v = sb("m1v", [128, NT], F32)
                m2v = sb("m2v", [128, NT], F32)
                k1 = sb("k1", [128, NT, 8], F32)
                k2 = sb("k2", [128, NT, 8], F32)
                dd = sb("dd", [128, NT], F32)
                selb = sb("selb", [128, NE], BF16)
                csA = sb("csA", [128, NT, 8], F32)
                csB = sb("csB", [128, NT, 8], F32)
                cs0 = sb("cs0", [128, NT, 8], F32)
                pos = sb("pos", [128, NT, 8], F32)
                ovf = sb("ovf", [128, NT, 8], F32)
                prod = sb("prod", [128, NT, 8], F32)
                idf = sb("idf", [128, 2, NT], F32)
                hrow = [sb(f"hrow{i}", [128, 1024], BF16) for i in range(4)]
                XGI = [f"xgi{j}" for j in range(4)]
                for j, (dst, src) in enumerate([(wrf[:], wr_d), (trif[:], tri_d), (eoff[:], eoff_d)]):
                    S.dma("sp", "rl", lambda e, dst=dst, src=src: e.dma_start(out=dst, in_=src), writes=[f"rl{j}"])
                RL = ["rl0", "rl1", "rl2"]
                S.op("dve", lambda e: e.tensor_copy(out=wrb[:].rearrange("p k e -> p (k e)"), in_=wrf[:]), reads=RL, writes=["wrb"])
                S.op("dve", lambda e: e.tensor_copy(out=trib[:], in_=trif[:]), reads=RL, writes=["trib"])
                RL += ["wrb", "trib"]
                HT = []
                for j in range(8):
                    col0 = HALO + j * 512
                    S.dma("sp", "rh", lambda e, j=j, col0=col0: e.dma_start(
                        out=ht[:, :, j * 512:(j + 1) * 512], in_=h2T_s[:, col0:col0 + 512].rearrange("(k p) n -> p k n", p=128)),
                        reads=[("h2T", 1 + j)], writes=[f"rht{j}"])
                    HT.append(f"rht{j}")
                psL, pkL = self.PS()

                def emit_l(e):
                    ins = None
                    for i in range(NT):
                        for k in range(KD):
                            ins = e.matmul(psL[:, i * 8:(i + 1) * 8], lhsT=ht[:, k, i * 128:(i + 1) * 128], rhs=wrb[:, k, :],
                                           start=(k == 0), stop=(k == KD - 1))
                    return ins
                S.op("pe", emit_l, reads=HT + RL, writes=[pkL])
                lgf = lg[:].rearrange("p t e -> p (t e)")
                S.op("act", lambda e: e.activation(out=lgf, in_=psL[:, 0:NE], func=AF.Copy), reads=[pkL], writes=["lg"])
                S.op("dve", lambda e: e.tensor_reduce(out=m1v[:], in_=lg[:], axis=mybir.AxisListType.X, op=ALU.max),
                     reads=["lg"], writes=["m1v"])
                S.op("dve", lambda e: e.tensor_tensor(out=k1[:], in0=lg[:], in1=m1v[:].unsqueeze(2).broadcast_to([128, NT, 8]),
                                                     op=ALU.is_equal), reads=["lg", "m1v"], writes=["k1"])
                S.op("dve", lambda e: e.scalar_tensor_tensor(out=lg2[:].rearrange("p t e -> p (t e)"),
                                                            in0=k1[:].rearrange("p t e -> p (t e)"), scalar=-1.0e30,
                                                            in1=lgf, op0=ALU.mult, op1=ALU.add),
                     reads=["k1", "lg"], writes=["lg2"])
                S.op("dve", lambda e: e.tensor_reduce(out=m2v[:], in_=lg2[:], axis=mybir.AxisListType.X, op=ALU.max),
                     reads=["lg2"], writes=["m2v"])
                S.op("dve", lambda e: e.tensor_tensor(out=k2[:], in0=lg2[:], in1=m2v[:].unsqueeze(2).broadcast_to([128, NT, 8]),
                                                     op=ALU.is_equal), reads=["lg2", "m2v"], writes=["k2"])
                S.op("dve", lambda e: e.tensor_tensor(out=dd[:], in0=m1v[:], in1=m2v[:], op=ALU.subtract),
                     reads=["m1v", "m2v"], writes=["dd"])
                S.op("act", lambda e: e.activation(out=gw[:, :, 0], in_=dd[:], func=AF.Sigmoid), reads=["dd"], writes=["gw"])
                S.op("act", lambda e: e.activation(out=gw[:, :, 1], in_=dd[:], func=AF.Sigmoid, scale=-1.0),
                     reads=["dd"], writes=["gw"])
                S.op("dve", lambda e: e.tensor_tensor(out=selb[:], in0=k1[:].rearrange("p t e -> p (t e)"),
                                                     in1=k2[:].rearrange("p t e -> p (t e)"), op=ALU.add),
                     reads=["k1", "k2"], writes=["selb"])
                psP, pkP = self.PS()

                def emit_p(e):
                    e.matmul(psP[:, 0:NE], lhsT=trib[:], rhs=selb[:], start=True, stop=True)
                    return e.matmul(psP[:, NE:2 * NE], lhsT=c["ones_b"][:], rhs=selb[:], start=True, stop=True)
                S.op("pe", emit_p, reads=["selb", "ones_b"] + RL, writes=[pkP])
                S.op("act", lambda e: e.activation(out=cs0[:].rearrange("p t e -> p (t e)"), in_=psP[:, NE:2 * NE], func=AF.Copy),
                     reads=[pkP], writes=["cs0"])
                cur, ck = cs0, "cs0"
                bufs = [(csA, "csA"), (csB, "csB")]
                for si, sh in enumerate((1, 2, 4, 8, 16)):
                    dst, dk = bufs[si % 2]
                    S.op("dve", lambda e, cur=cur, dst=dst, sh=sh: e.tensor_tensor(out=dst[:, sh:, :], in0=cur[:, sh:, :],
                                                                                 in1=cur[:, :NT - sh, :], op=ALU.add),
                         reads=[ck], writes=[dk])
                    S.op("dve", lambda e, cur=cur, dst=dst, sh=sh: e.tensor_copy(out=dst[:, :sh, :], in_=cur[:, :sh, :]),
                         reads=[ck, dk], writes=[dk])
                    cur, ck = dst, dk
                S.dma("sp", "cnto", lambda e, cur=cur: e.dma_start(out=cnt_s, in_=cur[:, NT - 1, :]), reads=[ck], writes=["cnt_s"])
                S.op("dve", lambda e, cur=cur: e.tensor_tensor(out=pos[:], in0=cur[:], in1=cs0[:], op=ALU.subtract),
                     reads=[ck, "cs0"], writes=["pos"])
                S.op("dve", lambda e: e.tensor_tensor(out=pos[:].rearrange("p t e -> p (t e)"), in0=psP[:, 0:NE],
                                                     in1=pos[:].rearrange("p t e -> p (t e)"), op=ALU.add),
                     reads=[pkP, "pos"], writes=["pos"])
                S.op("dve", lambda e: e.tensor_scalar(out=ovf[:], in0=pos[:], scalar1=float(CAP), scalar2=BIGIDX,
                                                     op0=ALU.is_ge, op1=ALU.mult), reads=["pos"], writes=["ovf"])
                S.op("dve", lambda e: e.tensor_tensor(out=pos[:].rearrange("p t e -> p (t e)"),
                                                     in0=pos[:].rearrange("p t e -> p (t e)"), in1=eoff[:], op=ALU.add),
                     reads=["pos"] + RL, writes=["pos"])
                S.op("dve", lambda e: e.tensor_tensor(out=pos[:], in0=pos[:], in1=ovf[:], op=ALU.add),
                     reads=["pos", "ovf"], writes=["pos"])
                for kk, km in enumerate((k1, k2)):
                    S.op("dve", lambda e, km=km: e.tensor_tensor(out=prod[:], in0=km[:], in1=pos[:], op=ALU.mult),
                         reads=["k1", "k2", "pos"], writes=["prod"])
                    S.op("dve", lambda e, kk=kk: e.tensor_reduce(out=idf[:, kk, :], in_=prod[:], axis=mybir.AxisListType.X,
                                                                op=ALU.add), reads=["prod"], writes=["idf"])
                    S.op("dve", lambda e, kk=kk: e.tensor_copy(out=idx[:, :, kk], in_=idf[:, kk, :]), reads=["idf"], writes=["idx"])
                    S.op("dve", lambda e, kk=kk: e.tensor_scalar(out=idf[:, kk, :], in0=idf[:, kk, :], scalar1=float(NSLOT),
                                                                scalar2=None, op0=ALU.min), reads=["idf", "idx"], writes=["idf"])
                    S.op("dve", lambda e, kk=kk: e.tensor_copy(out=idxg[:, :, kk], in_=idf[:, kk, :]), reads=["idf"], writes=["idxg"])
                for i in range(NT):
                    s = i % 4
                    psT, pkT = self.PS()
                    psTb = psT.bitcast(BF16)

                    def emit_t(e, psTb=psTb, i=i):
                        ins = None
                        for k in range(KD):
                            ins = e.transpose(psTb[:, k * 128:(k + 1) * 128], ht[:, k, i * 128:(i + 1) * 128], c["ident_b"][:])
                        return ins
                    S.op("pe", emit_t, reads=HT + ["ident_b"], writes=[pkT])
                    S.op("act", lambda e, psTb=psTb, s=s: e.activation(out=hrow[s][:], in_=psTb[:, 0:1024], func=AF.Copy),
                         reads=[pkT], writes=[f"hrow{s}"])
                    for kk in range(2):
                        S.dma("pool", f"rs{kk}_{s}", lambda e, s=s, i=i, kk=kk: e.indirect_dma_start(
                            out=xg_s[:, :], out_offset=bass.IndirectOffsetOnAxis(ap=idx[:, i, kk:kk + 1], axis=0),
                            in_=hrow[s][:], in_offset=None, bounds_check=S.bnd, oob_is_err=False),
                            reads=[f"hrow{s}", "idx"] + XGI, writes=["xg"])
            S.barrier()

            with ExitStack() as es:
                def sb(name, shape, dt):
                    return es.enter_context(nc.sbuf_tensor(f"e_{name}", list(shape), dt))
                NB = CAP // 128
                xr = [sb(f"xr{i}", [128, 1024], BF16) for i in range(2)]
                xgT = [sb(f"xgT{i}", [128, 8, CAP], BF16) for i in range(2)]
                yacc = sb("yacc", [128, NB, 1024], F32)
                col_tiles = []
                c0 = 0
                while c0 < CAP:
                    n = min(512, CAP - c0)
                    col_tiles.append((c0, n))
                    c0 += n
                jobs = []
                for ex in range(NEXP):
                    xb = ex % 2

                    def pre(ex=ex, xb=xb):
                        for blk in range(NB):
                            s = blk % 2
                            r0 = ex * CAP + blk * 128
                            S.dma("sp", f"ex{s}", lambda e, s=s, r0=r0: e.dma_start(out=xr[s][:], in_=xg_s[r0:r0 + 128, :]),
                                  reads=["xg"], writes=[f"xr{s}"])
                            psT, pkT = self.PS()
                            psTb = psT.bitcast(BF16)

                            def emit_t(e, psTb=psTb, s=s):
                                ins = None
                                for k in range(KD):
                                    ins = e.transpose(psTb[:, k * 128:(k + 1) * 128], xr[s][:, k * 128:(k + 1) * 128],
                                                      c["ident_b"][:])
                                return ins
                            S.op("pe", emit_t, reads=[f"xr{s}", "ident_b"], writes=[pkT])
                            S.op("act", lambda e, psTb=psTb, blk=blk: e.activation(
                                out=xgT[xb][:, :, blk * 128:(blk + 1) * 128],
                                in_=psTb[:, 0:1024].rearrange("p (k n) -> p k n", k=8), func=AF.Copy),
                                reads=[pkT], writes=[f"xgT{xb}"])

                    def emit_y(jg, nj, Hh, hkeys, w2g, wkeys):
                        for blk in range(NB):
                            for half in range(2):
                                ps, pk = self.PS()
                                self.mm_group(ps[:, :], [(Hh[:, jj, blk * 128:(blk + 1) * 128], w2g[:, jj, half * 512:(half + 1) * 512])
                                                         for jj in range(nj)], reads=hkeys + wkeys, writes=[pk])
                                dst = yacc[:, blk, half * 512:(half + 1) * 512]
                                if jg == 0:
                                    S.op("act", lambda e, ps=ps, dst=dst: e.activation(out=dst, in_=ps[:, :], func=AF.Copy),
                                         reads=[pk], writes=["yacc"])
                                else:
                                    S.op("dve", lambda e, ps=ps, dst=dst: e.tensor_tensor(out=dst, in0=ps[:, :], in1=dst, op=ALU.add),
                                         reads=[pk, "yacc"], writes=["yacc"])

                    def post(ex=ex):
                        S.dma("sp", "ey", lambda e: e.dma_start(
                            out=ys_s[ex * CAP:(ex + 1) * CAP, :].rearrange("(b p) n -> p b n", p=128), in_=yacc[:]),
                            reads=["yacc"], writes=["ys"])

                    jobs.append(dict(hs=xgT[xb], hs_keys=[f"xgT{xb}"], col_tiles=col_tiles, w1=ew1[0, ex], w3=ew3[0, ex],
                                     w2=ew2[0, ex], nF=D_EXP // 128, pre=pre, emit_y=emit_y, post=post, early_pre=True))
                self.ffn_stream("x", sb, jobs, CAP)
            S.barrier()

            with ExitStack() as es:
                def sb(name, shape, dt):
                    return es.enter_context(nc.sbuf_tensor(f"o_{name}", list(shape), dt))
                fing = sb("fing", [128, 1024], F32)
                xt4 = [sb(f"xt4_{i}", [128, 8, 512], F32) for i in range(2)]
                y1 = [sb(f"y1{i}", [128, 1024], F32) for i in range(4)]
                y2 = [sb(f"y2{i}", [128, 1024], F32) for i in range(4)]
                acc = [sb(f"acc{i}", [128, 1024], F32) for i in range(4)]
                jk = sb("jk", [128, 1024], F32)
                ssq = [sb(f"ssq{i}", [128, 1], F32) for i in range(2)]
                sdo = [sb(f"sdo{i}", [128, 1], F32) for i in range(2)]
                ro = [sb(f"ro{i}", [128, 1], F32) for i in range(2)]
                S.dma("sp", "ol", lambda e: e.dma_start(out=fing[:], in_=fing_d), writes=["fing"])

                def part1(i):
                    s = i % 4
                    ti = 1 + i // 4
                    xb = (i // 4) % 2
                    xo = (i % 4) * 128
                    if i % 4 == 0:
                        col0 = HALO + i * 128
                        S.dma("sp", f"ox{xb}", lambda e, xb=xb, col0=col0: e.dma_start(
                            out=xt4[xb][:], in_=xm_s[:, col0:col0 + 512].rearrange("(k p) n -> p k n", p=128)),
                            reads=[("xm", 1, ti)], writes=[f"oxt{xb}"])
                    for kk, yb in enumerate((y1, y2)):
                        S.dma("pool", f"og{kk}{s}", lambda e, yb=yb, s=s, i=i, kk=kk: e.indirect_dma_start(
                            out=yb[s][:], out_offset=None, in_=ys_s[:, :],
                            in_offset=bass.IndirectOffsetOnAxis(ap=idxg[:, i, kk:kk + 1], axis=0),
                            bounds_check=S.bnd2, oob_is_err=False),
                            reads=["ys", "idxg"], writes=[f"oy{kk}{s}"])
                    for half in range(2):
                        ps, pk = self.PS()

                        def emit_t(e, ps=ps, xb=xb, xo=xo, half=half):
                            ins = None
                            for k in range(4):
                                kk_ = half * 4 + k
                                ins = e.transpose(ps[:, k * 128:(k + 1) * 128], xt4[xb][:, kk_, xo:xo + 128], c["ident_f"][:])
                            return ins
                        S.op("pe", emit_t, reads=[f"oxt{xb}"] + self.CST, writes=[pk])
                        sl = slice(half * 512, (half + 1) * 512)
                        S.op("dve", lambda e, ps=ps, s=s, sl=sl, i=i: e.scalar_tensor_tensor(
                            out=acc[s][:, sl], in0=y1[s][:, sl], scalar=gw[:, i, 0:1], in1=ps[:, :], op0=ALU.mult, op1=ALU.add),
                            reads=[pk, f"oy0{s}", "gw"], writes=[f"oacc{s}"])
                        S.op("dve", lambda e, s=s, sl=sl, i=i: e.scalar_tensor_tensor(
                            out=acc[s][:, sl], in0=y2[s][:, sl], scalar=gw[:, i, 1:2], in1=acc[s][:, sl], op0=ALU.mult, op1=ALU.add),
                            reads=[f"oy1{s}", "gw", f"oacc{s}"], writes=[f"oacc{s}"])
                    r = i % 2
                    S.op("act", lambda e, s=s, r=r: e.activation(out=jk[:], in_=acc[s][:], func=AF.Square, accum_out=ssq[r][:]),
                         reads=[f"oacc{s}"], writes=["jk", f"ssq{r}"])
                    S.op("act", lambda e, r=r: e.activation(out=sdo[r][:], in_=ssq[r][:], func=AF.Sqrt, scale=1.0 / D, bias=EPS),
                         reads=[f"ssq{r}"], writes=[f"sdo{r}"])

                def part2(i):
                    s = i % 4
                    r = i % 2
                    S.op("dve", lambda e, r=r: e.reciprocal(out=ro[r][:], in_=sdo[r][:]), reads=[f"sdo{r}"], writes=[f"ro{r}"])
                    S.op("dve", lambda e, s=s, r=r: e.scalar_tensor_tensor(out=acc[s][:], in0=acc[s][:], scalar=ro[r][:, 0:1],
                                                                          in1=fing[:], op0=ALU.mult, op1=ALU.mult),
                         reads=[f"oacc{s}", f"ro{r}", "fing"], writes=[f"oacc{s}"])
                    S.dma("act", f"oo{s}", lambda e, s=s, i=i: e.dma_start(out=out_d[i * 128:(i + 1) * 128, :], in_=acc[s][:]),
                          reads=[f"oacc{s}"], writes=[("out", i)])

                part1(0)
                for i in range(NT):
                    if i + 1 < NT:
                        part1(i + 1)
                    part2(i)


def host_inputs(inputs):
    f = np.float32
    x = np.asarray(inputs["x"], dtype=f)

    def pc(v, nchunk):
        v = np.asarray(v, dtype=f)
        L = v.shape[0]
        return np.ascontiguousarray(v.reshape(L, nchunk, 128).transpose(2, 0, 1).reshape(128, L * nchunk))

    shared = {}
    shared["ident"] = np.eye(128, dtype=f)
    jj, ii = np.meshgrid(np.arange(128), np.arange(128), indexing="ij")
    shared["tril"] = (jj <= ii).astype(f)
    shared["tri"] = (jj < ii).astype(f)
    shared["eoff"] = np.ascontiguousarray(np.broadcast_to(np.tile((np.arange(8) * CAP).astype(f), 32)[None, :], (128, 256)))
    shared["gmix"] = pc(inputs["mix_norm_g"], 8)
    shared["gffn"] = pc(inputs["ffn_norm_g"], 8)
    cw = np.asarray(inputs["conv_w"], dtype=f)
    shared["convw"] = np.ascontiguousarray(cw.reshape(2, 31, 4, 128).transpose(3, 0, 2, 1).reshape(128, 248))
    shared["convb"] = pc(inputs["conv_b"], 4)
    shared["clng"] = pc(inputs["conv_ln_g"], 4)
    shared["clnb"] = pc(inputs["conv_ln_b"], 4)
    shared["pscale"] = pc(inputs["pool_scale"], 8)
    shared["slng"] = np.ascontiguousarray(np.broadcast_to(np.asarray(inputs["sgu_ln_g"], dtype=f).reshape(1, 1024), (128, 1024)))
    shared["slnb"] = np.ascontiguousarray(np.broadcast_to(np.asarray(inputs["sgu_ln_b"], dtype=f).reshape(1, 1024), (128, 1024)))
    ws = np.asarray(inputs["sgu_w_s"], dtype=f)
    shared["wsT"] = np.ascontiguousarray(ws.transpose(3, 0, 1, 2).reshape(128, 1024))
    shared["bs"] = np.ascontiguousarray(np.asarray(inputs["sgu_b_s"], dtype=f).reshape(1, 1024))
    shared["fing"] = np.ascontiguousarray(np.broadcast_to(np.asarray(inputs["final_norm_g"], dtype=f).reshape(1, 1024), (128, 1024)))
    wr = np.asarray(inputs["router_w"], dtype=f)[0]
    shared["wr"] = np.ascontiguousarray(wr.reshape(8, 128, 8).transpose(1, 0, 2).reshape(128, 64))
    for k in ("w_in", "conv_w_out", "pool_w", "sgu_w_out", "w_out", "dense_w1", "dense_w3", "dense_w2",
              "expert_w1", "expert_w3", "expert_w2"):
        shared[k] = np.ascontiguousarray(np.asarray(inputs[k], dtype=f))
    in_maps = []
    for core in range(NCORES):
        b, half = core // 2, core % 2
        t0 = half * T_OWN
        xs = np.zeros((TT, D), dtype=f)
        xs[HALO:] = x[b, t0:t0 + T_OWN]
        if half == 1:
            xs[:HALO] = x[b, t0 - HALO:t0]
        m = dict(shared)
        m["xT"] = np.ascontiguousarray(xs.T)
        m["hmask"] = np.full((128, 1), float(half), dtype=f)
        rc = np.zeros((4, 16), dtype=f)
        for g in range(4):
            w = 2 << g
            for t in range(16):
                rc[g, t] = 1.0 / (min(t + 1, w) if half == 0 else w)
        m["rc"] = np.ascontiguousarray(np.broadcast_to(rc.reshape(1, 64), (128, 64)))
        in_maps.append(m)
    return in_maps


_NC_CACHE = {}


def kernel(**inputs):
    in_maps = host_inputs(inputs)
    if "nc" not in _NC_CACHE:
        _NC_CACHE["nc"] = Builder().build()
    nc = _NC_CACHE["nc"]
    res = run_bass_kernel_spmd(nc, in_maps, core_ids=list(range(NCORES)))
    out = np.empty((4, 8192, D), dtype=np.float32)
    for core in range(NCORES):
        b, half = core // 2, core % 2
        out[b, half * T_OWN:(half + 1) * T_OWN] = res.results[core]["out"]
    return out
```

```python
from contextlib import ExitStack

import numpy as np
import concourse.bass as bass
import concourse.mybir as mybir
from concourse.bass_utils import run_bass_kernel_spmd

F32 = mybir.dt.float32
BF16 = mybir.dt.bfloat16
I32 = mybir.dt.int32
AF = mybir.ActivationFunctionType
ALU = mybir.AluOpType

NCORES = 8
D = 1024
KD = 8
T_OWN = 4096
HALO = 128
TT = T_OWN + HALO
D_IN = 5632
SPLIT_V = 2560
D_FF = 2816
D_EXP = 3584
NEXP = 8
CAP = 1536
NSLOT = NEXP * CAP
EPS = 1e-6
BIGIDX = float(1 << 20)

COMPUTE = ("pe", "act", "dve", "pool")
ALL_ENG = ("pe", "act", "dve", "pool", "sp")


class Sched:
    def __init__(self, nc):
        self.nc = nc
        self.streams = {e: [] for e in ALL_ENG}
        self.sem = {}
        self.cnt = {}
        for e in COMPUTE:
            self.sem[e] = nc.alloc_semaphore("prog_" + e)
            self.cnt[e] = 0
        self.known = {e: {} for e in ALL_ENG}
        self.last_write = {}
        self.readers = {}
        self.lanes = {}
        self.free_sems = {"pool": [], "sp": []}
        self.lane_q = {}
        self.nsem = 0

    def lane(self, name, queue="sp"):
        queue = "pool" if queue == "pool" else "sp"
        if name not in self.lanes:
            if self.free_sems[queue]:
                self.lanes[name] = self.free_sems[queue].pop()
            else:
                self.nsem += 1
                self.lanes[name] = [self.nc.alloc_semaphore(f"ln{self.nsem}"), 0]
            self.lane_q[name] = queue
        assert self.lane_q[name] == queue, (name, queue)
        return self.lanes[name]

    def _deps(self, reads, writes):
        deps = {}

        def add(d):
            if d is None:
                return
            k, v = d
            if deps.get(k, 0) < v:
                deps[k] = v

        for k in reads:
            add(self.last_write.get(k))
        for k in writes:
            add(self.last_write.get(k))
            for rk, rv in self.readers.get(k, {}).items():
                add((rk, rv))
        return deps

    def _emit_waits(self, eng, deps):
        st = self.streams[eng]
        kn = self.known[eng]
        for k, v in deps.items():
            if kn.get(k, 0) >= v:
                continue
            kn[k] = v
            st.append(("wait", self.semof(k), v))

    def _record(self, key, reads, writes):
        sk, sv = key
        for k in reads:
            r = self.readers.setdefault(k, {})
            if r.get(sk, 0) < sv:
                r[sk] = sv
        for k in writes:
            self.last_write[k] = key
            self.readers[k] = {}

    def op(self, eng, emit, reads=(), writes=()):
        deps = self._deps(reads, writes)
        sk = ("c", eng)
        self._emit_waits(eng, deps)
        self.cnt[eng] += 1
        self.streams[eng].append(("op", emit, self.semof(sk)))
        self._record((sk, self.cnt[eng]), reads, writes)

    def dma(self, queue, lane, emit, reads=(), writes=()):
        ln = self.lane(lane, queue)
        sk = ("l", lane)
        deps = self._deps(reads, writes)
        if ln[1] > 0 and deps.get(sk, 0) < ln[1]:
            deps[sk] = ln[1]
        self._emit_waits(queue, deps)
        ln[1] += 16
        self.streams[queue].append(("dma", emit, ln[0]))
        self._record((sk, ln[1]), reads, writes)

    def barrier(self):
        deps = {("c", e): self.cnt[e] for e in COMPUTE if self.cnt[e] > 0}
        for name, ln in self.lanes.items():
            if ln[1] > 0:
                deps[("l", name)] = ln[1]
        for eng in ALL_ENG:
            self._emit_waits(eng, dict(deps))
        for name, ln in self.lanes.items():
            self.free_sems[self.lane_q[name]].append(ln)
        self.lanes = {}
        self.lane_q = {}
        for eng in ALL_ENG:
            self.known[eng] = {k: v for k, v in self.known[eng].items() if k[0] == "c"}
        self.last_write = {k: v for k, v in self.last_write.items() if v[0][0] == "c"}
        for k in list(self.readers.keys()):
            self.readers[k] = {rk: rv for rk, rv in self.readers[k].items() if rk[0] == "c"}

    def final_wait(self, eng, keys):
        deps = {}
        for k in keys:
            d = self.last_write.get(k)
            if d is not None and deps.get(d[0], 0) < d[1]:
                deps[d[0]] = d[1]
        self._emit_waits(eng, deps)

    def semof(self, k):
        if k[0] == "c":
            return self.sem[k[1]]
        return self.lanes[k[1]][0]

    def emit(self):
        nc = self.nc
        with nc.Block() as block:
            def run(e, stream):
                for item in stream:
                    if item[0] == "wait":
                        e.wait_ge(item[1], item[2])
                    elif item[0] == "op":
                        item[1](e).then_inc(item[2], 1)
                    else:
                        item[1](e).then_inc(item[2], 16)

            @block.tensor
            def _(e):
                run(e, self.streams["pe"])

            @block.scalar
            def _(e):
                run(e, self.streams["act"])

            @block.vector
            def _(e):
                run(e, self.streams["dve"])

            @block.gpsimd
            def _(e):
                with e.register("bnd") as reg, e.register("bnd2") as reg2:
                    e.reg_mov(reg, NSLOT - 1)
                    e.reg_mov(reg2, NSLOT + 127)
                    self.bnd = reg
                    self.bnd2 = reg2
                    run(e, self.streams["pool"])

            @block.sync
            def _(e):
                run(e, self.streams["sp"])


def tiles_of():
    res = [(0, HALO)]
    for i in range(T_OWN // 512):
        res.append((HALO + i * 512, 512))
    return res


class Builder:
    def __init__(self, phases=99, debug=False):
        self.phases = phases
        self.debug = debug
        nc = bass.Bass("TRN2", target_bir_lowering=False)
        self.nc = nc
        self.S = Sched(nc)
        self.psn = 0
        self.ps = [nc.alloc_psum_tensor(f"psb{i}", [128, 512], F32) for i in range(8)]
        self.dram_in = {}
        self.uid = 0

    def din(self, name, shape, dt=F32):
        t = self.nc.dram_tensor(name, list(shape), dt, kind="ExternalInput").ap()
        self.dram_in[name] = t
        return t

    def dscr(self, name, shape, dt):
        kind = "ExternalOutput" if self.debug else "Internal"
        if self.debug:
            return self.nc.dram_tensor(name, list(shape), dt, kind="ExternalOutput").ap()
        return self.nc.dram_tensor(name, list(shape), dt).ap()

    def PS(self):
        i = self.psn % 8
        self.psn += 1
        return self.ps[i], f"ps{i}"

    def lname(self, base):
        self.uid += 1
        return f"{base}"

    def mm_group(self, out_ap, pairs, reads, writes):
        n = len(pairs)

        def emit(e, pairs=pairs, out_ap=out_ap, n=n):
            ins = None
            for j, (l, r) in enumerate(pairs):
                ins = e.matmul(out_ap, lhsT=l, rhs=r, start=(j == 0), stop=(j == n - 1))
            return ins

        self.S.op("pe", emit, reads=reads, writes=writes)

    def build(self):
        nc, S = self.nc, self.S
        din = self.din
        xT = din("xT", [D, TT])
        hmask = din("hmask", [128, 1])
        rc_d = din("rc", [128, 64])
        ident_d = din("ident", [128, 128])
        tril_d = din("tril", [128, 128])
        tri_d = din("tri", [128, 128])
        eoff_d = din("eoff", [128, 256])
        gmix_d = din("gmix", [128, 16])
        gffn_d = din("gffn", [128, 16])
        convw_d = din("convw", [128, 2 * 4 * 31])
        convb_d = din("convb", [128, 8])
        clng_d = din("clng", [128, 8])
        clnb_d = din("clnb", [128, 8])
        pscale_d = din("pscale", [128, 16])
        slng_d = din("slng", [128, 1024])
        slnb_d = din("slnb", [128, 1024])
        wsT_d = din("wsT", [128, 2 * 4 * 128])
        bs_d = din("bs", [1, 2 * 4 * 128])
        fing_d = din("fing", [128, 1024])
        wr_d = din("wr", [128, 64])
        w_in = din("w_in", [2, D, D_IN])
        conv_w_out = din("conv_w_out", [2, 512, D])
        pool_w = din("pool_w", [2, 4, 128, 256])
        sgu_w_out = din("sgu_w_out", [2, 512, D])
        w_out = din("w_out", [2, D, D])
        dense_w1 = din("dense_w1", [1, D, D_FF])
        dense_w3 = din("dense_w3", [1, D, D_FF])
        dense_w2 = din("dense_w2", [1, D_FF, D])
        expert_w1 = din("expert_w1", [1, NEXP, D, D_EXP])
        expert_w3 = din("expert_w3", [1, NEXP, D, D_EXP])
        expert_w2 = din("expert_w2", [1, NEXP, D_EXP, D])
        out_d = nc.dram_tensor("out", [T_OWN, D], F32, kind="ExternalOutput").ap()
        hT_s = self.dscr("hT_s", [D, TT], BF16)
        T_s = self.dscr("T_s", [D, TT], F32)
        xm_s = [self.dscr("xm0_s", [D, TT], F32), self.dscr("xm1_s", [D, TT], F32)]
        h2T_s = self.dscr("h2T_s", [D, TT], BF16)
        xf0_s = self.dscr("xf0_s", [D, TT], F32)
        xg_s = self.dscr("xg_s", [NSLOT, D], BF16)
        ys_s = self.dscr("ys_s", [NSLOT + 128, D], F32)
        cnt_s = self.dscr("cnt_s", [128, 8], F32)

        with ExitStack() as gs:
            def sb(name, shape, dt, es=gs):
                return es.enter_context(nc.sbuf_tensor("g_" + name, list(shape), dt))

            ident_f = sb("ident_f", [128, 128], F32)
            ident_b = sb("ident_b", [128, 128], BF16)
            ones_b = sb("ones_b", [128, 128], BF16)
            hmask_t = sb("hmask_t", [128, 1], F32)
            rc_t = sb("rc_t", [128, 64], F32)
            gmix = sb("gmix", [128, 16], F32)
            gffn = sb("gffn", [128, 16], F32)
            convw = sb("convw", [128, 248], F32)
            convb = sb("convb", [128, 8], F32)
            clng = sb("clng", [128, 8], F32)
            clnb = sb("clnb", [128, 8], F32)
            pscale = sb("pscale", [128, 16], F32)
            smalls = [(ident_f, ident_d), (hmask_t, hmask), (rc_t, rc_d), (gmix, gmix_d), (gffn, gffn_d),
                      (convw, convw_d), (convb, convb_d), (clng, clng_d), (clnb, clnb_d), (pscale, pscale_d)]
            for j, (t, d_) in enumerate(smalls):
                S.dma("sp", "cst", lambda e, t=t, d_=d_: e.dma_start(out=t[:], in_=d_), writes=[f"cst{j}"])
            CST = [f"cst{j}" for j in range(len(smalls))]
            S.op("dve", lambda e: e.tensor_copy(out=ident_b[:], in_=ident_f[:]), reads=CST, writes=["ident_b"])
            S.op("dve", lambda e: e.memset(ones_b[:], 1.0), writes=["ones_b"])
            CST += ["ident_b", "ones_b"]
            self.CST = CST
            self.c = dict(ident_f=ident_f, ident_b=ident_b, ones_b=ones_b, hmask=hmask_t, rc=rc_t, gmix=gmix,
                          gffn=gffn, convw=convw, convb=convb, clng=clng, clnb=clnb, pscale=pscale)

            tiles = tiles_of()
            ph = 0
            for layer in range(2):
                xsrc = xT if layer == 0 else xf0_s
                xsrc_key = "xT" if layer == 0 else "xf0"
                ph += 1
                if self.phases >= ph:
                    self.pass_a(layer, tiles, xsrc, xsrc_key, w_in, conv_w_out, hT_s, T_s)
                    S.barrier()
                ph += 1
                if self.phases >= ph:
                    self.pass_b(layer, tiles, w_in, pool_w, hT_s, T_s)
                    S.barrier()
                ph += 1
                if self.phases >= ph:
                    self.pass_c(layer, tiles, xsrc, xsrc_key, w_in, sgu_w_out, w_out, slng_d, slnb_d, wsT_d, bs_d,
                                tril_d, hT_s, T_s, xm_s[layer], h2T_s)
                    S.barrier()
                ph += 1
                if self.phases >= ph:
                    if layer == 0:
                        self.dense_ffn(tiles, dense_w1, dense_w3, dense_w2, h2T_s, xm_s[0], xf0_s, xg_s, ys_s)
                        S.barrier()
                    else:
                        self.moe(expert_w1, expert_w3, expert_w2, wr_d, tri_d, eoff_d, fing_d, h2T_s, xm_s[1],
                                 xg_s, ys_s, cnt_s, out_d)
            S.final_wait("sp", list(S.last_write.keys()))
        S.emit()
        return nc

    def norm_fm(self, xt, xkey, N, gcol, sq, sd, rs, h, hkey):
        S = self.S
        ones_b = self.c["ones_b"]
        for c in range(KD):
            S.op("act", lambda e, c=c: e.activation(out=sq[:, c, :N], in_=xt[:, c, :N], func=AF.Square),
                 reads=[xkey], writes=[f"sq{c}"])
        ps, pk = self.PS()
        self.mm_group(ps[:, :N], [(ones_b[:], sq[:, c, :N]) for c in range(KD)],
                      reads=[f"sq{c}" for c in range(KD)] + ["ones_b"], writes=[pk])
        S.op("act", lambda e: e.activation(out=sd[:, :N], in_=ps[:, :N], func=AF.Sqrt, scale=1.0 / D, bias=EPS),
             reads=[pk], writes=["sd"])
        S.op("dve", lambda e: e.reciprocal(out=rs[:, :N], in_=sd[:, :N]), reads=["sd"], writes=["rs"])
        for c in range(KD):
            S.op("dve", lambda e, c=c: e.scalar_tensor_tensor(out=h[:, c, :N], in0=xt[:, c, :N], scalar=gcol[:, c:c + 1],
                                                            in1=rs[:, :N], op0=ALU.mult, op1=ALU.mult),
                 reads=[xkey, "rs"] + self.CST, writes=[hkey])

    def load_w(self, dst, src, key):
        self.S.dma("pool", "w_" + key, lambda e: e.dma_start(out=dst, in_=src), writes=[key])

    def pass_a(self, layer, tiles, xsrc, xsrc_key, w_in, conv_w_out, hT_s, T_s):
        nc, S, c = self.nc, self.S, self.c
        with ExitStack() as es:
            def sb(name, shape, dt):
                return es.enter_context(nc.sbuf_tensor(f"a{layer}_{name}", list(shape), dt))
            xt = [sb(f"xt{i}", [128, 8, 512], F32) for i in range(2)]
            sq = sb("sq", [128, 8, 512], BF16)
            sd = sb("sd", [128, 512], F32)
            rs = sb("rs", [128, 512], F32)
            h = [sb(f"h{i}", [128, 8, 512], BF16) for i in range(2)]
            wA = sb("wA", [128, 8, 2048], BF16)
            wco = sb("wco", [128, 4, 1024], BF16)
            diag = sb("diag", [128, 124, 128], BF16)
            sgA = [sb(f"sgA{i}", [128, 8, 512], BF16) for i in range(2)]
            sg4 = sb("sg4", [128, 4, 512], BF16)
            abuf = [sb(f"abuf{i}", [128, 4, 30 + 512], BF16) for i in range(2)]
            cc = sb("cc", [128, 4, 512], F32)
            csq = sb("csq", [128, 4, 512], BF16)
            cbf = sb("cbf", [128, 4, 512], BF16)
            msq = sb("msq", [128, 512], F32)
            var = sb("var", [128, 512], F32)
            sd2 = sb("sd2", [128, 512], F32)
            rs2 = sb("rs2", [128, 512], F32)
            ctmp = [sb(f"ctmp{i}", [128, 512], F32) for i in range(2)]
            cact = sb("cact", [128, 4, 512], BF16)
            tout = [sb(f"tout{i}", [128, 512], F32) for i in range(4)]

            w_l = w_in[layer].rearrange("(k p) n -> p k n", p=128)
            for half in range(2):
                self.load_w(wA[:, :, 1024 + half * 512:1024 + (half + 1) * 512],
                            w_l[:, :, half * 512:(half + 1) * 512], f"wA{2 + half}")
            for half in range(2):
                self.load_w(wA[:, :, half * 512:(half + 1) * 512],
                            w_l[:, :, SPLIT_V + half * 512:SPLIT_V + (half + 1) * 512], f"wA{half}")
            WA = [f"wA{i}" for i in range(4)]
            cwo = conv_w_out[layer].rearrange("(k p) n -> p k n", p=128)
            for half in range(2):
                self.load_w(wco[:, :, half * 512:(half + 1) * 512], cwo[:, :, half * 512:(half + 1) * 512], f"wco{half}")
            WCO = ["wco0", "wco1"]
            for j in range(124):
                col = layer * 124 + j
                if j % 3 != 2:
                    S.op("dve", lambda e, j=j, col=col: e.tensor_scalar(out=diag[:, j, :], in0=c["ident_f"][:],
                                                                       scalar1=c["convw"][:, col:col + 1], scalar2=None,
                                                                       op0=ALU.mult),
                         reads=self.CST, writes=[f"diag_{j}"])
                else:
                    S.op("act", lambda e, j=j, col=col: e.activation(out=diag[:, j, :], in_=c["ident_f"][:], func=AF.Copy,
                                                                    scale=c["convw"][:, col:col + 1]),
                         reads=self.CST, writes=[f"diag_{j}"])
            S.op("pool", lambda e: e.memset(abuf[0][:, :, 0:30], 0.0), writes=[f"abuf0_{m}" for m in range(4)])
            nt = len(tiles)

            def stage0(i):
                if i >= nt:
                    return
                col0, N = tiles[i]
                s = i % 2
                S.dma("sp", f"ax{s}", lambda e: e.dma_start(out=xt[s][:, :, :N],
                                                           in_=xsrc[:, col0:col0 + N].rearrange("(k p) n -> p k n", p=128)),
                      reads=[(xsrc_key, i)], writes=[f"xt{s}"])
                self.norm_fm(xt[s], f"xt{s}", N, c["gmix"][:, layer * 8:(layer + 1) * 8], sq, sd, rs, h[s], f"h{s}")
                S.dma("pool", f"ah{s}", lambda e: e.dma_start(out=hT_s[:, col0:col0 + N].rearrange("(k p) n -> p k n", p=128),
                                                             in_=h[s][:, :, :N]),
                      reads=[f"h{s}"], writes=[("hT", i)])

            def is_full(i):
                return not (layer == 1 and i == 0)

            def front(i):
                if i >= nt:
                    return
                col0, N = tiles[i]
                s = i % 2
                hk = f"h{s}"
                ab = abuf[s]
                for m in range(4):
                    ps, pk = self.PS()
                    cb = 1024 + 512 + m * 128
                    self.mm_group(ps[:, :N], [(wA[:, k, cb:cb + 128], h[s][:, k, :N]) for k in range(KD)],
                                  reads=[hk] + WA, writes=[pk])
                    S.op("act", lambda e, ps=ps, m=m: e.activation(out=sg4[:, m, :N], in_=ps[:, :N], func=AF.Sigmoid),
                         reads=[pk], writes=[f"sg4{m}"])
                for m in range(4):
                    ps, pk = self.PS()
                    cb = 1024 + m * 128
                    self.mm_group(ps[:, :N], [(wA[:, k, cb:cb + 128], h[s][:, k, :N]) for k in range(KD)],
                                  reads=[hk] + WA, writes=[pk])
                    S.op("dve", lambda e, ps=ps, m=m: e.tensor_tensor(out=ab[:, m, 30:30 + N], in0=ps[:, :N],
                                                                    in1=sg4[:, m, :N], op=ALU.mult),
                         reads=[pk, f"sg4{m}"], writes=[f"abuf{s}_{m}"])
                    S.op("pool", lambda e, m=m: e.tensor_copy(out=abuf[1 - s][:, m, 0:30], in_=ab[:, m, N:N + 30]),
                         reads=[f"abuf{s}_{m}"], writes=[f"abuf{1 - s}_{m}"])
                if is_full(i):
                    for m in range(8):
                        ps, pk = self.PS()
                        self.mm_group(ps[:, :N], [(wA[:, k, m * 128:(m + 1) * 128], h[s][:, k, :N]) for k in range(KD)],
                                      reads=[hk] + WA, writes=[pk])
                        S.op("act", lambda e, ps=ps, m=m: e.activation(out=sgA[s][:, m, :N], in_=ps[:, :N], func=AF.Sigmoid),
                             reads=[pk], writes=[f"sgA{s}_{m}"])

            def back1(i):
                if not is_full(i):
                    return
                col0, N = tiles[i]
                s = i % 2
                ab = abuf[s]
                for m in range(4):
                    ps, pk = self.PS()
                    self.mm_group(ps[:, :N], [(diag[:, m * 31 + k, :], ab[:, m, k:k + N]) for k in range(31)],
                                  reads=[f"abuf{s}_{m}"] + [f"diag_{m * 31 + k}" for k in range(31)], writes=[pk])
                    S.op("act", lambda e, ps=ps, m=m: e.activation(out=cc[:, m, :N], in_=ps[:, :N], func=AF.Identity,
                                                                 bias=c["convb"][:, layer * 4 + m:layer * 4 + m + 1],
                                                                 scale=1.0),
                         reads=[pk] + self.CST, writes=[f"cc{m}"])
                    S.op("act", lambda e, ps=ps, m=m: e.activation(out=csq[:, m, :N], in_=ps[:, :N], func=AF.Square,
                                                                 bias=c["convb"][:, layer * 4 + m:layer * 4 + m + 1],
                                                                 scale=1.0),
                         reads=[pk] + self.CST, writes=[f"csq{m}"])
                    S.op("act", lambda e, ps=ps, m=m: e.activation(out=cbf[:, m, :N], in_=ps[:, :N], func=AF.Identity,
                                                                 bias=c["convb"][:, layer * 4 + m:layer * 4 + m + 1],
                                                                 scale=1.0),
                         reads=[pk] + self.CST, writes=[f"cbf{m}"])
                psM, pkM = self.PS()
                self.mm_group(psM[:, :N], [(c["ones_b"][:], cbf[:, m, :N]) for m in range(4)],
                              reads=[f"cbf{m}" for m in range(4)] + ["ones_b"], writes=[pkM])
                psQ, pkQ = self.PS()
                self.mm_group(psQ[:, :N], [(c["ones_b"][:], csq[:, m, :N]) for m in range(4)],
                              reads=[f"csq{m}" for m in range(4)] + ["ones_b"], writes=[pkQ])
                S.op("act", lambda e: e.activation(out=msq[:, :N], in_=psM[:, :N], func=AF.Square, scale=1.0 / 512),
                     reads=[pkM], writes=["msq"])
                S.op("dve", lambda e: e.scalar_tensor_tensor(out=var[:, :N], in0=psQ[:, :N], scalar=1.0 / 512,
                                                            in1=msq[:, :N], op0=ALU.mult, op1=ALU.subtract),
                     reads=[pkQ, "msq"], writes=["var"])
                S.op("act", lambda e: e.activation(out=sd2[:, :N], in_=var[:, :N], func=AF.Sqrt, scale=1.0, bias=EPS),
                     reads=["var"], writes=["sd2"])
                S.op("dve", lambda e: e.reciprocal(out=rs2[:, :N], in_=sd2[:, :N]), reads=["sd2"], writes=["rs2"])
                for m in range(4):
                    t = ctmp[m % 2]
                    tk = f"ctmp{m % 2}"
                    S.op("dve", lambda e, m=m, t=t: e.scalar_tensor_tensor(out=t[:, :N], in0=psM[:, :N], scalar=-1.0 / 512,
                                                                         in1=cc[:, m, :N], op0=ALU.mult, op1=ALU.add),
                         reads=[pkM, f"cc{m}"], writes=[tk])
                    S.op("dve", lambda e, t=t: e.tensor_tensor(out=t[:, :N], in0=t[:, :N], in1=rs2[:, :N], op=ALU.mult),
                         reads=[tk, "rs2"], writes=[tk])
                    S.op("act", lambda e, m=m, t=t: e.activation(out=cact[:, m, :N], in_=t[:, :N], func=AF.Silu,
                                                               scale=c["clng"][:, layer * 4 + m:layer * 4 + m + 1],
                                                               bias=c["clnb"][:, layer * 4 + m:layer * 4 + m + 1]),
                         reads=[tk] + self.CST, writes=[f"cact{m}"])

            def back2(i):
                if not is_full(i):
                    return
                col0, N = tiles[i]
                s = i % 2
                for cc_ in range(8):
                    ps, pk = self.PS()
                    self.mm_group(ps[:, :N], [(wco[:, k, cc_ * 128:(cc_ + 1) * 128], cact[:, k, :N]) for k in range(4)],
                                  reads=[f"cact{m}" for m in range(4)] + WCO, writes=[pk])
                    r = cc_ % 4
                    S.op("dve", lambda e, ps=ps, cc_=cc_, r=r: e.tensor_tensor(out=tout[r][:, :N], in0=ps[:, :N],
                                                                             in1=sgA[s][:, cc_, :N], op=ALU.mult),
                         reads=[pk, f"sgA{s}_{cc_}"], writes=[f"tout{r}"])
                    S.dma("pool", f"at{r}", lambda e, cc_=cc_, r=r: e.dma_start(
                        out=T_s[cc_ * 128:(cc_ + 1) * 128, col0:col0 + N], in_=tout[r][:, :N]),
                        reads=[f"tout{r}"], writes=[("T", i, cc_)])

            stage0(0)
            stage0(1)
            front(0)
            for i in range(nt):
                back1(i)
                front(i + 1)
                back2(i)
                stage0(i + 2)

    def pass_b(self, layer, tiles, w_in, pool_w, hT_s, T_s):
        nc, S, c = self.nc, self.S, self.c
        with ExitStack() as es:
            def sb(name, shape, dt):
                return es.enter_context(nc.sbuf_tensor(f"b{layer}_{name}", list(shape), dt))
            hb = [sb(f"hb{i}", [128, 8, 512], BF16) for i in range(2)]
            wB = sb("wB", [128, 8, 1536], BF16)
            wp = sb("wp", [128, 4, 256], BF16)
            sgB = [sb(f"sgB{i}", [128, 8, 512], BF16) for i in range(2)]
            L = 16 + 512
            pbuf = [sb(f"pbuf{i}", [128, 4, L], F32) for i in range(3)]
            P1 = sb("P1", [128, 4, L], F32)
            P2 = sb("P2", [128, 4, L], F32)
            pooled = sb("pooled", [128, 4, 512], BF16)
            t16 = sb("t16", [128, 16], F32)
            tin = [[sb(f"tin{j}_{i}", [128, 512], F32) for i in range(8)] for j in range(2)]
            tmpb = [sb(f"tmpb{i}", [128, 512], F32) for i in range(2)]
            tout = [sb(f"tout{i}", [128, 512], F32) for i in range(4)]

            w_l = w_in[layer].rearrange("(k p) n -> p k n", p=128)
            self.load_w(wB[:, :, 1024:1536], w_l[:, :, 1024:1536], "wB2")
            for half in range(2):
                cb = SPLIT_V + 1024 + half * 512
                self.load_w(wB[:, :, half * 512:(half + 1) * 512], w_l[:, :, cb:cb + 512], f"wB{half}")
            WB = ["wB0", "wB1", "wB2"]
            self.load_w(wp[:], pool_w[layer].rearrange("g p n -> p g n"), "wp")
            S.op("pool", lambda e: e.memset(pbuf[0][:, :, 0:16], 0.0), writes=["pbuf0"])
            nt = len(tiles)

            def is_full(i):
                return not (layer == 1 and i == 0)

            def stage0(i):
                if i >= nt:
                    return
                col0, N = tiles[i]
                s = i % 2
                S.dma("sp", f"bh{s}", lambda e: e.dma_start(out=hb[s][:, :, :N],
                                                           in_=hT_s[:, col0:col0 + N].rearrange("(k p) n -> p k n", p=128)),
                      reads=[("hT", i)], writes=[f"hb{s}"])

            def front(i):
                if i >= nt:
                    return
                col0, N = tiles[i]
                s = i % 2
                hk = f"hb{s}"
                s3 = i % 3
                n3 = (i + 1) % 3
                pb = pbuf[s3]
                for g in range(4):
                    ps, pk = self.PS()
                    cb = 1024 + g * 128
                    self.mm_group(ps[:, :N], [(wB[:, k, cb:cb + 128], hb[s][:, k, :N]) for k in range(KD)],
                                  reads=[hk] + WB, writes=[pk])
                    S.op("act", lambda e, ps=ps, g=g: e.activation(out=pb[:, g, 16:16 + N], in_=ps[:, :N], func=AF.Copy),
                         reads=[pk], writes=[f"pbuf{s3}"])
                S.op("pool", lambda e: e.tensor_copy(out=pbuf[n3][:, :, 0:16], in_=pb[:, :, N:N + 16]),
                     reads=[f"pbuf{s3}"], writes=[f"pbuf{n3}"])
                if is_full(i):
                    for cc_ in range(8):
                        S.dma("sp", f"bt{s}_{cc_}", lambda e, cc_=cc_: e.dma_start(
                            out=tin[s][cc_][:, :N], in_=T_s[cc_ * 128:(cc_ + 1) * 128, col0:col0 + N]),
                            reads=[("T", i, cc_)], writes=[f"tin{s}_{cc_}"])
                    for m in range(8):
                        ps, pk = self.PS()
                        self.mm_group(ps[:, :N], [(wB[:, k, m * 128:(m + 1) * 128], hb[s][:, k, :N]) for k in range(KD)],
                                      reads=[hk] + WB, writes=[pk])
                        S.op("act", lambda e, ps=ps, m=m: e.activation(out=sgB[s][:, m, :N], in_=ps[:, :N], func=AF.Sigmoid),
                             reads=[pk], writes=[f"sgB{s}_{m}"])

            def back(i):
                if not is_full(i):
                    return
                col0, N = tiles[i]
                s = i % 2
                pb = pbuf[i % 3]
                pbk = f"pbuf{i % 3}"
                Le = 16 + N
                S.op("pool", lambda e: e.tensor_tensor(out=P1[:, :, 1:Le], in0=pb[:, :, 1:Le], in1=pb[:, :, 0:Le - 1],
                                                      op=ALU.add), reads=[pbk], writes=["P1"])
                S.op("pool", lambda e: e.tensor_tensor(out=P2[:, 1:4, 3:Le], in0=P1[:, 1:4, 3:Le], in1=P1[:, 1:4, 1:Le - 2],
                                                      op=ALU.add), reads=["P1"], writes=["P2"])
                S.op("pool", lambda e: e.tensor_tensor(out=P1[:, 2:4, 7:Le], in0=P2[:, 2:4, 7:Le], in1=P2[:, 2:4, 3:Le - 4],
                                                      op=ALU.add), reads=["P2"], writes=["P1"])
                S.op("pool", lambda e: e.tensor_tensor(out=P2[:, 3:4, 15:Le], in0=P1[:, 3:4, 15:Le], in1=P1[:, 3:4, 7:Le - 8],
                                                      op=ALU.add), reads=["P1"], writes=["P2"])
                srcs = [P1, P2, P1, P2]
                skeys = [["P1"], ["P2"], ["P1"], ["P2"]]
                for g in range(4):
                    wv = float(2 << g)
                    S.op("dve", lambda e, g=g, wv=wv: e.scalar_tensor_tensor(
                        out=pooled[:, g, :N], in0=srcs[g][:, g, 16:16 + N], scalar=1.0 / wv, in1=pb[:, g, 16:16 + N],
                        op0=ALU.mult, op1=ALU.subtract), reads=skeys[g] + [pbk], writes=[f"pooled{g}"])
                    if i == 1:
                        S.op("dve", lambda e, g=g: e.tensor_tensor(out=t16[:], in0=srcs[g][:, g, 16:32],
                                                                  in1=c["rc"][:, g * 16:(g + 1) * 16], op=ALU.mult),
                             reads=skeys[g] + self.CST, writes=["t16"])
                        S.op("dve", lambda e, g=g: e.tensor_tensor(out=pooled[:, g, 0:16], in0=t16[:],
                                                                  in1=pb[:, g, 16:32], op=ALU.subtract),
                             reads=["t16", pbk], writes=[f"pooled{g}"])
                for cc_ in range(8):
                    g, jj = cc_ // 2, cc_ % 2
                    r = cc_ % 4
                    ps, pk = self.PS()
                    self.mm_group(ps[:, :N], [(wp[:, g, jj * 128:(jj + 1) * 128], pooled[:, g, :N])],
                                  reads=[f"pooled{g}", "wp"], writes=[pk])
                    tb = tmpb[cc_ % 2]
                    tbk = f"tmpb{cc_ % 2}"
                    S.op("dve", lambda e, ps=ps, cc_=cc_, tb=tb: e.scalar_tensor_tensor(
                        out=tb[:, :N], in0=ps[:, :N], scalar=c["pscale"][:, layer * 8 + cc_:layer * 8 + cc_ + 1],
                        in1=sgB[s][:, cc_, :N], op0=ALU.mult, op1=ALU.mult),
                        reads=[pk, f"sgB{s}_{cc_}"] + self.CST, writes=[tbk])
                    S.op("dve", lambda e, tb=tb, r=r, cc_=cc_: e.tensor_tensor(out=tout[r][:, :N], in0=tb[:, :N],
                                                                             in1=tin[s][cc_][:, :N], op=ALU.add),
                         reads=[tbk, f"tin{s}_{cc_}"], writes=[f"tout{r}"])
                    S.dma("pool", f"bo{r}", lambda e, cc_=cc_, r=r: e.dma_start(
                        out=T_s[cc_ * 128:(cc_ + 1) * 128, col0:col0 + N], in_=tout[r][:, :N]),
                        reads=[f"tout{r}"], writes=[("T", i, cc_)])

            stage0(0)
            stage0(1)
            front(0)
            for i in range(nt):
                front(i + 1)
                back(i)
                stage0(i + 2)

    def pass_c(self, layer, tiles, xsrc, xsrc_key, w_in, sgu_w_out, w_out, slng_d, slnb_d, wsT_d, bs_d, tril_d,
               hT_s, T_s, xm_s, h2T_s):
        nc, S, c = self.nc, self.S, self.c
        with ExitStack() as es:
            def sb(name, shape, dt):
                return es.enter_context(nc.sbuf_tensor(f"c{layer}_{name}", list(shape), dt))
            hb = [sb(f"hb{i}", [128, 8, 512], BF16) for i in range(2)]
            xt = [sb(f"xt{i}", [128, 8, 512], F32) for i in range(2)]
            wC = sb("wC", [128, 8, 2048], BF16)
            wso = sb("wso", [128, 4, 1024], BF16)
            wo = sb("wo", [128, 8, 1024], BF16)
            sgC = [sb(f"sgC{i}", [128, 8, 512], BF16) for i in range(2)]
            u = sb("u", [128, 4, 512], F32)
            st6 = sb("st6", [128, 4, 6], F32)
            mv = sb("mv", [128, 4, 2], F32)
            sdv = sb("sdv", [128, 4], F32)
            rv = sb("rv", [128, 4], F32)
            nb = sb("nb", [128, 4], F32)
            vtmp = [sb(f"vtmp{i}", [128, 512], F32) for i in range(4)]
            vn = sb("vn", [128, 4, 512], BF16)
            slng = sb("slng", [128, 512], F32)
            slnb = sb("slnb", [128, 512], F32)
            wsf = sb("wsf", [128, 512], F32)
            trl = sb("trl", [128, 128], F32)
            wsm = sb("wsm", [128, 4, 128], BF16)
            bsf = sb("bsf", [1, 512], F32)
            bsr = sb("bsr", [1, 512], F32)
            bsh = sb("bsh", [128, 512], BF16)
            bsl = sb("bsl", [128, 512], BF16)
            e0 = sb("e0", [128, 128], BF16)
            us = sb("us", [128, 4, 512], BF16)
            tin = [sb(f"tin{i}", [128, 512], F32) for i in range(4)]
            tmpc = [sb(f"tmpc{i}", [128, 512], F32) for i in range(2)]
            mixed = sb("mixed", [128, 8, 512], BF16)
            sq = sb("sq", [128, 8, 512], BF16)
            sd = sb("sd", [128, 512], F32)
            rs = sb("rs", [128, 512], F32)
            h2 = sb("h2", [128, 8, 512], BF16)

            w_l = w_in[layer].rearrange("(k p) n -> p k n", p=128)
            self.load_w(wC[:, :, 1536:2048], w_l[:, :, 2048:2560], "wC3")
            self.load_w(wC[:, :, 1024:1536], w_l[:, :, 1536:2048], "wC2")
            for half in range(2):
                cb = SPLIT_V + 2048 + half * 512
                self.load_w(wC[:, :, half * 512:(half + 1) * 512], w_l[:, :, cb:cb + 512], f"wC{half}")
            WC = [f"wC{i}" for i in range(4)]
            swo = sgu_w_out[layer].rearrange("(k p) n -> p k n", p=128)
            wol = w_out[layer].rearrange("(k p) n -> p k n", p=128)
            for half in range(2):
                self.load_w(wso[:, :, half * 512:(half + 1) * 512], swo[:, :, half * 512:(half + 1) * 512], f"wso{half}")
            for half in range(2):
                self.load_w(wo[:, :, half * 512:(half + 1) * 512], wol[:, :, half * 512:(half + 1) * 512], f"wo{half}")
            WSO = ["wso0", "wso1"]
            WO = ["wo0", "wo1"]
            loads = [(slng[:], slng_d[:, layer * 512:(layer + 1) * 512]), (slnb[:], slnb_d[:, layer * 512:(layer + 1) * 512]),
                     (wsf[:], wsT_d[:, layer * 512:(layer + 1) * 512]), (trl[:], tril_d),
                     (bsf[:], bs_d[:, layer * 512:(layer + 1) * 512])]
            for j, (dst, src) in enumerate(loads):
                S.dma("sp", "cl", lambda e, dst=dst, src=src: e.dma_start(out=dst, in_=src), writes=[f"cl{j}"])
            CL = [f"cl{j}" for j in range(len(loads))]
            for hd in range(4):
                S.op("dve", lambda e, hd=hd: e.tensor_tensor(out=wsm[:, hd, :], in0=wsf[:, hd * 128:(hd + 1) * 128],
                                                            in1=trl[:], op=ALU.mult), reads=CL, writes=["wsm"])
            S.op("dve", lambda e: e.memset(bsh[:], 0.0), writes=["bsh"])
            S.op("dve", lambda e: e.memset(bsl[:], 0.0), writes=["bsl"])
            S.op("dve", lambda e: e.memset(e0[:], 0.0), writes=["e0"])
            S.op("dve", lambda e: e.memset(e0[0:1, :], 1.0), reads=["e0"], writes=["e0"])
            S.op("dve", lambda e: e.tensor_copy(out=bsh[0:1, :], in_=bsf[:]), reads=CL + ["bsh"], writes=["bsh"])
            S.op("dve", lambda e: e.tensor_tensor(out=bsr[:], in0=bsf[:], in1=bsh[0:1, :], op=ALU.subtract),
                 reads=CL + ["bsh"], writes=["bsr"])
            S.op("dve", lambda e: e.tensor_copy(out=bsl[0:1, :], in_=bsr[:]), reads=["bsr", "bsl"], writes=["bsl"])
            CL += ["wsm", "bsh", "bsl", "e0"]
            nt = len(tiles)

            def stage0(i):
                if i >= nt:
                    return
                col0, N = tiles[i]
                s = i % 2
                S.dma("sp", f"ch{s}", lambda e: e.dma_start(out=hb[s][:, :, :N],
                                                           in_=hT_s[:, col0:col0 + N].rearrange("(k p) n -> p k n", p=128)),
                      reads=[("hT", i)], writes=[f"hb{s}"])
                S.dma("sp", f"cx{s}", lambda e: e.dma_start(out=xt[s][:, :, :N],
                                                           in_=xsrc[:, col0:col0 + N].rearrange("(k p) n -> p k n", p=128)),
                      reads=[(xsrc_key, i)], writes=[f"xt{s}"])

            def front(i):
                if i >= nt:
                    return
                col0, N = tiles[i]
                s = i % 2
                hk = f"hb{s}"
                nsub = N // 128
                vps = []
                for sub in range(nsub):
                    ps, pk = self.PS()
                    vps.append((ps, pk))
                    self.mm_group(ps[:, :], [(hb[s][:, k, sub * 128:(sub + 1) * 128], wC[:, k, 1536:2048]) for k in range(KD)],
                                  reads=[hk] + WC, writes=[pk])
                    S.op("dve", lambda e, ps=ps, sub=sub: e.bn_stats(out=st6[:, sub, :], in_=ps[:, :]), reads=[pk], writes=[f"st6_{sub}"])
                    S.op("dve", lambda e, sub=sub: e.bn_aggr(out=mv[:, sub, :], in_=st6[:, sub, :]), reads=[f"st6_{sub}"], writes=[f"mv_{sub}"])
                MV = [f"mv_{sub}" for sub in range(nsub)]
                S.op("act", lambda e: e.activation(out=sdv[:, :nsub], in_=mv[:, :nsub, 1], func=AF.Sqrt, scale=1.0, bias=EPS),
                     reads=MV, writes=["sdv"])
                S.op("dve", lambda e: e.reciprocal(out=rv[:, :nsub], in_=sdv[:, :nsub]), reads=["sdv"], writes=["rv"])
                S.op("dve", lambda e: e.scalar_tensor_tensor(out=nb[:, :nsub], in0=mv[:, :nsub, 0], scalar=-1.0, in1=rv[:, :nsub],
                                                            op0=ALU.mult, op1=ALU.mult), reads=MV + ["rv"], writes=["nb"])
                for sub in range(nsub):
                    ps, pk = vps[sub]
                    vt = vtmp[sub]
                    vk = f"vtmp{sub}"
                    S.op("act", lambda e, ps=ps, vt=vt, sub=sub: e.activation(out=vt[:], in_=ps[:, :], func=AF.Identity,
                                                                            scale=rv[:, sub:sub + 1], bias=nb[:, sub:sub + 1]),
                         reads=[pk, "rv", "nb"], writes=[vk])
                    S.op("pool", lambda e, vt=vt: e.tensor_tensor(out=vt[:], in0=vt[:], in1=slng[:], op=ALU.mult),
                         reads=[vk] + CL, writes=[vk])
                    S.op("pool", lambda e, vt=vt, sub=sub: e.tensor_tensor(out=vn[:, sub, :], in0=vt[:], in1=slnb[:], op=ALU.add),
                         reads=[vk] + CL, writes=[f"vn{sub}"])
                for m in range(4):
                    ps, pk = self.PS()
                    cb = 1024 + m * 128
                    self.mm_group(ps[:, :N], [(wC[:, k, cb:cb + 128], hb[s][:, k, :N]) for k in range(KD)],
                                  reads=[hk] + WC, writes=[pk])
                    S.op("act", lambda e, ps=ps, m=m: e.activation(out=u[:, m, :N], in_=ps[:, :N], func=AF.Copy),
                         reads=[pk], writes=[f"u{m}"])
                for m in range(8):
                    ps, pk = self.PS()
                    self.mm_group(ps[:, :N], [(wC[:, k, m * 128:(m + 1) * 128], hb[s][:, k, :N]) for k in range(KD)],
                                  reads=[hk] + WC, writes=[pk])
                    S.op("act", lambda e, ps=ps, m=m: e.activation(out=sgC[s][:, m, :N], in_=ps[:, :N], func=AF.Sigmoid),
                         reads=[pk], writes=[f"sgC{s}_{m}"])

            def back1(i):
                col0, N = tiles[i]
                nsub = N // 128
                for hd in range(4):
                    ps, pk = self.PS()

                    def emit(e, ps=ps, hd=hd):
                        ins = None
                        for sub in range(nsub):
                            o = ps[:, sub * 128:(sub + 1) * 128]
                            e.matmul(o, lhsT=vn[:, sub, hd * 128:(hd + 1) * 128], rhs=wsm[:, hd, :], start=True, stop=False)
                            e.matmul(o, lhsT=e0[:], rhs=bsh[:, hd * 128:(hd + 1) * 128], start=False, stop=False)
                            ins = e.matmul(o, lhsT=e0[:], rhs=bsl[:, hd * 128:(hd + 1) * 128], start=False, stop=True)
                        return ins
                    S.op("pe", emit, reads=[f"vn{sub}" for sub in range(nsub)] + CL, writes=[pk])
                    S.op("dve", lambda e, ps=ps, hd=hd: e.tensor_tensor(out=us[:, hd, :N], in0=ps[:, :N], in1=u[:, hd, :N],
                                                                      op=ALU.mult),
                         reads=[pk, f"u{hd}"], writes=[f"us{hd}"])

            def back2(i):
                col0, N = tiles[i]
                s = i % 2
                for cc_ in range(8):
                    r = cc_ % 4
                    S.dma("sp", f"ct{r}", lambda e, cc_=cc_, r=r: e.dma_start(
                        out=tin[r][:, :N], in_=T_s[cc_ * 128:(cc_ + 1) * 128, col0:col0 + N]),
                        reads=[("T", i, cc_)], writes=[f"tin{r}"])
                    ps, pk = self.PS()
                    self.mm_group(ps[:, :N], [(wso[:, k, cc_ * 128:(cc_ + 1) * 128], us[:, k, :N]) for k in range(4)],
                                  reads=[f"us{k}" for k in range(4)] + WSO, writes=[pk])
                    tc_ = tmpc[cc_ % 2]
                    tck = f"tmpc{cc_ % 2}"
                    S.op("dve", lambda e, ps=ps, cc_=cc_, tc_=tc_: e.tensor_tensor(out=tc_[:, :N], in0=ps[:, :N],
                                                                                 in1=sgC[s][:, cc_, :N], op=ALU.mult),
                         reads=[pk, f"sgC{s}_{cc_}"], writes=[tck])
                    S.op("pool", lambda e, cc_=cc_, tc_=tc_, r=r: e.tensor_tensor(out=mixed[:, cc_, :N], in0=tc_[:, :N],
                                                                                in1=tin[r][:, :N], op=ALU.add),
                         reads=[tck, f"tin{r}"], writes=[f"mixed{cc_}"])
                for cc_ in range(8):
                    ps, pk = self.PS()
                    self.mm_group(ps[:, :N], [(wo[:, k, cc_ * 128:(cc_ + 1) * 128], mixed[:, k, :N]) for k in range(KD)],
                                  reads=[f"mixed{k}" for k in range(8)] + WO, writes=[pk])
                    S.op("dve", lambda e, ps=ps, cc_=cc_: e.tensor_tensor(out=xt[s][:, cc_, :N], in0=ps[:, :N],
                                                                        in1=xt[s][:, cc_, :N], op=ALU.add),
                         reads=[pk, f"xt{s}"], writes=[f"xt{s}"])
                S.dma("pool", f"cxo{s}", lambda e: e.dma_start(out=xm_s[:, col0:col0 + N].rearrange("(k p) n -> p k n", p=128),
                                                              in_=xt[s][:, :, :N]),
                      reads=[f"xt{s}"], writes=[("xm", layer, i)])

            def back4(i):
                col0, N = tiles[i]
                s = i % 2
                self.norm_fm(xt[s], f"xt{s}", N, c["gffn"][:, layer * 8:(layer + 1) * 8], sq, sd, rs, h2, "h2")
                S.dma("pool", "ch2", lambda e: e.dma_start(out=h2T_s[:, col0:col0 + N].rearrange("(k p) n -> p k n", p=128),
                                                          in_=h2[:, :, :N]),
                      reads=["h2"], writes=[("h2T", i)])

            first = 1 if layer == 1 else 0
            stage0(first)
            stage0(first + 1)
            front(first)
            back1(first)
            for i in range(first, nt):
                front(i + 1)
                back2(i)
                if i + 1 < nt:
                    back1(i + 1)
                back4(i)
                stage0(i + 2)

    def ffn_stream(self, pfx, sb, jobs, RM):
        S = self.S
        w1g = [sb(f"{pfx}w1g{i}", [128, 8, 512], BF16) for i in range(2)]
        w3g = [sb(f"{pfx}w3g{i}", [128, 8, 512], BF16) for i in range(2)]
        w2g = [sb(f"{pfx}w2g{i}", [128, 4, 1024], BF16) for i in range(2)]
        Hh = [sb(f"{pfx}Hh{i}", [128, 4, RM], BF16) for i in range(2)]
        sgt = [sb(f"{pfx}sgt{i}", [128, 512], F32) for i in range(2)]
        groups = []
        for ji, job in enumerate(jobs):
            for jg in range((job["nF"] + 3) // 4):
                groups.append((ji, jg))

        def load(gi):
            ji, jg = groups[gi]
            job = jobs[ji]
            b = gi % 2
            nj = min(4, job["nF"] - jg * 4)
            w1v = job["w1"].rearrange("(k p) n -> p k n", p=128)
            w3v = job["w3"].rearrange("(k p) n -> p k n", p=128)
            w2v = job["w2"].rearrange("(j p) n -> p j n", p=128)
            self.load_w(w1g[b][:, :, :nj * 128], w1v[:, :, jg * 512:jg * 512 + nj * 128], f"{pfx}w1g{b}")
            self.load_w(w3g[b][:, :, :nj * 128], w3v[:, :, jg * 512:jg * 512 + nj * 128], f"{pfx}w3g{b}")
            self.load_w(w2g[b][:, :nj, :], w2v[:, jg * 4:jg * 4 + nj, :], f"{pfx}w2g{b}")

        load(0)
        jobs[0]["pre"]()
        n_sg = 0
        for gi, (ji, jg) in enumerate(groups):
            job = jobs[ji]
            b = gi % 2
            nj = min(4, job["nF"] - jg * 4)
            if gi + 1 < len(groups):
                load(gi + 1)
                if groups[gi + 1][0] != ji and jobs[ji + 1].get("early_pre"):
                    jobs[ji + 1]["pre"]()
            if jg == 0 and ji > 0 and not job.get("early_pre"):
                job["pre"]()
            hs, hs_keys = job["hs"], job["hs_keys"]
            for (c0, n) in job["col_tiles"]:
                for jj in range(nj):
                    psG, pkG = self.PS()
                    self.mm_group(psG[:, :n], [(w1g[b][:, k, jj * 128:(jj + 1) * 128], hs[:, k, c0:c0 + n]) for k in range(KD)],
                                  reads=hs_keys + [f"{pfx}w1g{b}"], writes=[pkG])
                    psU, pkU = self.PS()
                    self.mm_group(psU[:, :n], [(w3g[b][:, k, jj * 128:(jj + 1) * 128], hs[:, k, c0:c0 + n]) for k in range(KD)],
                                  reads=hs_keys + [f"{pfx}w3g{b}"], writes=[pkU])
                    st = sgt[n_sg % 2]
                    sk = f"{pfx}sgt{n_sg % 2}"
                    n_sg += 1
                    S.op("act", lambda e, psG=psG, st=st, n=n: e.activation(out=st[:, :n], in_=psG[:, :n], func=AF.Silu),
                         reads=[pkG], writes=[sk])
                    S.op("dve", lambda e, psU=psU, st=st, n=n, jj=jj, c0=c0, b=b: e.tensor_tensor(
                        out=Hh[b][:, jj, c0:c0 + n], in0=psU[:, :n], in1=st[:, :n], op=ALU.mult),
                        reads=[pkU, sk], writes=[f"{pfx}Hh{b}"])
            job["emit_y"](jg, nj, Hh[b], [f"{pfx}Hh{b}"], w2g[b], [f"{pfx}w2g{b}"])
            if gi + 1 == len(groups) or groups[gi + 1][0] != ji:
                job["post"]()

    def dense_ffn(self, tiles, dw1, dw3, dw2, h2T_s, xm_s, xf_s, xg_s, ys_s):
        nc, S, c = self.nc, self.S, self.c
        supers = [[0, 1, 2, 3, 4], [5, 6, 7, 8]]
        with ExitStack() as es:
            def sb(name, shape, dt):
                return es.enter_context(nc.sbuf_tensor(f"f_{name}", list(shape), dt))
            RM = 2176
            hs = sb("hs", [128, 8, RM], BF16)
            ya = sb("ya", [128, 8, RM], F32)
            zrow = sb("zrow", [128, 4, 1024], BF16)
            S.op("pool", lambda e: e.memset(zrow[:], 0.0), writes=["zrow"])
            zrowf = sb("zrowf", [128, 1024], F32)
            S.op("pool", lambda e: e.memset(zrowf[:], 0.0), writes=["zrowf"])

            def init_xg():
                for j in range(NSLOT // 512):
                    S.dma("sp", f"xgi{j % 4}", lambda e, j=j: e.dma_start(
                        out=xg_s[j * 512:(j + 1) * 512, :].rearrange("(b p) n -> p b n", p=128), in_=zrow[:]),
                        reads=["zrow"], writes=[f"xgi{j % 4}"])
                S.dma("sp", "xgi0", lambda e: e.dma_start(out=ys_s[NSLOT:NSLOT + 128, :], in_=zrowf[:]),
                      reads=["zrowf"], writes=["ysz"])
            jobs = []
            for si, st_ in enumerate(supers):
                base = tiles[st_[0]][0]
                col_tiles = [(tiles[t][0] - base, tiles[t][1]) for t in st_]
                R = sum(n for _, n in col_tiles)

                def pre(base=base, R=R, st_=st_, si=si):
                    S.dma("sp", "fh", lambda e: e.dma_start(
                        out=hs[:, :, :R], in_=h2T_s[:, base:base + R].rearrange("(k p) n -> p k n", p=128)),
                        reads=[("h2T", t) for t in st_], writes=["f_hs"])
                    S.dma("sp", "fx", lambda e: e.dma_start(
                        out=ya[:, :, :R], in_=xm_s[:, base:base + R].rearrange("(k p) n -> p k n", p=128)),
                        reads=[("xm", 0, t) for t in st_], writes=["f_ya"])
                    if si == 1:
                        init_xg()

                def emit_y(jg, nj, Hh, hkeys, w2g, wkeys, col_tiles=col_tiles):
                    for (c0, n) in col_tiles:
                        for cc_ in range(8):
                            ps, pk = self.PS()
                            self.mm_group(ps[:, :n], [(w2g[:, jj, cc_ * 128:(cc_ + 1) * 128], Hh[:, jj, c0:c0 + n])
                                                      for jj in range(nj)], reads=hkeys + wkeys, writes=[pk])
                            S.op("dve", lambda e, ps=ps, cc_=cc_, c0=c0, n=n: e.tensor_tensor(
                                out=ya[:, cc_, c0:c0 + n], in0=ps[:, :n], in1=ya[:, cc_, c0:c0 + n], op=ALU.add),
                                reads=[pk, "f_ya"], writes=["f_ya"])

                def post(si=si, base=base, R=R, st_=st_):
                    if si == 0:
                        S.op("dve", lambda e: e.tensor_scalar(out=ya[:, :, 0:HALO], in0=ya[:, :, 0:HALO],
                                                             scalar1=c["hmask"][:, 0:1], scalar2=None, op0=ALU.mult),
                             reads=["f_ya"] + self.CST, writes=["f_ya"])
                    S.dma("sp", "fo", lambda e: e.dma_start(
                        out=xf_s[:, base:base + R].rearrange("(k p) n -> p k n", p=128), in_=ya[:, :, :R]),
                        reads=["f_ya"], writes=[("xf0", t) for t in st_])

                jobs.append(dict(hs=hs, hs_keys=["f_hs"], col_tiles=col_tiles, w1=dw1[0], w3=dw3[0], w2=dw2[0],
                                 nF=D_FF // 128, pre=pre, emit_y=emit_y, post=post))
            self.ffn_stream("f", sb, jobs, RM)

    def moe(self, ew1, ew3, ew2, wr_d, tri_d, eoff_d, fing_d, h2T_s, xm_s, xg_s, ys_s, cnt_s, out_d):
        nc, S, c = self.nc, self.S, self.c
        NT = T_OWN // 128
        NE = NT * 8
        with ExitStack() as gs2:
            def sbg(name, shape, dt):
                return gs2.enter_context(nc.sbuf_tensor(f"m_{name}", list(shape), dt))
            gw = sbg("gw", [128, NT, 2], F32)
            idx = sbg("idx", [128, NT, 2], I32)
            idxg = sbg("idxg", [128, NT, 2], I32)
            with ExitStack() as es:
                def sb(name, shape, dt):
                    return es.enter_context(nc.sbuf_tensor(f"r_{name}", list(shape), dt))
                wrf = sb("wrf", [128, 64], F32)
                wrb = sb("wrb", [128, 8, 8], BF16)
                trif = sb("trif", [128, 128], F32)
                trib = sb("trib", [128, 128], BF16)
                eoff = sb("eoff", [128, NE], F32)
                ht = sb("ht", [128, 8, T_OWN], BF16)
                lg = sb("lg", [128, NT, 8], F32)
                lg2 = sb("lg2", [128, NT, 8], F32)
                m1v = sb("m1v", [128, NT], F32)
                m2v = sb("m2v", [128, NT], F32)
                k1 = sb("k1", [128, NT, 8], F32)
                k2 = sb("k2", [128, NT, 8], F32)
                dd = sb("dd", [128, NT], F32)
                selb = sb("selb", [128, NE], BF16)
                csA = sb("csA", [128, NT, 8], F32)
                csB = sb("csB", [128, NT, 8], F32)
                cs0 = sb("cs0", [128, NT, 8], F32)
                pos = sb("pos", [128, NT, 8], F32)
                ovf = sb("ovf", [128, NT, 8], F32)
                prod = sb("prod", [128, NT, 8], F32)
                idf = sb("idf", [128, 2, NT], F32)
                hrow = [sb(f"hrow{i}", [128, 1024], BF16) for i in range(4)]
                XGI = [f"xgi{j}" for j in range(4)]
                for j, (dst, src) in enumerate([(wrf[:], wr_d), (trif[:], tri_d), (eoff[:], eoff_d)]):
                    S.dma("sp", "rl", lambda e, dst=dst, src=src: e.dma_start(out=dst, in_=src), writes=[f"rl{j}"])
                RL = ["rl0", "rl1", "rl2"]
                S.op("dve", lambda e: e.tensor_copy(out=wrb[:].rearrange("p k e -> p (k e)"), in_=wrf[:]), reads=RL, writes=["wrb"])
                S.op("dve", lambda e: e.tensor_copy(out=trib[:], in_=trif[:]), reads=RL, writes=["trib"])
                RL += ["wrb", "trib"]
                HT = []
                for j in range(8):
                    col0 = HALO + j * 512
                    S.dma("sp", "rh", lambda e, j=j, col0=col0: e.dma_start(
                        out=ht[:, :, j * 512:(j + 1) * 512], in_=h2T_s[:, col0:col0 + 512].rearrange("(k p) n -> p k n", p=128)),
                        reads=[("h2T", 1 + j)], writes=[f"rht{j}"])
                    HT.append(f"rht{j}")
                psL, pkL = self.PS()

                def emit_l(e):
                    ins = None
                    for i in range(NT):
                        for k in range(KD):
                            ins = e.matmul(psL[:, i * 8:(i + 1) * 8], lhsT=ht[:, k, i * 128:(i + 1) * 128], rhs=wrb[:, k, :],
                                           start=(k == 0), stop=(k == KD - 1))
                    return ins
                S.op("pe", emit_l, reads=HT + RL, writes=[pkL])
                lgf = lg[:].rearrange("p t e -> p (t e)")
                S.op("act", lambda e: e.activation(out=lgf, in_=psL[:, 0:NE], func=AF.Copy), reads=[pkL], writes=["lg"])
                S.op("dve", lambda e: e.tensor_reduce(out=m1v[:], in_=lg[:], axis=mybir.AxisListType.X, op=ALU.max),
                     reads=["lg"], writes=["m1v"])
                S.op("dve", lambda e: e.tensor_tensor(out=k1[:], in0=lg[:], in1=m1v[:].unsqueeze(2).broadcast_to([128, NT, 8]),
                                                     op=ALU.is_equal), reads=["lg", "m1v"], writes=["k1"])
                S.op("dve", lambda e: e.scalar_tensor_tensor(out=lg2[:].rearrange("p t e -> p (t e)"),
                                                            in0=k1[:].rearrange("p t e -> p (t e)"), scalar=-1.0e30,
                                                            in1=lgf, op0=ALU.mult, op1=ALU.add),
                     reads=["k1", "lg"], writes=["lg2"])
                S.op("dve", lambda e: e.tensor_reduce(out=m2v[:], in_=lg2[:], axis=mybir.AxisListType.X, op=ALU.max),
                     reads=["lg2"], writes=["m2v"])
                S.op("dve", lambda e: e.tensor_tensor(out=k2[:], in0=lg2[:], in1=m2v[:].unsqueeze(2).broadcast_to([128, NT, 8]),
                                                     op=ALU.is_equal), reads=["lg2", "m2v"], writes=["k2"])
                S.op("dve", lambda e: e.tensor_tensor(out=dd[:], in0=m1v[:], in1=m2v[:], op=ALU.subtract),
                     reads=["m1v", "m2v"], writes=["dd"])
                S.op("act", lambda e: e.activation(out=gw[:, :, 0], in_=dd[:], func=AF.Sigmoid), reads=["dd"], writes=["gw"])
                S.op("act", lambda e: e.activation(out=gw[:, :, 1], in_=dd[:], func=AF.Sigmoid, scale=-1.0),
                     reads=["dd"], writes=["gw"])
                S.op("dve", lambda e: e.tensor_tensor(out=selb[:], in0=k1[:].rearrange("p t e -> p (t e)"),
                                                     in1=k2[:].rearrange("p t e -> p (t e)"), op=ALU.add),
                     reads=["k1", "k2"], writes=["selb"])
                psP, pkP = self.PS()

                def emit_p(e):
                    e.matmul(psP[:, 0:NE], lhsT=trib[:], rhs=selb[:], start=True, stop=True)
                    return e.matmul(psP[:, NE:2 * NE], lhsT=c["ones_b"][:], rhs=selb[:], start=True, stop=True)
                S.op("pe", emit_p, reads=["selb", "ones_b"] + RL, writes=[pkP])
                S.op("act", lambda e: e.activation(out=cs0[:].rearrange("p t e -> p (t e)"), in_=psP[:, NE:2 * NE], func=AF.Copy),
                     reads=[pkP], writes=["cs0"])
                cur, ck = cs0, "cs0"
                bufs = [(csA, "csA"), (csB, "csB")]
                for si, sh in enumerate((1, 2, 4, 8, 16)):
                    dst, dk = bufs[si % 2]
                    S.op("dve", lambda e, cur=cur, dst=dst, sh=sh: e.tensor_tensor(out=dst[:, sh:, :], in0=cur[:, sh:, :],
                                                                                 in1=cur[:, :NT - sh, :], op=ALU.add),
                         reads=[ck], writes=[dk])
                    S.op("dve", lambda e, cur=cur, dst=dst, sh=sh: e.tensor_copy(out=dst[:, :sh, :], in_=cur[:, :sh, :]),
                         reads=[ck, dk], writes=[dk])
                    cur, ck = dst, dk
                S.dma("sp", "cnto", lambda e, cur=cur: e.dma_start(out=cnt_s, in_=cur[:, NT - 1, :]), reads=[ck], writes=["cnt_s"])
                S.op("dve", lambda e, cur=cur: e.tensor_tensor(out=pos[:], in0=cur[:], in1=cs0[:], op=ALU.subtract),
                     reads=[ck, "cs0"], writes=["pos"])
                S.op("dve", lambda e: e.tensor_tensor(out=pos[:].rearrange("p t e -> p (t e)"), in0=psP[:, 0:NE],
                                                     in1=pos[:].rearrange("p t e -> p (t e)"), op=ALU.add),
                     reads=[pkP, "pos"], writes=["pos"])
                S.op("dve", lambda e: e.tensor_scalar(out=ovf[:], in0=pos[:], scalar1=float(CAP), scalar2=BIGIDX,
                                                     op0=ALU.is_ge, op1=ALU.mult), reads=["pos"], writes=["ovf"])
                S.op("dve", lambda e: e.tensor_tensor(out=pos[:].rearrange("p t e -> p (t e)"),
                                                     in0=pos[:].rearrange("p t e -> p (t e)"), in1=eoff[:], op=ALU.add),
                     reads=["pos"] + RL, writes=["pos"])
                S.op("dve", lambda e: e.tensor_tensor(out=pos[:], in0=pos[:], in1=ovf[:], op=ALU.add),
                     reads=["pos", "ovf"], writes=["pos"])
                for kk, km in enumerate((k1, k2)):
                    S.op("dve", lambda e, km=km: e.tensor_tensor(out=prod[:], in0=km[:], in1=pos[:], op=ALU.mult),
                         reads=["k1", "k2", "pos"], writes=["prod"])
                    S.op("dve", lambda e, kk=kk: e.tensor_reduce(out=idf[:, kk, :], in_=prod[:], axis=mybir.AxisListType.X,
                                                                op=ALU.add), reads=["prod"], writes=["idf"])
                    S.op("dve", lambda e, kk=kk: e.tensor_copy(out=idx[:, :, kk], in_=idf[:, kk, :]), reads=["idf"], writes=["idx"])
                    S.op("dve", lambda e, kk=kk: e.tensor_scalar(out=idf[:, kk, :], in0=idf[:, kk, :], scalar1=float(NSLOT),
                                                                scalar2=None, op0=ALU.min), reads=["idf", "idx"], writes=["idf"])
                    S.op("dve", lambda e, kk=kk: e.tensor_copy(out=idxg[:, :, kk], in_=idf[:, kk, :]), reads=["idf"], writes=["idxg"])
                for i in range(NT):
                    s = i % 4
                    psT, pkT = self.PS()
                    psTb = psT.bitcast(BF16)

                    def emit_t(e, psTb=psTb, i=i):
                        ins = None
                        for k in range(KD):
                            ins = e.transpose(psTb[:, k * 128:(k + 1) * 128], ht[:, k, i * 128:(i + 1) * 128], c["ident_b"][:])
                        return ins
                    S.op("pe", emit_t, reads=HT + ["ident_b"], writes=[pkT])
                    S.op("act", lambda e, psTb=psTb, s=s: e.activation(out=hrow[s][:], in_=psTb[:, 0:1024], func=AF.Copy),
                         reads=[pkT], writes=[f"hrow{s}"])
                    for kk in range(2):
                        S.dma("pool", f"rs{kk}_{s}", lambda e, s=s, i=i, kk=kk: e.indirect_dma_start(
                            out=xg_s[:, :], out_offset=bass.IndirectOffsetOnAxis(ap=idx[:, i, kk:kk + 1], axis=0),
                            in_=hrow[s][:], in_offset=None, bounds_check=S.bnd, oob_is_err=False),
                            reads=[f"hrow{s}", "idx"] + XGI, writes=["xg"])
            S.barrier()

            with ExitStack() as es:
                def sb(name, shape, dt):
                    return es.enter_context(nc.sbuf_tensor(f"e_{name}", list(shape), dt))
                NB = CAP // 128
                xr = [sb(f"xr{i}", [128, 1024], BF16) for i in range(2)]
                xgT = [sb(f"xgT{i}", [128, 8, CAP], BF16) for i in range(2)]
                yacc = sb("yacc", [128, NB, 1024], F32)
                col_tiles = []
                c0 = 0
                while c0 < CAP:
                    n = min(512, CAP - c0)
                    col_tiles.append((c0, n))
                    c0 += n
                jobs = []
                for ex in range(NEXP):
                    xb = ex % 2

                    def pre(ex=ex, xb=xb):
                        for blk in range(NB):
                            s = blk % 2
                            r0 = ex * CAP + blk * 128
                            S.dma("sp", f"ex{s}", lambda e, s=s, r0=r0: e.dma_start(out=xr[s][:], in_=xg_s[r0:r0 + 128, :]),
                                  reads=["xg"], writes=[f"xr{s}"])
                            psT, pkT = self.PS()
                            psTb = psT.bitcast(BF16)

                            def emit_t(e, psTb=psTb, s=s):
                                ins = None
                                for k in range(KD):
                                    ins = e.transpose(psTb[:, k * 128:(k + 1) * 128], xr[s][:, k * 128:(k + 1) * 128],
                                                      c["ident_b"][:])
                                return ins
                            S.op("pe", emit_t, reads=[f"xr{s}", "ident_b"], writes=[pkT])
                            S.op("act", lambda e, psTb=psTb, blk=blk: e.activation(
                                out=xgT[xb][:, :, blk * 128:(blk + 1) * 128],
                                in_=psTb[:, 0:1024].rearrange("p (k n) -> p k n", k=8), func=AF.Copy),
                                reads=[pkT], writes=[f"xgT{xb}"])

                    def emit_y(jg, nj, Hh, hkeys, w2g, wkeys):
                        for blk in range(NB):
                            for half in range(2):
                                ps, pk = self.PS()
                                self.mm_group(ps[:, :], [(Hh[:, jj, blk * 128:(blk + 1) * 128], w2g[:, jj, half * 512:(half + 1) * 512])
                                                         for jj in range(nj)], reads=hkeys + wkeys, writes=[pk])
                                dst = yacc[:, blk, half * 512:(half + 1) * 512]
                                if jg == 0:
                                    S.op("act", lambda e, ps=ps, dst=dst: e.activation(out=dst, in_=ps[:, :], func=AF.Copy),
                                         reads=[pk], writes=["yacc"])
                                else:
                                    S.op("dve", lambda e, ps=ps, dst=dst: e.tensor_tensor(out=dst, in0=ps[:, :], in1=dst, op=ALU.add),
                                         reads=[pk, "yacc"], writes=["yacc"])

                    def post(ex=ex):
                        S.dma("sp", "ey", lambda e: e.dma_start(
                            out=ys_s[ex * CAP:(ex + 1) * CAP, :].rearrange("(b p) n -> p b n", p=128), in_=yacc[:]),
                            reads=["yacc"], writes=["ys"])

                    jobs.append(dict(hs=xgT[xb], hs_keys=[f"xgT{xb}"], col_tiles=col_tiles, w1=ew1[0, ex], w3=ew3[0, ex],
                                     w2=ew2[0, ex], nF=D_EXP // 128, pre=pre, emit_y=emit_y, post=post, early_pre=True))
                self.ffn_stream("x", sb, jobs, CAP)
            S.barrier()

            with ExitStack() as es:
                def sb(name, shape, dt):
                    return es.enter_context(nc.sbuf_tensor(f"o_{name}", list(shape), dt))
                fing = sb("fing", [128, 1024], F32)
                xt4 = [sb(f"xt4_{i}", [128, 8, 512], F32) for i in range(2)]
                y1 = [sb(f"y1{i}", [128, 1024], F32) for i in range(4)]
                y2 = [sb(f"y2{i}", [128, 1024], F32) for i in range(4)]
                acc = [sb(f"acc{i}", [128, 1024], F32) for i in range(4)]
                jk = sb("jk", [128, 1024], F32)
                ssq = [sb(f"ssq{i}", [128, 1], F32) for i in range(2)]
                sdo = [sb(f"sdo{i}", [128, 1], F32) for i in range(2)]
                ro = [sb(f"ro{i}", [128, 1], F32) for i in range(2)]
                S.dma("sp", "ol", lambda e: e.dma_start(out=fing[:], in_=fing_d), writes=["fing"])

                def part1(i):
                    s = i % 4
                    ti = 1 + i // 4
                    xb = (i // 4) % 2
                    xo = (i % 4) * 128
                    if i % 4 == 0:
                        col0 = HALO + i * 128
                        S.dma("sp", f"ox{xb}", lambda e, xb=xb, col0=col0: e.dma_start(
                            out=xt4[xb][:], in_=xm_s[:, col0:col0 + 512].rearrange("(k p) n -> p k n", p=128)),
                            reads=[("xm", 1, ti)], writes=[f"oxt{xb}"])
                    for kk, yb in enumerate((y1, y2)):
                        S.dma("pool", f"og{kk}{s}", lambda e, yb=yb, s=s, i=i, kk=kk: e.indirect_dma_start(
                            out=yb[s][:], out_offset=None, in_=ys_s[:, :],
                            in_offset=bass.IndirectOffsetOnAxis(ap=idxg[:, i, kk:kk + 1], axis=0),
                            bounds_check=S.bnd2, oob_is_err=False),
                            reads=["ys", "idxg"], writes=[f"oy{kk}{s}"])
                    for half in range(2):
                        ps, pk = self.PS()

                        def emit_t(e, ps=ps, xb=xb, xo=xo, half=half):
                            ins = None
                            for k in range(4):
                                kk_ = half * 4 + k
                                ins = e.transpose(ps[:, k * 128:(k + 1) * 128], xt4[xb][:, kk_, xo:xo + 128], c["ident_f"][:])
                            return ins
                        S.op("pe", emit_t, reads=[f"oxt{xb}"] + self.CST, writes=[pk])
                        sl = slice(half * 512, (half + 1) * 512)
                        S.op("dve", lambda e, ps=ps, s=s, sl=sl, i=i: e.scalar_tensor_tensor(
                            out=acc[s][:, sl], in0=y1[s][:, sl], scalar=gw[:, i, 0:1], in1=ps[:, :], op0=ALU.mult, op1=ALU.add),
                            reads=[pk, f"oy0{s}", "gw"], writes=[f"oacc{s}"])
                        S.op("dve", lambda e, s=s, sl=sl, i=i: e.scalar_tensor_tensor(
                            out=acc[s][:, sl], in0=y2[s][:, sl], scalar=gw[:, i, 1:2], in1=acc[s][:, sl], op0=ALU.mult, op1=ALU.add),
                            reads=[f"oy1{s}", "gw", f"oacc{s}"], writes=[f"oacc{s}"])
                    r = i % 2
                    S.op("act", lambda e, s=s, r=r: e.activation(out=jk[:], in_=acc[s][:], func=AF.Square, accum_out=ssq[r][:]),
                         reads=[f"oacc{s}"], writes=["jk", f"ssq{r}"])
                    S.op("act", lambda e, r=r: e.activation(out=sdo[r][:], in_=ssq[r][:], func=AF.Sqrt, scale=1.0 / D, bias=EPS),
                         reads=[f"ssq{r}"], writes=[f"sdo{r}"])

                def part2(i):
                    s = i % 4
                    r = i % 2
                    S.op("dve", lambda e, r=r: e.reciprocal(out=ro[r][:], in_=sdo[r][:]), reads=[f"sdo{r}"], writes=[f"ro{r}"])
                    S.op("dve", lambda e, s=s, r=r: e.scalar_tensor_tensor(out=acc[s][:], in0=acc[s][:], scalar=ro[r][:, 0:1],
                                                                          in1=fing[:], op0=ALU.mult, op1=ALU.mult),
                         reads=[f"oacc{s}", f"ro{r}", "fing"], writes=[f"oacc{s}"])
                    S.dma("act", f"oo{s}", lambda e, s=s, i=i: e.dma_start(out=out_d[i * 128:(i + 1) * 128, :], in_=acc[s][:]),
                          reads=[f"oacc{s}"], writes=[("out", i)])

                part1(0)
                for i in range(NT):
                    if i + 1 < NT:
                        part1(i + 1)
                    part2(i)


def host_inputs(inputs):
    f = np.float32
    x = np.asarray(inputs["x"], dtype=f)

    def pc(v, nchunk):
        v = np.asarray(v, dtype=f)
        L = v.shape[0]
        return np.ascontiguousarray(v.reshape(L, nchunk, 128).transpose(2, 0, 1).reshape(128, L * nchunk))

    shared = {}
    shared["ident"] = np.eye(128, dtype=f)
    jj, ii = np.meshgrid(np.arange(128), np.arange(128), indexing="ij")
    shared["tril"] = (jj <= ii).astype(f)
    shared["tri"] = (jj < ii).astype(f)
    shared["eoff"] = np.ascontiguousarray(np.broadcast_to(np.tile((np.arange(8) * CAP).astype(f), 32)[None, :], (128, 256)))
    shared["gmix"] = pc(inputs["mix_norm_g"], 8)
    shared["gffn"] = pc(inputs["ffn_norm_g"], 8)
    cw = np.asarray(inputs["conv_w"], dtype=f)
    shared["convw"] = np.ascontiguousarray(cw.reshape(2, 31, 4, 128).transpose(3, 0, 2, 1).reshape(128, 248))
    shared["convb"] = pc(inputs["conv_b"], 4)
    shared["clng"] = pc(inputs["conv_ln_g"], 4)
    shared["clnb"] = pc(inputs["conv_ln_b"], 4)
    shared["pscale"] = pc(inputs["pool_scale"], 8)
    shared["slng"] = np.ascontiguousarray(np.broadcast_to(np.asarray(inputs["sgu_ln_g"], dtype=f).reshape(1, 1024), (128, 1024)))
    shared["slnb"] = np.ascontiguousarray(np.broadcast_to(np.asarray(inputs["sgu_ln_b"], dtype=f).reshape(1, 1024), (128, 1024)))
    ws = np.asarray(inputs["sgu_w_s"], dtype=f)
    shared["wsT"] = np.ascontiguousarray(ws.transpose(3, 0, 1, 2).reshape(128, 1024))
    shared["bs"] = np.ascontiguousarray(np.asarray(inputs["sgu_b_s"], dtype=f).reshape(1, 1024))
    shared["fing"] = np.ascontiguousarray(np.broadcast_to(np.asarray(inputs["final_norm_g"], dtype=f).reshape(1, 1024), (128, 1024)))
    wr = np.asarray(inputs["router_w"], dtype=f)[0]
    shared["wr"] = np.ascontiguousarray(wr.reshape(8, 128, 8).transpose(1, 0, 2).reshape(128, 64))
    for k in ("w_in", "conv_w_out", "pool_w", "sgu_w_out", "w_out", "dense_w1", "dense_w3", "dense_w2",
              "expert_w1", "expert_w3", "expert_w2"):
        shared[k] = np.ascontiguousarray(np.asarray(inputs[k], dtype=f))
    in_maps = []
    for core in range(NCORES):
        b, half = core // 2, core % 2
        t0 = half * T_OWN
        xs = np.zeros((TT, D), dtype=f)
        xs[HALO:] = x[b, t0:t0 + T_OWN]
        if half == 1:
            xs[:HALO] = x[b, t0 - HALO:t0]
        m = dict(shared)
        m["xT"] = np.ascontiguousarray(xs.T)
        m["hmask"] = np.full((128, 1), float(half), dtype=f)
        rc = np.zeros((4, 16), dtype=f)
        for g in range(4):
            w = 2 << g
            for t in range(16):
                rc[g, t] = 1.0 / (min(t + 1, w) if half == 0 else w)
        m["rc"] = np.ascontiguousarray(np.broadcast_to(rc.reshape(1, 64), (128, 64)))
        in_maps.append(m)
    return in_maps


_NC_CACHE = {}


def kernel(**inputs):
    in_maps = host_inputs(inputs)
    if "nc" not in _NC_CACHE:
        _NC_CACHE["nc"] = Builder().build()
    nc = _NC_CACHE["nc"]
    res = run_bass_kernel_spmd(nc, in_maps, core_ids=list(range(NCORES)))
    out = np.empty((4, 8192, D), dtype=np.float32)
    for core in range(NCORES):
        b, half = core // 2, core % 2
        out[b, half * T_OWN:(half + 1) * T_OWN] = res.results[core]["out"]
    return out
```
